# Optimizing a Trainium2 kernel written in Bass

```python
import jax, jax.numpy as jnp
from jax import lax
import numpy as np

D_MODEL = 1024
BATCH = 8
SEQ = 4096
DEPTH = 2

GRID_W = 64
CTX_LEN = 256
N_MIXERS = 2
N_HEADS = 8
N_KV_HEADS = 2
HEAD_DIM = 128
KV_GROUP = N_HEADS // N_KV_HEADS
ROPE_THETA = 10000.0
Q_BLOCK = 128
CONV_WIDTH = 3
N_EXPERTS = 256
TOP_K = 8
N_GROUPS = 8
TOPK_GROUPS = 4
EXPERT_FF = 256
SHARED_FF = 256
ROUTED_SCALE = 2.5
MOE_BLOCK = 128
LN_EPS = 1e-5
QK_EPS = 1e-6
DN_ALPHA = (2 * DEPTH) ** 0.25
DN_BETA = (8 * DEPTH) ** -0.25

kernel_name = "hybrid_gqa_shortconv_moe_dit"


def layer_norm(x, g, b):
    xf = x.astype(jnp.float32)
    mu = xf.mean(-1, keepdims=True)
    var = jnp.square(xf - mu).mean(-1, keepdims=True)
    return ((xf - mu) * lax.rsqrt(var + LN_EPS)).astype(x.dtype) * g + b


def rms_norm(x, g):
    xf = x.astype(jnp.float32)
    return (xf * lax.rsqrt(jnp.mean(xf * xf, -1, keepdims=True) + QK_EPS)).astype(x.dtype) * g


def axial_rope_angles(n_tok):
    rows = n_tok // GRID_W
    row = jnp.repeat(jnp.arange(rows, dtype=jnp.float32), GRID_W)
    col = jnp.tile(jnp.arange(GRID_W, dtype=jnp.float32), rows)
    axis_dim = HEAD_DIM // 2
    freqs = ROPE_THETA ** (-jnp.arange(0, axis_dim, 2, dtype=jnp.float32) / axis_dim)
    return jnp.concatenate([row[:, None] * freqs, col[:, None] * freqs], axis=-1)


def apply_rope(x, ang):
    xf = x.astype(jnp.float32).reshape(*x.shape[:-1], HEAD_DIM // 2, 2)
    cos = jnp.cos(ang)[None, :, None, :]
    sin = jnp.sin(ang)[None, :, None, :]
    x0, x1 = xf[..., 0], xf[..., 1]
    out = jnp.stack([x0 * cos - x1 * sin, x0 * sin + x1 * cos], axis=-1)
    return out.reshape(x.shape).astype(x.dtype)


def gqa(q, k, v):
    b, sq = q.shape[:2]
    qg = q.reshape(b, sq, N_KV_HEADS, KV_GROUP, HEAD_DIM)
    s = jnp.einsum('bqkgd,bskd->bkgqs', qg, k) * (HEAD_DIM ** -0.5)
    p = jax.nn.softmax(s.astype(jnp.float32), axis=-1).astype(v.dtype)
    o = jnp.einsum('bkgqs,bskd->bqkgd', p, v)
    return o.reshape(b, sq, N_HEADS * HEAD_DIM)


def attention_mixer(hx, hc, w_qkv, q_g, k_g, w_o, ctx_out):
    hq = N_HEADS * HEAD_DIM
    kd = N_KV_HEADS * HEAD_DIM

    def kv_heads(kv):
        k = kv[..., :kd].reshape(*kv.shape[:2], N_KV_HEADS, HEAD_DIM)
        v = kv[..., kd:].reshape(*kv.shape[:2], N_KV_HEADS, HEAD_DIM)
        return rms_norm(k, k_g), v

    qkv = hx @ w_qkv
    qx = rms_norm(qkv[..., :hq].reshape(*hx.shape[:2], N_HEADS, HEAD_DIM), q_g)
    kx, vx = kv_heads(qkv[..., hq:])
    ang = axial_rope_angles(hx.shape[1])
    qx, kx = apply_rope(qx, ang), apply_rope(kx, ang)
    if ctx_out:
        qkv_c = hc @ w_qkv
        qc = rms_norm(qkv_c[..., :hq].reshape(*hc.shape[:2], N_HEADS, HEAD_DIM), q_g)
        kc, vc = kv_heads(qkv_c[..., hq:])
    else:
        kc, vc = kv_heads(hc @ w_qkv[:, hq:])
    k_all = jnp.concatenate([kc, kx], axis=1)
    v_all = jnp.concatenate([vc, vx], axis=1)
    b, n = hx.shape[:2]
    nb = n // Q_BLOCK
    q_blocks = qx.reshape(b, nb, Q_BLOCK, N_HEADS, HEAD_DIM).transpose(1, 0, 2, 3, 4)
    o = lax.map(lambda qb: gqa(qb, k_all, v_all), q_blocks)
    yx = o.transpose(1, 0, 2, 3).reshape(b, n, hq) @ w_o
    yc = gqa(qc, kc, vc) @ w_o if ctx_out else None
    return yx, yc


def short_conv_mixer(hx, hc, w_in, taps, w_out, ctx_out):
    pad = CONV_WIDTH // 2

    def mix(h):
        n = h.shape[1]
        bg, cg, v = jnp.split(h @ w_in, 3, axis=-1)
        u = jnp.pad(cg * v, ((0, 0), (pad, pad), (0, 0)))
        conv = sum(u[:, j:j + n] * taps[j] for j in range(CONV_WIDTH))
        return (bg * conv) @ w_out

    return mix(hx), (mix(hc) if ctx_out else None)


def moe_ffn(h, router_w, router_bias, w_gate_up, w_down, sw_gate_up, sw_down):
    t, d = h.shape
    scores = jax.nn.sigmoid((h @ router_w).astype(jnp.float32))
    biased = scores + router_bias.astype(jnp.float32)
    per_group = N_EXPERTS // N_GROUPS
    grp_score = lax.top_k(biased.reshape(t, N_GROUPS, per_group), 2)[0].sum(-1)
    _, top_grp = lax.top_k(grp_score, TOPK_GROUPS)
    grp_mask = jnp.any(top_grp[:, :, None] == jnp.arange(N_GROUPS)[None, None, :], axis=1)
    masked = jnp.where(jnp.repeat(grp_mask, per_group, axis=1), biased, -jnp.inf)
    _, eidx = lax.top_k(masked, TOP_K)
    gates = jnp.take_along_axis(scores, eidx, axis=1)
    gates = (gates / gates.sum(-1, keepdims=True) * ROUTED_SCALE).astype(h.dtype)

    tk = t * TOP_K
    eid = eidx.reshape(tk)
    tok = jnp.repeat(jnp.arange(t, dtype=jnp.int32), TOP_K)
    gw = gates.reshape(tk)
    order = jnp.argsort(eid)
    e_s, tok_s, gw_s = eid[order], tok[order], gw[order]
    counts = jnp.bincount(eid, length=N_EXPERTS)
    starts = jnp.cumsum(counts) - counts
    pcounts = (counts + MOE_BLOCK - 1) // MOE_BLOCK * MOE_BLOCK
    pends = jnp.cumsum(pcounts)
    pstarts = pends - pcounts
    dest = pstarts[e_s] + jnp.arange(tk, dtype=jnp.int32) - starts[e_s]
    n_blk = (tk + N_EXPERTS * (MOE_BLOCK - 1) + MOE_BLOCK - 1) // MOE_BLOCK
    p = n_blk * MOE_BLOCK
    buf_tok = jnp.full((p,), t, jnp.int32).at[dest].set(tok_s)
    buf_w = jnp.zeros((p,), h.dtype).at[dest].set(gw_s)
    blk_e = jnp.minimum(jnp.searchsorted(pends, jnp.arange(n_blk, dtype=jnp.int32) * MOE_BLOCK,
                                         side='right'), N_EXPERTS - 1)
    h_pad = jnp.concatenate([h, jnp.zeros((1, d), h.dtype)], axis=0)

    def expert_block(acc, xs):
        tb, wb, e = xs
        g, u = jnp.split(h_pad[tb] @ w_gate_up[e], 2, axis=-1)
        yb = (jax.nn.silu(g) * u) @ w_down[e]
        return acc.at[tb].add(yb * wb[:, None]), None

    routed, _ = lax.scan(expert_block, jnp.zeros_like(h_pad),
                         (buf_tok.reshape(n_blk, MOE_BLOCK), buf_w.reshape(n_blk, MOE_BLOCK), blk_e))
    sg, su = jnp.split(h @ sw_gate_up, 2, axis=-1)
    return (jax.nn.silu(sg) * su) @ sw_down + routed[:t]


def setup_inputs(seed: int = 0) -> dict:
    key = jax.random.key(seed)
    ks = jax.random.split(key, 21)

    def nrm(k, shape, scale):
        return jax.random.normal(k, shape, jnp.float32) * scale

    n_attn = (DEPTH + N_MIXERS - 1) // N_MIXERS
    n_conv = DEPTH // N_MIXERS
    qkv_dim = (N_HEADS + 2 * N_KV_HEADS) * HEAD_DIM
    hq = N_HEADS * HEAD_DIM
    d = D_MODEL
    return {
        "x": nrm(ks[0], (BATCH, SEQ, d), 1.0),
        "c": nrm(ks[1], (BATCH, d), 1.0),
        "ctx": nrm(ks[2], (BATCH, CTX_LEN, d), 1.0),
        "c_ctx": nrm(ks[3], (d,), 1.0),
        "ada_w": nrm(ks[4], (DEPTH, d, 6 * d), 0.5 * d ** -0.5),
        "ada_b": nrm(ks[5], (DEPTH, 6 * d), 0.02),
        "ln_g": 1.0 + nrm(ks[6], (DEPTH, 2, d), 0.02),
        "ln_b": nrm(ks[7], (DEPTH, 2, d), 0.02),
        "attn_w_qkv": nrm(ks[8], (n_attn, d, qkv_dim), d ** -0.5),
        "attn_q_norm": 1.0 + nrm(ks[9], (n_attn, HEAD_DIM), 0.02),
        "attn_k_norm": 1.0 + nrm(ks[10], (n_attn, HEAD_DIM), 0.02),
        "attn_w_o": nrm(ks[11], (n_attn, hq, d), DN_BETA * hq ** -0.5),
        "conv_w_in": nrm(ks[12], (n_conv, d, 3 * d), d ** -0.5),
        "conv_taps": nrm(ks[13], (n_conv, CONV_WIDTH, d), CONV_WIDTH ** -0.5),
        "conv_w_out": nrm(ks[14], (n_conv, d, d), DN_BETA * d ** -0.5),
        "router_w": nrm(ks[15], (DEPTH, d, N_EXPERTS), d ** -0.5),
        "router_bias": nrm(ks[16], (DEPTH, N_EXPERTS), 0.01),
        "exp_w_gate_up": nrm(ks[17], (DEPTH, N_EXPERTS, d, 2 * EXPERT_FF), d ** -0.5),
        "exp_w_down": nrm(ks[18], (DEPTH, N_EXPERTS, EXPERT_FF, d), DN_BETA * EXPERT_FF ** -0.5),
        "shared_w_gate_up": nrm(ks[19], (DEPTH, d, 2 * SHARED_FF), d ** -0.5),
        "shared_w_down": nrm(ks[20], (DEPTH, SHARED_FF, d), DN_BETA * SHARED_FF ** -0.5),
    }


def reference(x, c, ctx, c_ctx, ada_w, ada_b, ln_g, ln_b, attn_w_qkv, attn_q_norm, attn_k_norm,
              attn_w_o, conv_w_in, conv_taps, conv_w_out, router_w, router_bias, exp_w_gate_up,
              exp_w_down, shared_w_gate_up, shared_w_down):
    b, n, d = x.shape
    cl = ctx.shape[1]
    silu_c = jax.nn.silu(c)
    silu_cc = jax.nn.silu(c_ctx)
    for i in range(DEPTH):
        last = i == DEPTH - 1
        is_attn = i % N_MIXERS == 0
        j = i // N_MIXERS
        mx = (silu_c @ ada_w[i] + ada_b[i]).reshape(b, 6, 1, d)
        mc = (silu_cc @ ada_w[i] + ada_b[i]).reshape(6, 1, d)

        hx = x * (1.0 + mx[:, 1]) + mx[:, 0]
        hc = ctx * (1.0 + mc[1]) + mc[0] if (is_attn or not last) else None
        if is_attn:
            yx, yc = attention_mixer(hx, hc, attn_w_qkv[j], attn_q_norm[j], attn_k_norm[j],
                                     attn_w_o[j], not last)
        else:
            yx, yc = short_conv_mixer(hx, hc, conv_w_in[j], conv_taps[j], conv_w_out[j], not last)
        x = layer_norm(DN_ALPHA * x + mx[:, 2] * yx, ln_g[i, 0], ln_b[i, 0])
        if not last:
            ctx = layer_norm(DN_ALPHA * ctx + mc[2] * yc, ln_g[i, 0], ln_b[i, 0])

        hx = (x * (1.0 + mx[:, 4]) + mx[:, 3]).reshape(b * n, d)
        moe_args = (router_w[i], router_bias[i], exp_w_gate_up[i], exp_w_down[i],
                    shared_w_gate_up[i], shared_w_down[i])
        if not last:
            hc = (ctx * (1.0 + mc[4]) + mc[3]).reshape(b * cl, d)
            out = moe_ffn(jnp.concatenate([hc, hx], axis=0), *moe_args)
            ctx = layer_norm(DN_ALPHA * ctx + mc[5] * out[:b * cl].reshape(b, cl, d),
                             ln_g[i, 1], ln_b[i, 1])
            yx = out[b * cl:].reshape(b, n, d)
        else:
            yx = moe_ffn(hx, *moe_args).reshape(b, n, d)
        x = layer_norm(DN_ALPHA * x + mx[:, 5] * yx, ln_g[i, 1], ln_b[i, 1])
    return x
```

```python
import contextlib
import numpy as np
import concourse.bass as bass
import concourse.mybir as mybir
from concourse.bass_utils import run_bass_kernel_spmd

F32 = mybir.dt.float32
BF16 = mybir.dt.bfloat16
I32 = mybir.dt.int32
U32 = mybir.dt.uint32
AF = mybir.ActivationFunctionType
ALU = mybir.AluOpType
AX = mybir.AxisListType

P = 128
D = 1024
KC = 8
SEQ = 4096
CTX = 256
NT = SEQ // P
NKC = (SEQ + CTX) // P
NE = 256
NBLK = 512
NROW = NBLK * P
DN_ALPHA = 4.0 ** 0.25
LN_EPS = 1e-5
QK_EPS = 1e-6
SAME_SYNC = True


class Buf:
    __slots__ = ("lw", "rd")

    def __init__(self):
        self.lw = None
        self.rd = {}


class Sched:
    COMPUTE = ("pe", "act", "dve", "pool")

    def __init__(self, nc, st, nds=20):
        self.nc = nc
        self.names = ["pe", "act", "dve", "pool", "sp"]
        self.csem = {n: st.enter_context(nc.semaphore("c_" + n)) for n in self.names}
        self.cnt = {n: 0 for n in self.names}
        self.dsem = [st.enter_context(nc.semaphore("d%d" % i)) for i in range(nds)]
        self.dcnt = [0] * nds
        self.dnext = 0
        self.prog = {n: [] for n in self.names}
        self.known = {n: {} for n in self.names}
        self.bufs = {}
        self.ninstr = 0

    def B(self, *key):
        b = self.bufs.get(key)
        if b is None:
            b = self.bufs[key] = Buf()
        return b

    def _semobj(self, key):
        return self.csem[key] if isinstance(key, str) else self.dsem[key[1]]

    def _wait(self, eng, ev):
        key, val = ev
        if val <= 0 or self.known[eng].get(key, 0) >= val:
            return
        self.known[eng][key] = val
        sem = self._semobj(key)
        self.prog[eng].append(lambda e, sem=sem, val=val: e.wait_ge(sem, val))
        self.ninstr += 1

    def _sync(self, eng, reads, writes):
        evs = []
        for b in reads:
            if b.lw is not None:
                evs.append((b.lw, True))
        for b in writes:
            if b.lw is not None:
                evs.append((b.lw, True))
            for k, v in b.rd.items():
                evs.append(((k, v), False))
        for ev, strong in evs:
            if ev[0] == eng:
                if eng == "pe" or not (SAME_SYNC and strong):
                    continue
            self._wait(eng, ev)

    def _mark(self, ev, reads, writes):
        for b in reads:
            if b.rd.get(ev[0], 0) < ev[1]:
                b.rd[ev[0]] = ev[1]
        for b in writes:
            b.lw = ev
            b.rd = {}

    def op(self, eng, fn, reads=(), writes=()):
        self._sync(eng, reads, writes)
        self.cnt[eng] += 1
        ev = (eng, self.cnt[eng])
        sem = self.csem[eng]
        self.prog[eng].append(lambda e, fn=fn, sem=sem: fn(e).then_inc(sem, 1))
        self.ninstr += 1
        self._mark(ev, reads, writes)

    def dma(self, q, fn, reads=(), writes=()):
        self._sync(q, reads, writes)
        j = self.dnext
        self.dnext = (j + 1) % len(self.dsem)
        self._wait(q, (("d", j), self.dcnt[j]))
        self.dcnt[j] += 16
        ev = (("d", j), self.dcnt[j])
        sem = self.dsem[j]
        self.prog[q].append(lambda e, fn=fn, sem=sem: fn(e).then_inc(sem, 16))
        self.ninstr += 1
        self._mark(ev, reads, writes)

    def barrier(self):
        evs = [(n, self.cnt[n]) for n in self.COMPUTE]
        evs += [(("d", j), c) for j, c in enumerate(self.dcnt)]
        for n in self.names:
            for ev in evs:
                if ev[0] != n:
                    self._wait(n, ev)
        self.bufs = {}

    def reg(self, e, val):
        r = self.regcache.get(val)
        if r is None:
            r = self.regcache[val] = e.to_reg(val)
        return r

    def emit(self):
        self.regcache = {}
        with self.nc.Block() as block:
            for n, deco in (("pe", block.tensor), ("act", block.scalar), ("dve", block.vector),
                            ("pool", block.gpsimd), ("sp", block.sync)):
                prog = self.prog[n]

                def body(e, prog=prog):
                    for f in prog:
                        f(e)
                deco(body)
                self.prog[n] = []


class Ctx:
    pass


def build_program(debug=None, debug_outs=(), lite=False):
    nc = bass.Bass("TRN2", target_bir_lowering=False)
    g = Ctx()
    g.nc = nc
    g.debug = debug
    g.sfx = ""
    g.netab = 1 if lite else NE

    def din(name, shape, dt=F32):
        return nc.dram_tensor(name, list(shape), dt, kind="ExternalInput").ap()

    def dscr(name, shape, dt=F32):
        if debug_outs and name in debug_outs:
            return nc.dram_tensor(name, list(shape), dt, kind="ExternalOutput").ap()
        return nc.dram_tensor(name, list(shape), dt).ap()

    g.x = din("x", [SEQ, D])
    g.ctx = din("ctx", [CTX, D])
    g.cT = din("cT", [P, KC])
    g.ccT = din("ccT", [P, KC])
    g.ada_w = din("ada_w", [2, D, 6 * D])
    g.ada_b = din("ada_b", [2, 6 * D])
    g.ln_g = din("ln_g", [2, 2, D])
    g.ln_b = din("ln_b", [2, 2, D])
    g.w_qkv = din("w_qkv", [D, 1536])
    g.qn = din("qn", [1, P])
    g.kn = din("kn", [1, P])
    g.w_o = din("w_o", [D, D])
    g.w_in = din("w_in", [D, 3 * D])
    g.taps = din("taps", [3, D])
    g.w_out = din("w_out", [D, D])
    g.router_w = din("router_w", [2, D, NE])
    g.router_b = din("router_b", [2, NE])
    g.ew = [din("ew%d" % l, [(1 if lite else NE) * P, 6144]) for l in range(2)]
    g.c_iotab = din("c_iotab", [P, NBLK + 1])
    g.c_pidx = din("c_pidx", [P, 1])
    g.c_bigv = din("c_bigv", [P, 2])
    g.c_ucum = din("c_ucum", [P, 2 * NE])
    g.sgu = din("sgu", [2, D, 512])
    g.sdn = din("sdn", [2, 256, D])
    g.c_ident = din("c_ident", [P, P])
    g.c_cos = din("c_cos", [SEQ, 64])
    g.c_sin = din("c_sin", [SEQ, 64])
    g.c_iota = din("c_iota", [P, NE])
    g.c_cbase = din("c_cbase", [P, NE])
    g.c_triu = din("c_triu", [P, P])
    g.c_tokid = din("c_tokid", [P, NT * 2], I32)
    g.y = nc.dram_tensor("y", [SEQ, D], F32, kind="ExternalOutput").ap()

    g.MODD = dscr("MODD", [3, P, 6 * D])
    g.X1 = dscr("X1", [SEQ, D])
    g.X2 = dscr("X2", [SEQ, D])
    g.X3 = dscr("X3", [SEQ, D])
    g.HB = dscr("HB", [SEQ + 1, D], BF16)
    g.SH = dscr("SH", [SEQ, D])
    g.TBL = dscr("TBL", [NROW + 1, 2], I32)
    g.YE = dscr("YE", [NROW + 1, D], BF16)
    g.UU = dscr("UU", [SEQ + 2, D])
    g.BG = dscr("BG", [SEQ, D])
    g.WI = dscr("WI", [P, 3 * NBLK], I32)
    g.DBG = dscr("DBG", [P, 4096])

    with contextlib.ExitStack() as st:
        S = Sched(nc, st)
        g.S = S
        stages = [
            ("adaln", phase_adaln),
            ("attn", phase_attn),
            ("moe0", lambda g_, st_: phase_moe(g_, st_, 0, g.X1, g.X2)),
            ("conv", phase_conv),
            ("moe1", lambda g_, st_: phase_moe(g_, st_, 1, g.X3, g.y)),
        ]
        for name, fn in stages:
            fn(g, st)
            if debug and debug.startswith(name):
                break
        S.barrier()
        S.emit()
    return nc


def run_phase(g, fn):
    S = g.S
    with contextlib.ExitStack() as ph:
        S.barrier()
        fn(ph)
        S.barrier()
        S.emit()


def sbt(g, ph, name, shape, dt=F32):
    return ph.enter_context(g.nc.sbuf_tensor(name + g.sfx, list(shape), dt))


def pst(g, ph, name, shape, dt=F32):
    return ph.enter_context(g.nc.psum_tensor(name + g.sfx, list(shape), dt))


def phase_adaln(g, st):
    S = g.S

    def body(ph):
        sc = sbt(g, ph, "ad_sc", [P, 2, KC])
        scs = sbt(g, ph, "ad_scs", [P, 2, KC])
        scb = sbt(g, ph, "ad_scb", [P, 2, KC, P])
        wt = [sbt(g, ph, "ad_wt%d" % i, [P, KC, 512]) for i in range(2)]
        bt = [sbt(g, ph, "ad_bt%d" % i, [P, 512]) for i in range(2)]
        res = [sbt(g, ph, "ad_res%d" % i, [P, 512]) for i in range(3)]
        ps = [pst(g, ph, "ad_ps%d" % i, [P, 512]) for i in range(2)]
        S.dma("sp", lambda e: e.dma_start(out=sc[:, 0, :], in_=g.cT[:, :]), writes=[S.B("sc")])
        S.dma("sp", lambda e: e.dma_start(out=sc[:, 1, :], in_=g.ccT[:, :]), writes=[S.B("sc")])
        S.op("act", lambda e: e.activation(out=scs[:], in_=sc[:], func=AF.Silu),
             reads=[S.B("sc")], writes=[S.B("scs")])
        S.op("dve", lambda e: e.tensor_copy(out=scb[:], in_=scs[:].unsqueeze(3).to_broadcast([P, 2, KC, P])),
             reads=[S.B("scs")], writes=[S.B("scb")])
        it = 0
        rs = 0
        for layer in range(2):
            kinds = [(0, 0), (1, 1)] if layer == 0 else [(0, 2)]
            for n in range(12):
                s = it % 2
                it += 1
                wsrc = g.ada_w[layer, :, n * 512:(n + 1) * 512].rearrange("(k p) n -> p k n", p=P)
                S.dma("sp", lambda e, s=s, wsrc=wsrc: e.dma_start(out=wt[s][:], in_=wsrc),
                      writes=[S.B("wt", s)])
                bsrc = g.ada_b[layer:layer + 1, n * 512:(n + 1) * 512].to_broadcast([P, 512])
                S.dma("sp", lambda e, s=s, bsrc=bsrc: e.dma_start(out=bt[s][:], in_=bsrc),
                      writes=[S.B("bt", s)])
                for kind, idx in kinds:
                    pp = (it + kind) % 2
                    for k in range(KC):
                        S.op("pe", lambda e, pp=pp, kind=kind, k=k, s=s: e.matmul(
                            ps[pp][:], lhsT=scb[:, kind, k, :], rhs=wt[s][:, k, :],
                            start=(k == 0), stop=(k == KC - 1)),
                            reads=[S.B("scb"), S.B("wt", s)], writes=[S.B("ps", pp)])
                    r = rs % 3
                    rs += 1
                    add1 = 1.0 if n in (2, 3, 8, 9) else 0.0
                    S.op("dve", lambda e, r=r, pp=pp, s=s, add1=add1: e.scalar_tensor_tensor(
                        out=res[r][:], in0=ps[pp][:], scalar=add1, in1=bt[s][:], op0=ALU.add, op1=ALU.add),
                        reads=[S.B("ps", pp), S.B("bt", s)], writes=[S.B("res", r)])
                    dst = g.MODD[idx, :, n * 512:(n + 1) * 512]
                    S.dma("sp", lambda e, r=r, dst=dst: e.dma_start(out=dst, in_=res[r][:]),
                          reads=[S.B("res", r)], writes=[S.B("MODD", idx, n)])

    run_phase(g, body)


def load_mod(g, S, tile, idx, j, q="sp"):
    src = g.MODD[idx, :, j * D:(j + 1) * D]
    S.dma(q, lambda e: e.dma_start(out=tile[:], in_=src), writes=[S.B("modt", tile.name)])
    return S.B("modt", tile.name)


def load_bcast(g, S, tile, src_row, q="sp"):
    n = tile.shape[1]
    src = src_row.to_broadcast([P, n])
    S.dma(q, lambda e: e.dma_start(out=tile[:], in_=src), writes=[S.B("modt", tile.name)])
    return S.B("modt", tile.name)


def modulate_transpose(g, S, xt, xb, sc_t, sh_t, tmp, tmpb, hb, hbb, tp, tpb, hT, hTb, ident, identb, extra_reads=()):
    S.op("dve", lambda e: e.tensor_tensor(out=tmp[:], in0=xt, in1=sc_t[:], op=ALU.mult),
         reads=[xb, S.B("modt", sc_t.name)], writes=[tmpb])
    S.op("pool", lambda e: e.tensor_tensor(out=hb[:], in0=tmp[:], in1=sh_t[:], op=ALU.add),
         reads=[tmpb, S.B("modt", sh_t.name)], writes=[hbb])
    for k in range(KC):
        S.op("pe", lambda e, k=k: e.transpose(tp[:, k, :], hb[:, k * P:(k + 1) * P], ident[:]),
             reads=[hbb, identb], writes=[tpb])
    S.op("act", lambda e: e.activation(out=hT[:], in_=tp[:], func=AF.Copy), reads=[tpb], writes=[hTb])


def rms_rope(g, S, pfx, src_ps, src_b, nh, gain, gainb, cs, sn, csb, do_rope, sq, ss, rstd, qn, t1, t2, t3, t4, out_bf, out_b):
    B = lambda n: S.B("y1") if n == "sq" else S.B(pfx, n)
    S.op("act", lambda e: e.activation(out=sq[:, 0:nh * P], in_=src_ps, func=AF.Square),
         reads=[src_b], writes=[B("sq")])
    S.op("dve", lambda e: e.tensor_reduce(out=ss[:, 0:nh], in_=sq[:, 0:nh * P].rearrange("p (h d) -> p h d", d=P),
                                          axis=AX.X, op=ALU.add), reads=[B("sq")], writes=[B("ss")])
    S.op("dve", lambda e: e.tensor_scalar(out=ss[:, 0:nh], in0=ss[:, 0:nh], scalar1=1.0 / P, scalar2=QK_EPS,
                                          op0=ALU.mult, op1=ALU.add), reads=[B("ss")], writes=[B("ss")])
    S.op("act", lambda e: e.activation(out=ss[:, 0:nh], in_=ss[:, 0:nh], func=AF.Sqrt),
         reads=[B("ss")], writes=[B("ss")])
    S.op("dve", lambda e: e.reciprocal(out=rstd[:, 0:nh], in_=ss[:, 0:nh]), reads=[B("ss")], writes=[B("rstd")])
    S.op("dve", lambda e: e.tensor_tensor(
        out=qn[:, 0:nh, :], in0=src_ps.rearrange("p (h d) -> p h d", d=P),
        in1=rstd[:, 0:nh].unsqueeze(2).to_broadcast([P, nh, P]), op=ALU.mult),
        reads=[src_b, B("rstd")], writes=[B("qn")])
    if not do_rope:
        S.op("dve", lambda e: e.tensor_tensor(
            out=out_bf[:, 0:nh, :], in0=qn[:, 0:nh, :], in1=gain[:].unsqueeze(1).to_broadcast([P, nh, P]),
            op=ALU.mult), reads=[B("qn"), gainb], writes=[out_b])
        return
    S.op("pool", lambda e: e.tensor_tensor(
        out=qn[:, 0:nh, :], in0=qn[:, 0:nh, :], in1=gain[:].unsqueeze(1).to_broadcast([P, nh, P]),
        op=ALU.mult), reads=[B("qn"), gainb], writes=[B("qn")])
    q4 = qn[:, 0:nh, :].rearrange("p h (j two) -> p h j two", two=2)
    x0 = q4[:, :, :, 0]
    x1 = q4[:, :, :, 1]
    o4 = out_bf[:, 0:nh, :].rearrange("p h (j two) -> p h j two", two=2)
    cb = cs[:].unsqueeze(1).to_broadcast([P, nh, 64])
    sb_ = sn[:].unsqueeze(1).to_broadcast([P, nh, 64])
    S.op("dve", lambda e: e.tensor_tensor(out=t1[:, 0:nh, :], in0=x0, in1=cb, op=ALU.mult),
         reads=[B("qn"), csb], writes=[B("t1")])
    S.op("pool", lambda e: e.tensor_tensor(out=t2[:, 0:nh, :], in0=x1, in1=sb_, op=ALU.mult),
         reads=[B("qn"), csb], writes=[B("t2")])
    S.op("dve", lambda e: e.tensor_tensor(out=t3[:, 0:nh, :], in0=x0, in1=sb_, op=ALU.mult),
         reads=[B("qn"), csb], writes=[B("t3")])
    S.op("pool", lambda e: e.tensor_tensor(out=t4[:, 0:nh, :], in0=x1, in1=cb, op=ALU.mult),
         reads=[B("qn"), csb], writes=[B("t4")])
    S.op("dve", lambda e: e.tensor_tensor(out=o4[:, :, :, 0], in0=t1[:, 0:nh, :], in1=t2[:, 0:nh, :], op=ALU.subtract),
         reads=[B("t1"), B("t2")], writes=[out_b])
    S.op("dve", lambda e: e.tensor_tensor(out=o4[:, :, :, 1], in0=t3[:, 0:nh, :], in1=t4[:, 0:nh, :], op=ALU.add),
         reads=[B("t3"), B("t4")], writes=[out_b])


def layer_norm_store(g, S, pfx, t2, t2b, lng, lnb, stats, mv, rstd, xo, xob, dst, dstb):
    B = lambda n: S.B(pfx, n)
    for hh in range(2):
        S.op("dve", lambda e, hh=hh: e.bn_stats(out=stats[:, hh, :], in_=t2[:, hh * 512:(hh + 1) * 512]),
             reads=[t2b], writes=[B("stats")])
    S.op("dve", lambda e: e.bn_aggr(out=mv[:], in_=stats[:].rearrange("p a b -> p (a b)")),
         reads=[B("stats")], writes=[B("mv")])
    S.op("dve", lambda e: e.tensor_scalar(out=rstd[:], in0=mv[:, 1:2], scalar1=LN_EPS, scalar2=None, op0=ALU.add),
         reads=[B("mv")], writes=[B("rstd")])
    S.op("act", lambda e: e.activation(out=rstd[:], in_=rstd[:], func=AF.Sqrt), reads=[B("rstd")], writes=[B("rstd")])
    S.op("dve", lambda e: e.reciprocal(out=rstd[:], in_=rstd[:]), reads=[B("rstd")], writes=[B("rstd")])
    S.op("dve", lambda e: e.tensor_scalar(out=t2[:], in0=t2[:], scalar1=mv[:, 0:1], scalar2=rstd[:, 0:1],
                                          op0=ALU.subtract, op1=ALU.mult),
         reads=[t2b, B("mv"), B("rstd")], writes=[t2b])
    S.op("pool", lambda e: e.tensor_tensor(out=t2[:], in0=t2[:], in1=lng[:], op=ALU.mult),
         reads=[t2b, S.B("modt", lng.name)], writes=[t2b])
    S.op("dve", lambda e: e.tensor_tensor(out=xo[:], in0=t2[:], in1=lnb[:], op=ALU.add),
         reads=[t2b, S.B("modt", lnb.name)], writes=[xob])
    S.dma("sp", lambda e: e.dma_start(out=dst, in_=xo[:]), reads=[xob], writes=[dstb])


def phase_attn(g, st):
    S = g.S
    nc = g.nc

    def body(ph):
        wqkv = sbt(g, ph, "a_wqkv", [P, KC, 1536], BF16)
        wo = sbt(g, ph, "a_wo", [P, KC, D], BF16)
        ident = sbt(g, ph, "a_ident", [P, P], BF16)
        ones = sbt(g, ph, "a_ones", [P, P], BF16)
        KT = sbt(g, ph, "a_KT", [P, 2, NKC * P], BF16)
        V = sbt(g, ph, "a_V", [P, NKC, 256], BF16)
        shx = sbt(g, ph, "a_shx", [P, D])
        scx = sbt(g, ph, "a_scx", [P, D])
        gate = sbt(g, ph, "a_gate", [P, D])
        lng = sbt(g, ph, "a_lng", [P, D])
        lnb = sbt(g, ph, "a_lnb", [P, D])
        gq = sbt(g, ph, "a_gq", [P, P])
        gk = sbt(g, ph, "a_gk", [P, P])
        xt = sbt(g, ph, "a_xt", [P, 4, D])
        tmp = sbt(g, ph, "a_tmp", [P, D])
        hb = [sbt(g, ph, "a_hb%d" % i, [P, D], BF16) for i in range(2)]
        hT = [sbt(g, ph, "a_hT%d" % i, [P, KC, P], BF16) for i in range(2)]
        ss = sbt(g, ph, "a_ss", [P, 8])
        rstd = sbt(g, ph, "a_rstd", [P, 8])
        qn = sbt(g, ph, "a_qn", [P, 4, P])
        t1 = sbt(g, ph, "a_t1", [P, 4, 64])
        t2_ = sbt(g, ph, "a_t2", [P, 4, 64])
        t3 = sbt(g, ph, "a_t3", [P, 4, 64])
        t4 = sbt(g, ph, "a_t4", [P, 4, 64])
        qr = [sbt(g, ph, "a_qr%d" % i, [P, 8, P], BF16) for i in range(2)]
        cs = [sbt(g, ph, "a_cs%d" % i, [P, 64]) for i in range(2)]
        sn = [sbt(g, ph, "a_sn%d" % i, [P, 64]) for i in range(2)]
        QT = [sbt(g, ph, "a_QT%d" % i, [P, 8, 512], BF16) for i in range(2)]
        pT = [sbt(g, ph, "a_pT%d" % i, [P, 512], BF16) for i in range(3)]
        rz = sbt(g, ph, "a_rz", [P, 512])
        OT = sbt(g, ph, "a_OT", [P, 8, 512], BF16)
        y1 = sbt(g, ph, "a_y1", [P, D])
        stats = sbt(g, ph, "a_stats", [P, 2, 6])
        mv = sbt(g, ph, "a_mv", [P, 2])
        lrs = sbt(g, ph, "a_lrs", [P, 1])
        xo = [sbt(g, ph, "a_xo%d" % i, [P, D]) for i in range(1)]
        ps_s = [pst(g, ph, "a_ps_s%d" % i, [P, 512]) for i in range(2)]
        ps_o = pst(g, ph, "a_ps_o", [P, 512])
        ps_z = pst(g, ph, "a_ps_z", [P, 512])
        ps_tp = pst(g, ph, "a_ps_tp", [P, KC, P], BF16)
        ps_p = [pst(g, ph, "a_ps_p%d" % i, [P, 512]) for i in range(2)]
        ps_tq = pst(g, ph, "a_ps_tq", [P, 8, P], BF16)

        S.dma("pool", lambda e: e.dma_start(out=wqkv[:], in_=g.w_qkv.rearrange("(k p) n -> p k n", p=P)),
              writes=[S.B("wqkv")])
        S.dma("pool", lambda e: e.dma_start(out=wo[:], in_=g.w_o.rearrange("(k p) n -> p k n", p=P)),
              writes=[S.B("wo")])
        S.dma("pool", lambda e: e.dma_start(out=ident[:], in_=g.c_ident[:, :]), writes=[S.B("ident")])
        S.op("dve", lambda e: e.memset(ones[:], 1.0), writes=[S.B("ones")])
        load_mod(g, S, shx, 0, 0)
        load_mod(g, S, scx, 0, 1)
        shc, scc = gate, lng
        load_mod(g, S, shc, 1, 0)
        load_mod(g, S, scc, 1, 1)
        load_bcast(g, S, lnb, g.ln_b[0, 0:1, :])
        gqb = load_bcast(g, S, gq, g.qn[0:1, :])
        gkb = load_bcast(g, S, gk, g.kn[0:1, :])
        S.op("dve", lambda e: e.tensor_scalar(out=gq[:], in0=gq[:], scalar1=float(P) ** -0.5, scalar2=None,
                                              op0=ALU.mult), reads=[gqb], writes=[gqb])

        for i in range(NKC):
            s = i % 2
            is_ctx = i < 2
            src = g.ctx[i * P:(i + 1) * P, :] if is_ctx else g.x[(i - 2) * P:(i - 1) * P, :]
            xb = S.B("xq", s)
            S.dma("sp", lambda e, s=s, src=src: e.dma_start(out=xt[:, s, :], in_=src), writes=[xb])
            if not is_ctx:
                r0 = (i - 2) * P
                S.dma("sp", lambda e, s=s, r0=r0: e.dma_start(out=cs[s][:], in_=g.c_cos[r0:r0 + P, :]),
                      writes=[S.B("cs", s)])
                S.dma("sp", lambda e, s=s, r0=r0: e.dma_start(out=sn[s][:], in_=g.c_sin[r0:r0 + P, :]),
                      writes=[S.B("cs", s)])
            modulate_transpose(g, S, xt[:, s, :], xb, scc if is_ctx else scx, shc if is_ctx else shx,
                               tmp, S.B("tmp"), hb[s], S.B("hb", s), ps_tp, S.B("ps_tp"), hT[s], S.B("hT", s),
                               ident, S.B("ident"))
            pp = s
            for k in range(KC):
                S.op("pe", lambda e, k=k, s=s, pp=pp: e.matmul(ps_p[pp][:], lhsT=hT[s][:, k, :],
                                                               rhs=wqkv[:, k, 1024:1536],
                                                               start=(k == 0), stop=(k == KC - 1)),
                     reads=[S.B("hT", s), S.B("wqkv")], writes=[S.B("ps_p", pp)])
            rms_rope(g, S, "r", ps_p[pp][:, 0:256], S.B("ps_p", pp), 2, gk, gkb, cs[s], sn[s], S.B("cs", s),
                     not is_ctx, y1, ss, rstd, qn, t1, t2_, t3, t4, qr[s], S.B("qr", s, 0))
            for hh in range(2):
                S.op("pe", lambda e, hh=hh, s=s: e.transpose(ps_tq[:, hh, :], qr[s][:, hh, :], ident[:]),
                     reads=[S.B("qr", s, 0), S.B("ident")], writes=[S.B("ps_tq")])
            S.op("act", lambda e, i=i: e.activation(out=KT[:, :, i * P:(i + 1) * P], in_=ps_tq[:, 0:2, :],
                                                    func=AF.Copy),
                 reads=[S.B("ps_tq")], writes=[S.B("KT", i)])
            S.op("act", lambda e, i=i, pp=pp: e.activation(out=V[:, i, :], in_=ps_p[pp][:, 256:512], func=AF.Copy),
                 reads=[S.B("ps_p", pp)], writes=[S.B("V", i)])

        load_mod(g, S, gate, 0, 2)
        load_bcast(g, S, lng, g.ln_g[0, 0:1, :])
        kt_all = [S.B("KT", i) for i in range(NKC)]
        v_all = [S.B("V", i) for i in range(NKC)]
        def q_a1(ti):
            s = ti % 2
            xb = S.B("xq", s)
            S.dma("sp", lambda e, s=s, ti=ti: e.dma_start(out=xt[:, s, :], in_=g.x[ti * P:(ti + 1) * P, :]), writes=[xb])
            S.dma("sp", lambda e, s=s, ti=ti: e.dma_start(out=cs[s][:], in_=g.c_cos[ti * P:(ti + 1) * P, :]),
                  writes=[S.B("cs", s)])
            S.dma("sp", lambda e, s=s, ti=ti: e.dma_start(out=sn[s][:], in_=g.c_sin[ti * P:(ti + 1) * P, :]),
                  writes=[S.B("cs", s)])
            S.op("dve", lambda e, s=s: e.tensor_tensor(out=tmp[:], in0=xt[:, s, :], in1=scx[:], op=ALU.mult),
                 reads=[xb, S.B("modt", scx.name)], writes=[S.B("tmp")])
            S.op("pool", lambda e, s=s: e.tensor_tensor(out=hb[s][:], in0=tmp[:], in1=shx[:], op=ALU.add),
                 reads=[S.B("tmp"), S.B("modt", shx.name)], writes=[S.B("hb", s)])

        def q_a2(ti):
            s = ti % 2
            for k in range(KC):
                S.op("pe", lambda e, k=k, s=s: e.transpose(ps_tp[:, k, :], hb[s][:, k * P:(k + 1) * P], ident[:]),
                     reads=[S.B("hb", s), S.B("ident")], writes=[S.B("ps_tp")])
            S.op("act", lambda e, s=s: e.activation(out=hT[s][:], in_=ps_tp[:], func=AF.Copy),
                 reads=[S.B("ps_tp")], writes=[S.B("hT", s)])

        def q_b(ti):
            s = ti % 2
            for n in range(2):
                for k in range(KC):
                    S.op("pe", lambda e, k=k, s=s, n=n: e.matmul(ps_p[n][:], lhsT=hT[s][:, k, :],
                                                                 rhs=wqkv[:, k, n * 512:(n + 1) * 512],
                                                                 start=(k == 0), stop=(k == KC - 1)),
                         reads=[S.B("hT", s), S.B("wqkv")], writes=[S.B("ps_p", n)])
            for n in range(2):
                rms_rope(g, S, "r", ps_p[n][:, :], S.B("ps_p", n), 4, gq, gqb, cs[s], sn[s],
                         S.B("cs", s), True, y1, ss, rstd, qn, t1, t2_, t3, t4,
                         qr[s][:, n * 4:(n + 1) * 4, :], S.B("qr", s, n))

        def q_c(ti):
            s = ti % 2
            xs_ = (ti // 4) % 2
            j = ti % 4
            for hh in range(8):
                S.op("pe", lambda e, hh=hh, s=s: e.transpose(ps_tq[:, hh, :], qr[s][:, hh, :], ident[:]),
                     reads=[S.B("qr", s, hh // 4), S.B("ident")], writes=[S.B("ps_tq")])
            S.op("act", lambda e, xs_=xs_, j=j: e.activation(out=QT[xs_][:, :, j * P:(j + 1) * P], in_=ps_tq[:],
                                                             func=AF.Copy),
                 reads=[S.B("ps_tq")], writes=[S.B("QT", xs_)])

        for ti in range(4):
            q_a1(ti)
            q_a2(ti)
            q_b(ti)
            q_c(ti)
        NQB = SEQ // 512
        for qb in range(NQB):
            xs = qb % 2
            for h in range(8):
                kv = h // 4
                seq = []
                for kc in range(NKC):
                    seq.append(kc)
                def issue_s(kc, h=h, kv=kv, xs=xs):
                    b = kc % 2
                    S.op("pe", lambda e, kc=kc, b=b: e.matmul(ps_s[b][:], lhsT=KT[:, kv, kc * P:(kc + 1) * P],
                                                             rhs=QT[xs][:, h, :], start=True, stop=True),
                         reads=[kt_all[kc], S.B("QT", xs)], writes=[S.B("ps_s", b)])
                    pb = kc % 3
                    S.op("act", lambda e, b=b, pb=pb: e.activation(out=pT[pb][:], in_=ps_s[b][:], func=AF.Exp),
                         reads=[S.B("ps_s", b)], writes=[S.B("pT", pb)])

                def issue_pv(kc, h=h, kv=kv):
                    pb = kc % 3
                    S.op("pe", lambda e, kc=kc, pb=pb: e.matmul(ps_o[:], lhsT=V[:, kc, kv * P:(kv + 1) * P],
                                                               rhs=pT[pb][:], start=(kc == 0),
                                                               stop=(kc == NKC - 1)),
                         reads=[v_all[kc], S.B("pT", pb)], writes=[S.B("ps_o")])
                    S.op("pe", lambda e, kc=kc, pb=pb: e.matmul(ps_z[:], lhsT=ones[:], rhs=pT[pb][:],
                                                               start=(kc == 0), stop=(kc == NKC - 1)),
                         reads=[S.B("ones"), S.B("pT", pb)], writes=[S.B("ps_z")])
                issue_s(0)
                nt = (qb + 1) * 4 + h // 2
                for kc in range(NKC):
                    if kc + 1 < NKC:
                        issue_s(kc + 1)
                    issue_pv(kc)
                    if qb + 1 < NQB:
                        if h % 2 == 0 and kc == 2:
                            q_a1(nt)
                        elif h % 2 == 0 and kc == 12:
                            q_a2(nt)
                        elif h % 2 == 0 and kc == 20:
                            q_b(nt)
                        elif h % 2 == 1 and kc == 10:
                            q_c(nt)
                S.op("dve", lambda e: e.reciprocal(out=rz[:], in_=ps_z[:]), reads=[S.B("ps_z")], writes=[S.B("rz")])
                S.op("dve", lambda e, h=h: e.tensor_tensor(out=OT[:, h, :], in0=ps_o[:], in1=rz[:], op=ALU.mult),
                     reads=[S.B("ps_o"), S.B("rz")], writes=[S.B("OT", h)])
            for j in range(4):
                ti = qb * 4 + j
                o = 0
                for n in range(2):
                    for h in range(8):
                        S.op("pe", lambda e, h=h, n=n, j=j: e.matmul(ps_p[n][:], lhsT=OT[:, h, j * P:(j + 1) * P],
                                                                     rhs=wo[:, h, n * 512:(n + 1) * 512],
                                                                     start=(h == 0), stop=(h == 7)),
                             reads=[S.B("OT", h), S.B("wo")], writes=[S.B("ps_p", n)])
                    S.op("dve", lambda e, n=n: e.tensor_tensor(out=y1[:, n * 512:(n + 1) * 512], in0=ps_p[n][:],
                                                               in1=gate[:, n * 512:(n + 1) * 512], op=ALU.mult),
                         reads=[S.B("ps_p", n), S.B("modt", gate.name)], writes=[S.B("y1")])
                rs_ = 2 + ti % 2
                S.dma("sp", lambda e, rs_=rs_, ti=ti: e.dma_start(out=xt[:, rs_, :], in_=g.x[ti * P:(ti + 1) * P, :]),
                      writes=[S.B("xq", rs_)])
                S.op("dve", lambda e, rs_=rs_: e.scalar_tensor_tensor(
                    out=y1[:], in0=xt[:, rs_, :], scalar=DN_ALPHA, in1=y1[:], op0=ALU.mult, op1=ALU.add),
                    reads=[S.B("xq", rs_), S.B("y1")], writes=[S.B("y1")])
                layer_norm_store(g, S, "lna", y1, S.B("y1"), lng, lnb, stats, mv, lrs, xo[o], S.B("xo", o),
                                 g.X1[ti * P:(ti + 1) * P, :], S.B("X1", ti))

    run_phase(g, body)


def phase_moe(g, st, layer, XIN, XOUT):
    S = g.S
    nc = g.nc
    midx = 0 if layer == 0 else 2
    g.sfx = "_L%d" % layer
    YEv = g.YE[0:NROW, :].rearrange("(p j) d -> p j d", j=512)

    with contextlib.ExitStack() as outer:
        dest_all = sbt(g, outer, "m_dest", [P, NT, 8], I32)
        gate_all = sbt(g, outer, "m_gate", [P, NT, 8])

        def body_r(ph):
            wr = sbt(g, ph, "r_wr", [P, KC, NE], BF16)
            wsgu = sbt(g, ph, "r_wsgu", [P, KC, 512], BF16)
            wsdn = sbt(g, ph, "r_wsdn", [P, 2, D], BF16)
            rb = sbt(g, ph, "r_rb", [P, NE])
            iotaE = sbt(g, ph, "r_iota", [P, NE])
            iotaB = sbt(g, ph, "r_iotaB", [P, NBLK])
            pidx = sbt(g, ph, "r_pidx", [P, 1])
            identf = sbt(g, ph, "r_identf", [P, P])
            triu = sbt(g, ph, "r_triu", [P, P], BF16)
            ones = sbt(g, ph, "r_ones", [P, P], BF16)
            ident = sbt(g, ph, "r_ident", [P, P], BF16)
            tokid = sbt(g, ph, "r_tokid", [P, NT, 2], I32)
            sh3 = sbt(g, ph, "r_sh3", [P, D])
            sc4 = sbt(g, ph, "r_sc4", [P, D])
            cum = sbt(g, ph, "r_cum", [P, NE], BF16)
            tinit = sbt(g, ph, "r_tinit", [P, 1024], I32)
            zrow = sbt(g, ph, "r_zrow", [1, D], BF16)
            xt = [sbt(g, ph, "r_xt%d" % i, [P, D]) for i in range(2)]
            tmp = sbt(g, ph, "r_tmp", [P, D])
            hb = [sbt(g, ph, "r_hb%d" % i, [P, D], BF16) for i in range(2)]
            hT = [sbt(g, ph, "r_hT%d" % i, [P, KC, P], BF16) for i in range(2)]
            sgt = sbt(g, ph, "r_sgt", [P, 2, P])
            actT = sbt(g, ph, "r_actT", [P, 2, P], BF16)
            sho = [sbt(g, ph, "r_sho%d" % i, [P, D]) for i in range(2)]
            sc = sbt(g, ph, "r_sc", [P, NE])
            bi = sbt(g, ph, "r_bi", [P, NE])
            m8 = sbt(g, ph, "r_m8", [P, 8, 8])
            gs = sbt(g, ph, "r_gs", [P, 8])
            gm8 = sbt(g, ph, "r_gm8", [P, 8])
            gmask = sbt(g, ph, "r_gmask", [P, 8])
            mk = sbt(g, ph, "r_mk", [P, NE])
            v8 = sbt(g, ph, "r_v8", [P, 8])
            selb = sbt(g, ph, "r_selb", [P, NE], BF16)
            Gu = sbt(g, ph, "r_Gu", [P, NE])
            G = sbt(g, ph, "r_G", [P, NE])
            den = sbt(g, ph, "r_den", [P, 1])
            e8 = sbt(g, ph, "r_e8", [P, 8], U32)
            eall = sbt(g, ph, "r_eall", [P, NT, 8])
            posk = sbt(g, ph, "r_posk", [P, NT, 8])
            psk = sbt(g, ph, "r_psk", [P, NT, 8])
            junk = sbt(g, ph, "r_junk", [P, NE])
            cnt = sbt(g, ph, "r_cnt", [P, NE])
            cnti = sbt(g, ph, "r_cnti", [P, NE], I32)
            pc = sbt(g, ph, "r_pc", [P, NE])
            onesf = sbt(g, ph, "r_onesf", [P, NE])
            pend = sbt(g, ph, "r_pend", [P, NE])
            pstart = sbt(g, ph, "r_pstart", [P, NE])
            pendT = sbt(g, ph, "r_pendT", [P, 2])
            cmpb = sbt(g, ph, "r_cmpb", [P, 2, NBLK], BF16)
            blkf = sbt(g, ph, "r_blkf", [P, NBLK])
            wif = sbt(g, ph, "r_wif", [P, NBLK])
            wi_all = sbt(g, ph, "r_wi", [P, 3, NBLK], I32)
            pcT = sbt(g, ph, "r_pcT", [P, 2])
            pcb = sbt(g, ph, "r_pcb", [P, 2, P], BF16)
            ucum = sbt(g, ph, "r_ucum", [P, 2, NE], BF16)
            bigv = sbt(g, ph, "r_bigv", [P, 2])
            phf = sbt(g, ph, "r_phf", [P, NT * 8])
            lof = sbt(g, ph, "r_lof", [P, NT * 8])
            hif = sbt(g, ph, "r_hif", [P, NT * 8])
            ps_tp = pst(g, ph, "r_ps_tp", [P, KC, P], BF16)
            ps_r = pst(g, ph, "r_ps_r", [P, 512])
            ps_g = pst(g, ph, "r_ps_g", [P, 4, P])
            ps_d = [pst(g, ph, "r_ps_d%d" % i, [P, 512]) for i in range(2)]
            ps_pos = pst(g, ph, "r_ps_pos", [P, 512])

            S.dma("pool", lambda e: e.dma_start(out=wr[:], in_=g.router_w[layer].rearrange("(k p) n -> p k n", p=P)),
                  writes=[S.B("wr")])
            S.dma("pool", lambda e: e.dma_start(out=wsgu[:], in_=g.sgu[layer].rearrange("(k p) n -> p k n", p=P)),
                  writes=[S.B("wsgu")])
            S.dma("pool", lambda e: e.dma_start(out=wsdn[:], in_=g.sdn[layer].rearrange("(k p) n -> p k n", p=P)),
                  writes=[S.B("wsdn")])
            S.dma("pool", lambda e: e.dma_start(out=triu[:], in_=g.c_triu[:, :]), writes=[S.B("triu")])
            S.dma("pool", lambda e: e.dma_start(out=ident[:], in_=g.c_ident[:, :]), writes=[S.B("ident")])
            S.dma("sp", lambda e: e.dma_start(out=identf[:], in_=g.c_ident[:, :]), writes=[S.B("identf")])
            S.dma("sp", lambda e: e.dma_start(out=iotaE[:], in_=g.c_iota[:, :]), writes=[S.B("iotaE")])
            S.dma("sp", lambda e: e.dma_start(out=iotaB[:], in_=g.c_iotab[:, 0:NBLK]), writes=[S.B("iotaB")])
            S.dma("sp", lambda e: e.dma_start(out=pidx[:], in_=g.c_pidx[:, :]), writes=[S.B("pidx")])
            S.dma("sp", lambda e: e.dma_start(out=bigv[:], in_=g.c_bigv[:, :]), writes=[S.B("bigv")])
            S.dma("pool", lambda e: e.dma_start(out=ucum[:], in_=g.c_ucum.rearrange("p (c n) -> p c n", c=2)),
                  writes=[S.B("ucum")])
            S.dma("sp", lambda e: e.dma_start(out=tokid[:], in_=g.c_tokid.rearrange("p (t two) -> p t two", two=2)),
                  writes=[S.B("tokid")])
            load_bcast(g, S, rb, g.router_b[layer:layer + 1, :])
            load_mod(g, S, sh3, midx, 3)
            load_mod(g, S, sc4, midx, 4)
            S.op("dve", lambda e: e.memset(ones[:], 1.0), writes=[S.B("ones")])
            S.op("dve", lambda e: e.memset(onesf[:], 1.0), writes=[S.B("onesf")])
            S.op("dve", lambda e: e.memset(cum[:], 0.0), writes=[S.B("cum")])
            S.op("dve", lambda e: e.memset(zrow[:], 0.0), writes=[S.B("zrow")])
            S.op("pool", lambda e: e.iota(tinit[:], [[0, 1024]], base=(1 << 20), channel_multiplier=0), writes=[S.B("tinit")])
            S.dma("sp", lambda e: e.dma_start(out=g.TBL[0:NROW, :].rearrange("(p r) two -> p (r two)", p=P),
                                              in_=tinit[:]), reads=[S.B("tinit")], writes=[S.B("TBLinit")])
            S.dma("sp", lambda e: e.dma_start(out=g.HB[SEQ:SEQ + 1, :], in_=zrow[:]), reads=[S.B("zrow")])

            for i in range(NT):
                s = i % 2
                xb = S.B("xt", s)
                S.dma("sp", lambda e, s=s, i=i: e.dma_start(out=xt[s][:], in_=XIN[i * P:(i + 1) * P, :]), writes=[xb])
                modulate_transpose(g, S, xt[s][:], xb, sc4, sh3, tmp, S.B("tmp"), hb[s], S.B("hb", s),
                                   ps_tp, S.B("ps_tp"), hT[s], S.B("hT", s), ident, S.B("ident"))
                S.dma("sp", lambda e, s=s, i=i: e.dma_start(out=g.HB[i * P:(i + 1) * P, :], in_=hb[s][:]),
                      reads=[S.B("hb", s)])
                for k in range(KC):
                    S.op("pe", lambda e, k=k, s=s: e.matmul(ps_r[:, 0:NE], lhsT=hT[s][:, k, :], rhs=wr[:, k, :],
                                                            start=(k == 0), stop=(k == KC - 1)),
                         reads=[S.B("hT", s), S.B("wr")], writes=[S.B("ps_r")])
                for m in range(4):
                    for k in range(KC):
                        S.op("pe", lambda e, k=k, s=s, m=m: e.matmul(ps_g[:, m, :], lhsT=wsgu[:, k, m * P:(m + 1) * P],
                                                                     rhs=hT[s][:, k, :], start=(k == 0),
                                                                     stop=(k == KC - 1)),
                             reads=[S.B("hT", s), S.B("wsgu")], writes=[S.B("ps_g")])
                S.op("act", lambda e: e.activation(out=sgt[:], in_=ps_g[:, 0:2, :], func=AF.Silu),
                     reads=[S.B("ps_g")], writes=[S.B("sgt")])
                S.op("dve", lambda e: e.tensor_tensor(out=actT[:], in0=sgt[:], in1=ps_g[:, 2:4, :], op=ALU.mult),
                     reads=[S.B("sgt"), S.B("ps_g")], writes=[S.B("actT")])
                for n in range(2):
                    for j in range(2):
                        S.op("pe", lambda e, n=n, j=j: e.matmul(ps_d[n][:], lhsT=actT[:, j, :],
                                                                rhs=wsdn[:, j, n * 512:(n + 1) * 512],
                                                                start=(j == 0), stop=(j == 1)),
                             reads=[S.B("actT"), S.B("wsdn")], writes=[S.B("ps_d", n)])
                    S.op("act", lambda e, n=n, s=s: e.activation(out=sho[s][:, n * 512:(n + 1) * 512], in_=ps_d[n][:],
                                                                 func=AF.Copy),
                         reads=[S.B("ps_d", n)], writes=[S.B("sho", s)])
                S.dma("sp", lambda e, s=s, i=i: e.dma_start(out=g.SH[i * P:(i + 1) * P, :], in_=sho[s][:]),
                      reads=[S.B("sho", s)])
                S.op("act", lambda e: e.activation(out=sc[:], in_=ps_r[:, 0:NE], func=AF.Sigmoid),
                     reads=[S.B("ps_r")], writes=[S.B("sc")])
                S.op("dve", lambda e: e.tensor_tensor(out=bi[:], in0=sc[:], in1=rb[:], op=ALU.add),
                     reads=[S.B("sc"), S.B("modt", rb.name)], writes=[S.B("bi")])
                for gi in range(8):
                    S.op("dve", lambda e, gi=gi: e.max(out=m8[:, gi, :], in_=bi[:, gi * 32:(gi + 1) * 32]),
                         reads=[S.B("bi")], writes=[S.B("m8")])
                S.op("dve", lambda e: e.tensor_tensor(out=gs[:], in0=m8[:, :, 0], in1=m8[:, :, 1], op=ALU.add),
                     reads=[S.B("m8")], writes=[S.B("gs")])
                S.op("dve", lambda e: e.max(out=gm8[:], in_=gs[:]), reads=[S.B("gs")], writes=[S.B("gm8")])
                S.op("dve", lambda e: e.tensor_scalar(out=gmask[:], in0=gs[:], scalar1=gm8[:, 3:4], scalar2=None,
                                                      op0=ALU.is_ge), reads=[S.B("gs"), S.B("gm8")], writes=[S.B("gmask")])
                S.op("dve", lambda e: e.scalar_tensor_tensor(
                    out=mk[:].rearrange("p (a b) -> p a b", b=32), in0=bi[:].rearrange("p (a b) -> p a b", b=32),
                    scalar=2.0, in1=gmask[:].unsqueeze(2).to_broadcast([P, 8, 32]), op0=ALU.add, op1=ALU.mult),
                    reads=[S.B("bi"), S.B("gmask")], writes=[S.B("mk")])
                S.op("dve", lambda e: e.max(out=v8[:], in_=mk[:]), reads=[S.B("mk")], writes=[S.B("v8")])
                S.op("dve", lambda e: e.max_index(out=e8[:], in_max=v8[:], in_values=mk[:]),
                     reads=[S.B("mk"), S.B("v8")], writes=[S.B("e8")])
                S.op("dve", lambda e, i=i: e.tensor_copy(out=eall[:, i, :], in_=e8[:]), reads=[S.B("e8")],
                     writes=[S.B("eall", i)])
                S.op("dve", lambda e: e.tensor_scalar(out=selb[:], in0=mk[:], scalar1=v8[:, 7:8], scalar2=None,
                                                      op0=ALU.is_ge), reads=[S.B("mk"), S.B("v8")], writes=[S.B("selb")])
                S.op("dve", lambda e: e.scalar_tensor_tensor(out=Gu[:], in0=sc[:], scalar=1.0, in1=selb[:],
                                                             op0=ALU.mult, op1=ALU.mult, accum_out=den[:]),
                     reads=[S.B("sc"), S.B("selb")], writes=[S.B("Gu"), S.B("den")])
                S.op("dve", lambda e: e.reciprocal(out=den[:], in_=den[:]), reads=[S.B("den")], writes=[S.B("den")])
                S.op("dve", lambda e: e.tensor_scalar(out=G[:], in0=Gu[:], scalar1=den[:, 0:1], scalar2=2.5,
                                                      op0=ALU.mult, op1=ALU.mult),
                     reads=[S.B("Gu"), S.B("den")], writes=[S.B("G")])
                S.op("pe", lambda e: e.matmul(ps_pos[:, 0:NE], lhsT=triu[:], rhs=selb[:], start=True, stop=False),
                     reads=[S.B("triu"), S.B("selb")], writes=[S.B("ps_pos")])
                S.op("pe", lambda e: e.matmul(ps_pos[:, 0:NE], lhsT=ones[:], rhs=cum[:], start=False, stop=True),
                     reads=[S.B("ones"), S.B("cum")], writes=[S.B("ps_pos")])
                S.op("dve", lambda e: e.tensor_tensor(out=cum[:], in0=cum[:], in1=selb[:], op=ALU.add),
                     reads=[S.B("cum"), S.B("selb")], writes=[S.B("cum")])
                for k in range(8):
                    S.op("dve", lambda e, i=i, k=k: e.scalar_tensor_tensor(
                        out=junk[:], in0=iotaE[:], scalar=eall[:, i, k:k + 1], in1=G[:], op0=ALU.is_equal, op1=ALU.mult,
                        accum_out=gate_all[:, i, k:k + 1]),
                        reads=[S.B("iotaE"), S.B("eall", i), S.B("G")], writes=[S.B("junk"), S.B("gate", i)])
                    S.op("dve", lambda e, i=i, k=k: e.scalar_tensor_tensor(
                        out=junk[:], in0=iotaE[:], scalar=eall[:, i, k:k + 1], in1=ps_pos[:, 0:NE], op0=ALU.is_equal,
                        op1=ALU.mult, accum_out=posk[:, i, k:k + 1]),
                        reads=[S.B("iotaE"), S.B("eall", i), S.B("ps_pos")], writes=[S.B("junk"), S.B("posk", i)])

            S.op("pe", lambda e: e.matmul(ps_pos[:, 0:NE], lhsT=ones[:], rhs=cum[:], start=True, stop=True),
                 reads=[S.B("ones"), S.B("cum")], writes=[S.B("ps_pos")])
            S.op("dve", lambda e: e.tensor_copy(out=cnt[:], in_=ps_pos[:, 0:NE]), reads=[S.B("ps_pos")], writes=[S.B("cnt")])
            S.op("dve", lambda e: e.memset(pc[:], 0.0), writes=[S.B("pc")])
            for j in range(32):
                S.op("dve", lambda e, j=j: e.scalar_tensor_tensor(out=pc[:], in0=cnt[:], scalar=float(P * j), in1=pc[:],
                                                                  op0=ALU.is_gt, op1=ALU.add),
                     reads=[S.B("cnt"), S.B("pc")], writes=[S.B("pc")])
            S.op("dve", lambda e: e.tensor_scalar(out=pc[:], in0=pc[:], scalar1=float(P), scalar2=None, op0=ALU.mult),
                 reads=[S.B("pc")], writes=[S.B("pc")])
            for c in range(2):
                S.op("dve", lambda e, c=c: e.scalar_tensor_tensor(
                    out=junk[:, 0:P], in0=pc[:, c * P:(c + 1) * P], scalar=1.0, in1=identf[:], op0=ALU.mult,
                    op1=ALU.mult, accum_out=pcT[:, c:c + 1]),
                    reads=[S.B("pc"), S.B("identf")], writes=[S.B("junk"), S.B("pcT")])
            for c in range(2):
                S.op("dve", lambda e, c=c: e.tensor_copy(out=pcb[:, c, :], in_=pcT[:, c:c + 1].to_broadcast([P, P])),
                     reads=[S.B("pcT")], writes=[S.B("pcb")])
            for c in range(2):
                S.op("pe", lambda e, c=c: e.matmul(ps_pos[:, 0:NE], lhsT=pcb[:, c, :], rhs=ucum[:, c, :],
                                                   start=(c == 0), stop=(c == 1)),
                     reads=[S.B("pcb"), S.B("ucum")], writes=[S.B("ps_pos")])
            S.op("dve", lambda e: e.tensor_copy(out=pend[:], in_=ps_pos[:, 0:NE]), reads=[S.B("ps_pos")],
                 writes=[S.B("pend")])
            S.op("dve", lambda e: e.tensor_tensor(out=pstart[:], in0=pend[:], in1=pc[:], op=ALU.subtract),
                 reads=[S.B("pend"), S.B("pc")], writes=[S.B("pstart")])
            for c in range(2):
                S.op("dve", lambda e, c=c: e.scalar_tensor_tensor(
                    out=junk[:, 0:P], in0=pend[:, c * P:(c + 1) * P], scalar=1.0, in1=identf[:], op0=ALU.mult,
                    op1=ALU.mult, accum_out=pendT[:, c:c + 1]),
                    reads=[S.B("pend"), S.B("identf")], writes=[S.B("junk"), S.B("pendT")])
            S.op("dve", lambda e: e.tensor_tensor(out=pendT[:], in0=pendT[:], in1=bigv[:], op=ALU.add),
                 reads=[S.B("pendT"), S.B("bigv")], writes=[S.B("pendT")])
            for c in range(2):
                S.op("dve", lambda e, c=c: e.tensor_scalar(out=cmpb[:, c, :], in0=iotaB[:], scalar1=pendT[:, c:c + 1],
                                                           scalar2=None, op0=ALU.is_ge),
                     reads=[S.B("iotaB"), S.B("pendT")], writes=[S.B("cmpb")])
            for c in range(2):
                S.op("pe", lambda e, c=c: e.matmul(ps_r[:, 0:NBLK], lhsT=ones[:], rhs=cmpb[:, c, :], start=(c == 0),
                                                   stop=(c == 1)),
                     reads=[S.B("ones"), S.B("cmpb")], writes=[S.B("ps_r")])
            S.op("dve", lambda e: e.tensor_copy(out=blkf[:], in_=ps_r[:, 0:NBLK]), reads=[S.B("ps_r")],
                 writes=[S.B("blkf")])
            S.op("dve", lambda e: e.tensor_scalar(out=wif[:], in0=blkf[:], scalar1=float(P) if g.netab == NE else 0.0, scalar2=pidx[:, 0:1],
                                                  op0=ALU.mult, op1=ALU.add),
                 reads=[S.B("blkf"), S.B("pidx")], writes=[S.B("wif")])
            S.op("dve", lambda e: e.tensor_scalar(out=blkf[:], in0=iotaB[:], scalar1=pend[:, NE - 1:NE], scalar2=1.0e6,
                                                  op0=ALU.is_ge, op1=ALU.mult),
                 reads=[S.B("iotaB"), S.B("pend"), S.B("wif")], writes=[S.B("blkf")])
            S.op("dve", lambda e: e.tensor_tensor(out=wif[:], in0=wif[:], in1=blkf[:], op=ALU.add),
                 reads=[S.B("wif"), S.B("blkf")], writes=[S.B("wif")])
            S.op("dve", lambda e: e.tensor_copy(out=wi_all[:, 2, :], in_=wif[:]), reads=[S.B("wif")], writes=[S.B("wi")])
            S.op("dve", lambda e: e.tensor_scalar(out=wi_all[:, 0, :], in0=wif[:], scalar1=2.0, scalar2=None,
                                                  op0=ALU.mult), reads=[S.B("wif")], writes=[S.B("wi")])
            S.op("dve", lambda e: e.tensor_scalar(out=wi_all[:, 1, :], in0=wif[:], scalar1=2.0, scalar2=1.0,
                                                  op0=ALU.mult, op1=ALU.add), reads=[S.B("wif")], writes=[S.B("wi")])
            S.dma("sp", lambda e: e.dma_start(out=g.WI[:, :], in_=wi_all[:].rearrange("p a b -> p (a b)")),
                  reads=[S.B("wi")])
            for i in range(NT):
                for k in range(8):
                    S.op("dve", lambda e, i=i, k=k: e.scalar_tensor_tensor(
                        out=junk[:], in0=iotaE[:], scalar=eall[:, i, k:k + 1], in1=pstart[:], op0=ALU.is_equal,
                        op1=ALU.mult, accum_out=psk[:, i, k:k + 1]),
                        reads=[S.B("iotaE"), S.B("eall", i), S.B("pstart")], writes=[S.B("junk"), S.B("psk")])
            pskb = [S.B("psk")] + [S.B("posk", i) for i in range(NT)]
            posf = posk[:].rearrange("p a b -> p (a b)")
            pskf = psk[:].rearrange("p a b -> p (a b)")
            S.op("dve", lambda e: e.memset(phf[:], 0.0), writes=[S.B("phf")])
            for j in range(1, 32):
                S.op("dve", lambda e, j=j: e.scalar_tensor_tensor(out=phf[:], in0=posf, scalar=float(P * j), in1=phf[:],
                                                                  op0=ALU.is_ge, op1=ALU.add),
                     reads=pskb + [S.B("phf")], writes=[S.B("phf")])
            S.op("dve", lambda e: e.scalar_tensor_tensor(out=lof[:], in0=phf[:], scalar=-float(P), in1=posf,
                                                         op0=ALU.mult, op1=ALU.add),
                 reads=pskb + [S.B("phf")], writes=[S.B("lof")])
            S.op("dve", lambda e: e.scalar_tensor_tensor(out=hif[:], in0=pskf, scalar=1.0 / P, in1=phf[:],
                                                         op0=ALU.mult, op1=ALU.add),
                 reads=pskb + [S.B("phf")], writes=[S.B("hif")])
            S.op("dve", lambda e: e.scalar_tensor_tensor(out=dest_all[:].rearrange("p a b -> p (a b)"), in0=lof[:],
                                                         scalar=512.0, in1=hif[:], op0=ALU.mult, op1=ALU.add),
                 reads=[S.B("lof"), S.B("hif")], writes=[S.B("dest")])
            if g.debug and g.debug.endswith("_r"):
                dbg = sbt(g, ph, "r_dbg", [P, 4096])
                S.op("dve", lambda e: e.memset(dbg[:], 0.0), writes=[S.B("dbg")])
                items = [(cnt[:], 256), (pc[:], 256), (pend[:], 256), (pstart[:], 256), (blkf[:], 512),
                         (psk[:].rearrange("p a b -> p (a b)"), 256), (posk[:].rearrange("p a b -> p (a b)"), 256),
                         (eall[:].rearrange("p a b -> p (a b)"), 256), (dest_all[:].rearrange("p a b -> p (a b)"), 256),
                         (gate_all[:].rearrange("p a b -> p (a b)"), 256), (phf[:], 256), (pendT[:], 2)]
                off = 0
                allb = [S.B(n) for n in ("cnt", "pc", "pend", "pstart", "blkf", "psk", "dest", "phf", "pendT")]
                allb += [S.B("posk", i) for i in range(NT)] + [S.B("eall", i) for i in range(NT)] + [S.B("gate", i) for i in range(NT)]
                for ap, n in items:
                    S.op("dve", lambda e, ap=ap, n=n, off=off: e.tensor_copy(out=dbg[:, off:off + n], in_=ap),
                         reads=allb, writes=[S.B("dbg")])
                    off += n
                S.dma("sp", lambda e: e.dma_start(out=g.DBG[:, :], in_=dbg[:]), reads=[S.B("dbg")])
            for i in range(NT):
                for k in range(8):
                    S.dma("pool", lambda e, i=i, k=k: e.indirect_dma_start(
                        out=g.TBL[:, :], out_offset=bass.IndirectOffsetOnAxis(ap=dest_all[:, i, k:k + 1], axis=0),
                        in_=tokid[:, i, :], in_offset=None, bounds_check=S.reg(e, NROW - 1), oob_is_err=False),
                        reads=[S.B("dest"), S.B("tokid"), S.B("TBLinit")])

        run_phase(g, body_r)
        if g.debug == 'moe%d_r' % layer:
            return

        def body_e(ph):
            ident = sbt(g, ph, "e_ident", [P, P], BF16)
            idx2 = sbt(g, ph, "e_idx", [P, 512, 2], I32)
            wi = sbt(g, ph, "e_wi", [P, 3, NBLK], I32)
            NS = 4
            wf = [sbt(g, ph, "e_wf%d" % i, [P, 6144]) for i in range(NS)]
            wgu = [sbt(g, ph, "e_wgu%d" % i, [P, KC, 512], BF16) for i in range(NS)]
            wdn = [sbt(g, ph, "e_wdn%d" % i, [P, 2, D], BF16) for i in range(NS)]
            xg = [sbt(g, ph, "e_xg%d" % i, [P, D], BF16) for i in range(NS)]
            xT = [sbt(g, ph, "e_xT%d" % i, [P, KC, P], BF16) for i in range(2)]
            sgt = [sbt(g, ph, "e_sgt%d" % i, [P, 2, P]) for i in range(2)]
            actT = [sbt(g, ph, "e_actT%d" % i, [P, 2, P], BF16) for i in range(2)]
            ysb = [sbt(g, ph, "e_ysb%d" % i, [P, D], BF16) for i in range(2)]
            ps_t = [pst(g, ph, "e_ps_t%d" % i, [P, KC, P], BF16) for i in range(2)]
            ps_a = [pst(g, ph, "e_ps_a%d" % i, [P, 4, P]) for i in range(2)]
            ps_y = [pst(g, ph, "e_ps_y%d" % i, [P, 512]) for i in range(4)]

            S.dma("pool", lambda e: e.dma_start(out=ident[:], in_=g.c_ident[:, :]), writes=[S.B("ident")])
            S.dma("sp", lambda e: e.dma_start(out=idx2[:], in_=g.TBL[0:NROW, :].rearrange("(p j) two -> p j two", j=512)),
                  writes=[S.B("idx2")])
            S.dma("sp", lambda e: e.dma_start(out=wi[:].rearrange("p a b -> p (a b)"), in_=g.WI[:, :]),
                  writes=[S.B("wi")])
            for i_ in range(NS):
                S.op("dve", lambda e, i_=i_: e.memset(xg[i_][:], 0.0), writes=[S.B("xg", i_)])

            def fetch(b):
                s = b % NS
                S.dma("pool", lambda e, s=s, b=b: e.indirect_dma_start(
                    out=wf[s][:], out_offset=None, in_=g.ew[layer][:, :],
                    in_offset=bass.IndirectOffsetOnAxis(ap=wi[:, 2, b:b + 1], axis=0),
                    bounds_check=S.reg(e, g.netab * P - 1), oob_is_err=False),
                    reads=[S.B("wi")], writes=[S.B("wf", s)])
                S.dma("pool", lambda e, s=s, b=b: e.indirect_dma_start(
                    out=xg[s][:], out_offset=None, in_=g.HB[:, :],
                    in_offset=bass.IndirectOffsetOnAxis(ap=idx2[:, b, 0:1], axis=0),
                    bounds_check=S.reg(e, SEQ - 1), oob_is_err=False),
                    reads=[S.B("idx2")], writes=[S.B("xg", s)])

            def cast_w(b):
                s = b % NS
                s2 = b % 2
                wgv = wf[s][:, 0:4096].rearrange("p (k n) -> p k n", n=512)
                wdv = wf[s][:, 4096:6144].rearrange("p (k n) -> p k n", n=D)
                S.op("act", lambda e, s=s, wgv=wgv: e.activation(out=wgu[s][:, 0:5, :], in_=wgv[:, 0:5, :], func=AF.Copy),
                     reads=[S.B("wf", s)], writes=[S.B("wgu", s, 0)])
                S.op("dve", lambda e, s=s, wgv=wgv: e.tensor_copy(out=wgu[s][:, 5:8, :], in_=wgv[:, 5:8, :]),
                     reads=[S.B("wf", s)], writes=[S.B("wgu", s, 1)])
                S.op("dve", lambda e, s=s, wdv=wdv: e.tensor_copy(out=wdn[s][:], in_=wdv),
                     reads=[S.B("wf", s)], writes=[S.B("wdn", s)])

            def stage_a(b):
                s = b % NS
                s2 = b % 2
                cast_w(b)
                for k in range(KC):
                    S.op("pe", lambda e, s=s, s2=s2, k=k: e.transpose(ps_t[s2][:, k, :], xg[s][:, k * P:(k + 1) * P],
                                                                      ident[:]),
                         reads=[S.B("xg", s), S.B("ident")], writes=[S.B("ps_t", s2)])
                S.op("act", lambda e, s2=s2: e.activation(out=xT[s2][:], in_=ps_t[s2][:], func=AF.Copy),
                     reads=[S.B("ps_t", s2)], writes=[S.B("xT", s2)])

            def stage_b(b):
                s = b % NS
                s2 = b % 2
                wb = [S.B("wgu", s, 0), S.B("wgu", s, 1)]
                for m in range(4):
                    for k in range(KC):
                        S.op("pe", lambda e, s=s, s2=s2, m=m, k=k: e.matmul(
                            ps_a[s2][:, m, :], lhsT=wgu[s][:, k, m * P:(m + 1) * P], rhs=xT[s2][:, k, :],
                            start=(k == 0), stop=(k == KC - 1)),
                            reads=[S.B("xT", s2)] + wb, writes=[S.B("ps_a", s2)])
                S.op("act", lambda e, s2=s2: e.activation(out=sgt[s2][:], in_=ps_a[s2][:, 0:2, :], func=AF.Silu),
                     reads=[S.B("ps_a", s2)], writes=[S.B("sgt", s2)])
                S.op("dve", lambda e, s2=s2: e.tensor_tensor(out=actT[s2][:], in0=sgt[s2][:], in1=ps_a[s2][:, 2:4, :],
                                                             op=ALU.mult),
                     reads=[S.B("sgt", s2), S.B("ps_a", s2)], writes=[S.B("actT", s2)])

            def stage_c(b):
                s = b % NS
                s2 = b % 2
                for n in range(2):
                    pi = s2 * 2 + n
                    for j in range(2):
                        S.op("pe", lambda e, s=s, s2=s2, n=n, j=j, pi=pi: e.matmul(
                            ps_y[pi][:], lhsT=actT[s2][:, j, :], rhs=wdn[s][:, j, n * 512:(n + 1) * 512],
                            start=(j == 0), stop=(j == 1)),
                            reads=[S.B("actT", s2), S.B("wdn", s)], writes=[S.B("ps_y", pi)])
                    if n == 0:
                        S.op("act", lambda e, s2=s2, n=n, pi=pi: e.activation(
                            out=ysb[s2][:, n * 512:(n + 1) * 512], in_=ps_y[pi][:], func=AF.Copy),
                            reads=[S.B("ps_y", pi)], writes=[S.B("ysb", s2)])
                    else:
                        S.op("dve", lambda e, s2=s2, n=n, pi=pi: e.tensor_copy(
                            out=ysb[s2][:, n * 512:(n + 1) * 512], in_=ps_y[pi][:]),
                            reads=[S.B("ps_y", pi)], writes=[S.B("ysb", s2)])
                S.dma("sp", lambda e, s2=s2, b=b: e.dma_start(out=YEv[:, b, :], in_=ysb[s2][:]),
                      reads=[S.B("ysb", s2)])

            fetch(0)
            fetch(1)
            fetch(2)
            for it in range(NBLK + 2):
                if it + 3 < NBLK:
                    fetch(it + 3)
                if it < NBLK:
                    stage_a(it)
                if 0 <= it - 1 < NBLK:
                    stage_b(it - 1)
                if 0 <= it - 2 < NBLK:
                    stage_c(it - 2)

        run_phase(g, body_e)
        if g.debug == 'moe%d_e' % layer:
            return

        def body_c(ph):
            gate5 = sbt(g, ph, "c_gate5", [P, D])
            lng = sbt(g, ph, "c_lng", [P, D])
            lnb = sbt(g, ph, "c_lnb", [P, D])
            xt = [sbt(g, ph, "c_xt%d" % i, [P, D]) for i in range(2)]
            acc = [sbt(g, ph, "c_acc%d" % i, [P, D]) for i in range(2)]
            R = [sbt(g, ph, "c_R%d" % i, [P, 8, D], BF16) for i in range(2)]
            stats = sbt(g, ph, "c_stats", [P, 2, 6])
            mv = sbt(g, ph, "c_mv", [P, 2])
            lrs = sbt(g, ph, "c_lrs", [P, 1])
            xo = [sbt(g, ph, "c_xo%d" % i, [P, D]) for i in range(2)]
            load_mod(g, S, gate5, midx, 5)
            load_bcast(g, S, lng, g.ln_g[layer, 1:2, :])
            load_bcast(g, S, lnb, g.ln_b[layer, 1:2, :])
            for i in range(NT):
                s = i % 2
                S.dma("sp", lambda e, s=s, i=i: e.dma_start(out=xt[s][:], in_=XIN[i * P:(i + 1) * P, :]),
                      writes=[S.B("xt", s)])
                S.dma("sp", lambda e, s=s, i=i: e.dma_start(out=acc[s][:], in_=g.SH[i * P:(i + 1) * P, :]),
                      writes=[S.B("acc", s)])
                for k in range(8):
                    S.dma("pool", lambda e, s=s, i=i, k=k: e.indirect_dma_start(
                        out=R[s][:, k, :], out_offset=None, in_=g.YE[:, :],
                        in_offset=bass.IndirectOffsetOnAxis(ap=dest_all[:, i, k:k + 1], axis=0),
                        bounds_check=S.reg(e, NROW - 1), oob_is_err=False),
                        writes=[S.B("R", s, k)])
                for k in range(8):
                    S.op("dve", lambda e, s=s, i=i, k=k: e.scalar_tensor_tensor(
                        out=acc[s][:], in0=R[s][:, k, :], scalar=gate_all[:, i, k:k + 1], in1=acc[s][:],
                        op0=ALU.mult, op1=ALU.add),
                        reads=[S.B("R", s, k), S.B("acc", s)], writes=[S.B("acc", s)])
                S.op("pool", lambda e, s=s: e.tensor_tensor(out=acc[s][:], in0=acc[s][:], in1=gate5[:], op=ALU.mult),
                     reads=[S.B("acc", s), S.B("modt", gate5.name)], writes=[S.B("acc", s)])
                S.op("dve", lambda e, s=s: e.scalar_tensor_tensor(out=acc[s][:], in0=xt[s][:], scalar=DN_ALPHA,
                                                                  in1=acc[s][:], op0=ALU.mult, op1=ALU.add),
                     reads=[S.B("xt", s), S.B("acc", s)], writes=[S.B("acc", s)])
                layer_norm_store(g, S, "lnc", acc[s], S.B("acc", s), lng, lnb, stats, mv, lrs, xo[s], S.B("xo", s),
                                 XOUT[i * P:(i + 1) * P, :], S.B("XOUT", i))

        run_phase(g, body_c)


def phase_conv(g, st):
    S = g.S
    g.sfx = ""

    def body1(ph):
        win = sbt(g, ph, "v_win", [P, KC, 3 * D], BF16)
        ident = sbt(g, ph, "v_ident", [P, P], BF16)
        shx = sbt(g, ph, "v_shx", [P, D])
        scx = sbt(g, ph, "v_scx", [P, D])
        zrow = sbt(g, ph, "v_zrow", [1, D])
        xt = [sbt(g, ph, "v_xt%d" % i, [P, D]) for i in range(2)]
        tmp = sbt(g, ph, "v_tmp", [P, D])
        hb = [sbt(g, ph, "v_hb%d" % i, [P, D], BF16) for i in range(2)]
        hT = [sbt(g, ph, "v_hT%d" % i, [P, KC, P], BF16) for i in range(2)]
        bgt = [sbt(g, ph, "v_bgt%d" % i, [P, D]) for i in range(2)]
        vt = sbt(g, ph, "v_vt", [P, D])
        ut = [sbt(g, ph, "v_ut%d" % i, [P, D]) for i in range(2)]
        ps_tp = pst(g, ph, "v_ps_tp", [P, KC, P], BF16)
        ps = [pst(g, ph, "v_ps%d" % i, [P, 512]) for i in range(6)]
        for c in range(2):
            S.dma("pool", lambda e, c=c: e.dma_start(
                out=win[:, :, c * 1536:(c + 1) * 1536],
                in_=g.w_in[:, c * 1536:(c + 1) * 1536].rearrange("(k p) n -> p k n", p=P)), writes=[S.B("win", c)])
        S.dma("pool", lambda e: e.dma_start(out=ident[:], in_=g.c_ident[:, :]), writes=[S.B("ident")])
        load_mod(g, S, shx, 2, 0)
        load_mod(g, S, scx, 2, 1)
        S.op("dve", lambda e: e.memset(zrow[:], 0.0), writes=[S.B("zrow")])
        S.dma("sp", lambda e: e.dma_start(out=g.UU[0:1, :], in_=zrow[:]), reads=[S.B("zrow")])
        S.dma("sp", lambda e: e.dma_start(out=g.UU[SEQ + 1:SEQ + 2, :], in_=zrow[:]), reads=[S.B("zrow")])
        wb = [S.B("win", 0), S.B("win", 1)]
        for i in range(NT):
            s = i % 2
            xb = S.B("xt", s)
            S.dma("sp", lambda e, s=s, i=i: e.dma_start(out=xt[s][:], in_=g.X2[i * P:(i + 1) * P, :]), writes=[xb])
            modulate_transpose(g, S, xt[s][:], xb, scx, shx, tmp, S.B("tmp"), hb[s], S.B("hb", s),
                               ps_tp, S.B("ps_tp"), hT[s], S.B("hT", s), ident, S.B("ident"))
            for n in range(6):
                for k in range(KC):
                    S.op("pe", lambda e, s=s, n=n, k=k: e.matmul(ps[n][:], lhsT=hT[s][:, k, :],
                                                                 rhs=win[:, k, n * 512:(n + 1) * 512],
                                                                 start=(k == 0), stop=(k == KC - 1)),
                         reads=[S.B("hT", s)] + wb, writes=[S.B("ps", n)])
            for n in range(2):
                S.op("act", lambda e, s=s, n=n: e.activation(out=bgt[s][:, n * 512:(n + 1) * 512], in_=ps[n][:],
                                                             func=AF.Copy),
                     reads=[S.B("ps", n)], writes=[S.B("bgt", s)])
                S.op("act", lambda e, n=n: e.activation(out=vt[:, n * 512:(n + 1) * 512], in_=ps[4 + n][:],
                                                        func=AF.Copy),
                     reads=[S.B("ps", 4 + n)], writes=[S.B("vt")])
                S.op("dve", lambda e, s=s, n=n: e.tensor_tensor(out=ut[s][:, n * 512:(n + 1) * 512], in0=ps[2 + n][:],
                                                                in1=vt[:, n * 512:(n + 1) * 512], op=ALU.mult),
                     reads=[S.B("ps", 2 + n), S.B("vt")], writes=[S.B("ut", s)])
            S.dma("sp", lambda e, s=s, i=i: e.dma_start(out=g.BG[i * P:(i + 1) * P, :], in_=bgt[s][:]),
                  reads=[S.B("bgt", s)])
            S.dma("sp", lambda e, s=s, i=i: e.dma_start(out=g.UU[1 + i * P:1 + (i + 1) * P, :], in_=ut[s][:]),
                  reads=[S.B("ut", s)])

    run_phase(g, body1)

    def body2(ph):
        wout = sbt(g, ph, "w_wout", [P, KC, D], BF16)
        ident = sbt(g, ph, "w_ident", [P, P], BF16)
        tp = [sbt(g, ph, "w_tap%d" % i, [P, D]) for i in range(3)]
        gate = sbt(g, ph, "w_gate", [P, D])
        lng = sbt(g, ph, "w_lng", [P, D])
        lnb = sbt(g, ph, "w_lnb", [P, D])
        xt = [sbt(g, ph, "w_xt%d" % i, [P, D]) for i in range(2)]
        up = [sbt(g, ph, "w_up%d" % i, [P, D]) for i in range(2)]
        uc = [sbt(g, ph, "w_uc%d" % i, [P, D]) for i in range(2)]
        un = [sbt(g, ph, "w_un%d" % i, [P, D]) for i in range(2)]
        bgt = [sbt(g, ph, "w_bgt%d" % i, [P, D]) for i in range(2)]
        zb = sbt(g, ph, "w_zb", [P, D], BF16)
        zT = sbt(g, ph, "w_zT", [P, KC, P], BF16)
        y1 = sbt(g, ph, "w_y1", [P, D])
        stats = sbt(g, ph, "w_stats", [P, 2, 6])
        mv = sbt(g, ph, "w_mv", [P, 2])
        lrs = sbt(g, ph, "w_lrs", [P, 1])
        xo = [sbt(g, ph, "w_xo%d" % i, [P, D]) for i in range(2)]
        ps_tp = pst(g, ph, "w_ps_tp", [P, KC, P], BF16)
        ps_p = [pst(g, ph, "w_ps_p%d" % i, [P, 512]) for i in range(2)]
        S.dma("pool", lambda e: e.dma_start(out=wout[:], in_=g.w_out.rearrange("(k p) n -> p k n", p=P)),
              writes=[S.B("wout")])
        S.dma("pool", lambda e: e.dma_start(out=ident[:], in_=g.c_ident[:, :]), writes=[S.B("ident")])
        for j in range(3):
            load_bcast(g, S, tp[j], g.taps[j:j + 1, :])
        load_mod(g, S, gate, 2, 2)
        load_bcast(g, S, lng, g.ln_g[1, 0:1, :])
        load_bcast(g, S, lnb, g.ln_b[1, 0:1, :])
        for i in range(NT):
            s = i % 2
            S.dma("sp", lambda e, s=s, i=i: e.dma_start(out=xt[s][:], in_=g.X2[i * P:(i + 1) * P, :]),
                  writes=[S.B("xt", s)])
            S.dma("sp", lambda e, s=s, i=i: e.dma_start(out=up[s][:], in_=g.UU[i * P:(i + 1) * P, :]),
                  writes=[S.B("up", s)])
            S.dma("sp", lambda e, s=s, i=i: e.dma_start(out=uc[s][:], in_=g.UU[1 + i * P:1 + (i + 1) * P, :]),
                  writes=[S.B("uc", s)])
            S.dma("sp", lambda e, s=s, i=i: e.dma_start(out=un[s][:], in_=g.UU[2 + i * P:2 + (i + 1) * P, :]),
                  writes=[S.B("un", s)])
            S.dma("sp", lambda e, s=s, i=i: e.dma_start(out=bgt[s][:], in_=g.BG[i * P:(i + 1) * P, :]),
                  writes=[S.B("bgt", s)])
            S.op("dve", lambda e, s=s: e.tensor_tensor(out=up[s][:], in0=up[s][:], in1=tp[0][:], op=ALU.mult),
                 reads=[S.B("up", s), S.B("modt", tp[0].name)], writes=[S.B("up", s)])
            S.op("pool", lambda e, s=s: e.tensor_tensor(out=uc[s][:], in0=uc[s][:], in1=tp[1][:], op=ALU.mult),
                 reads=[S.B("uc", s), S.B("modt", tp[1].name)], writes=[S.B("uc", s)])
            S.op("pool", lambda e, s=s: e.tensor_tensor(out=un[s][:], in0=un[s][:], in1=tp[2][:], op=ALU.mult),
                 reads=[S.B("un", s), S.B("modt", tp[2].name)], writes=[S.B("un", s)])
            S.op("dve", lambda e, s=s: e.tensor_tensor(out=up[s][:], in0=up[s][:], in1=uc[s][:], op=ALU.add),
                 reads=[S.B("up", s), S.B("uc", s)], writes=[S.B("up", s)])
            S.op("dve", lambda e, s=s: e.tensor_tensor(out=up[s][:], in0=up[s][:], in1=un[s][:], op=ALU.add),
                 reads=[S.B("up", s), S.B("un", s)], writes=[S.B("up", s)])
            S.op("dve", lambda e, s=s: e.tensor_tensor(out=zb[:], in0=up[s][:], in1=bgt[s][:], op=ALU.mult),
                 reads=[S.B("up", s), S.B("bgt", s)], writes=[S.B("zb")])
            for k in range(KC):
                S.op("pe", lambda e, k=k: e.transpose(ps_tp[:, k, :], zb[:, k * P:(k + 1) * P], ident[:]),
                     reads=[S.B("zb"), S.B("ident")], writes=[S.B("ps_tp")])
            S.op("act", lambda e: e.activation(out=zT[:], in_=ps_tp[:], func=AF.Copy),
                 reads=[S.B("ps_tp")], writes=[S.B("zT")])
            for n in range(2):
                for k in range(KC):
                    S.op("pe", lambda e, n=n, k=k: e.matmul(ps_p[n][:], lhsT=zT[:, k, :],
                                                            rhs=wout[:, k, n * 512:(n + 1) * 512],
                                                            start=(k == 0), stop=(k == KC - 1)),
                         reads=[S.B("zT"), S.B("wout")], writes=[S.B("ps_p", n)])
                S.op("dve", lambda e, n=n: e.tensor_tensor(out=y1[:, n * 512:(n + 1) * 512], in0=ps_p[n][:],
                                                           in1=gate[:, n * 512:(n + 1) * 512], op=ALU.mult),
                     reads=[S.B("ps_p", n), S.B("modt", gate.name)], writes=[S.B("y1")])
            S.op("dve", lambda e, s=s: e.scalar_tensor_tensor(out=y1[:], in0=xt[s][:], scalar=DN_ALPHA, in1=y1[:],
                                                              op0=ALU.mult, op1=ALU.add),
                 reads=[S.B("xt", s), S.B("y1")], writes=[S.B("y1")])
            layer_norm_store(g, S, "lnv", y1, S.B("y1"), lng, lnb, stats, mv, lrs, xo[s], S.B("xo", s),
                             g.X3[i * P:(i + 1) * P, :], S.B("X3", i))

    run_phase(g, body2)


_CACHE = {}


def _consts():
    ident = np.eye(P, dtype=np.float32)
    t = np.arange(SEQ)
    row = (t // 64).astype(np.float32)
    col = (t % 64).astype(np.float32)
    freqs = (np.float32(10000.0) ** (-np.arange(0, 64, 2, dtype=np.float32) / np.float32(64))).astype(np.float32)
    ang = np.concatenate([row[:, None] * freqs[None, :], col[:, None] * freqs[None, :]], axis=-1).astype(np.float32)
    cos = np.cos(ang).astype(np.float32)
    sin = np.sin(ang).astype(np.float32)
    iota = np.tile(np.arange(NE, dtype=np.float32)[None, :], (P, 1))
    cbase = np.zeros((P, NE), dtype=np.float32)
    iotab = np.zeros((P, NBLK + 1), dtype=np.float32)
    iotab[:, :NBLK] = (np.arange(NBLK) * P)[None, :]
    iotab[:, NBLK] = np.arange(P)
    triu = np.triu(np.ones((P, P), dtype=np.float32), k=1)
    tokid = np.zeros((P, NT, 2), dtype=np.int32)
    tokid[:, :, 0] = np.arange(NT)[None, :] * P + np.arange(P)[:, None]
    tokid = tokid.reshape(P, NT * 2)
    pidx = np.arange(P, dtype=np.float32).reshape(P, 1)
    bigv = np.zeros((P, 2), dtype=np.float32)
    bigv[P - 1, 1] = 1e9
    ee = np.arange(NE)
    ucum = np.zeros((P, 2, NE), dtype=np.float32)
    for c in range(2):
        ucum[:, c, :] = ((c * P + np.arange(P))[:, None] <= ee[None, :]).astype(np.float32)
    ucum = ucum.reshape(P, 2 * NE)
    return dict(c_pidx=pidx, c_bigv=bigv, c_ucum=ucum, c_ident=ident, c_cos=cos, c_sin=sin, c_iota=iota, c_cbase=cbase, c_triu=triu, c_tokid=tokid, c_iotab=iotab)


def _relayout(w, nk, lite):
    w = np.asarray(w, dtype=np.float32)
    if lite:
        w = w[:, 0:1]
    L, E, R, N = w.shape
    return np.ascontiguousarray(w.reshape(L, E, nk, P, N).transpose(0, 1, 3, 2, 4))


def _merge_w(inputs, l, lite):
    a = _relayout(inputs["exp_w_gate_up"][l:l + 1], KC, lite).reshape(-1, 4096)
    b = _relayout(inputs["exp_w_down"][l:l + 1], 2, lite).reshape(-1, 2048)
    return np.ascontiguousarray(np.concatenate([a, b], axis=1))


def make_in_maps(inputs, lite=False, cores=range(8)):
    f = lambda a: np.ascontiguousarray(np.asarray(a, dtype=np.float32))
    shared = dict(
        ada_w=f(inputs["ada_w"]), ada_b=f(inputs["ada_b"]), ln_g=f(inputs["ln_g"]), ln_b=f(inputs["ln_b"]),
        w_qkv=f(inputs["attn_w_qkv"][0]), qn=f(inputs["attn_q_norm"][0]).reshape(1, P),
        kn=f(inputs["attn_k_norm"][0]).reshape(1, P), w_o=f(inputs["attn_w_o"][0]),
        w_in=f(inputs["conv_w_in"][0]), taps=f(inputs["conv_taps"][0]), w_out=f(inputs["conv_w_out"][0]),
        router_w=f(inputs["router_w"]), router_b=f(inputs["router_bias"]),
        ew0=_merge_w(inputs, 0, lite), ew1=_merge_w(inputs, 1, lite),
        sgu=f(inputs["shared_w_gate_up"]), sdn=f(inputs["shared_w_down"]),
    )
    shared.update(_consts())
    ccT = f(np.asarray(inputs["c_ctx"]).reshape(KC, P).T)
    maps = []
    for b in cores:
        m = dict(shared)
        m["x"] = f(inputs["x"][b])
        m["ctx"] = f(inputs["ctx"][b])
        m["cT"] = f(np.asarray(inputs["c"][b]).reshape(KC, P).T)
        m["ccT"] = ccT
        maps.append(m)
    return maps


def kernel(**inputs):
    if "nc" not in _CACHE:
        _CACHE["nc"] = build_program()
    nc = _CACHE["nc"]
    maps = make_in_maps(inputs)
    res = run_bass_kernel_spmd(nc, maps, core_ids=list(range(8)))
    return np.stack([np.asarray(r["y"], dtype=np.float32) for r in res.results], axis=0)
```

```python
import contextlib
import numpy as np
import concourse.bass as bass
import concourse.mybir as mybir
from concourse.bass_utils import run_bass_kernel_spmd

F32 = mybir.dt.float32
BF16 = mybir.dt.bfloat16
I32 = mybir.dt.int32
U32 = mybir.dt.uint32
AF = mybir.ActivationFunctionType
ALU = mybir.AluOpType
AX = mybir.AxisListType

P = 128
D = 1024
KC = 8
SEQ = 4096
CTX = 256
NT = SEQ // P
NKC = (SEQ + CTX) // P
NE = 256
NBLK = 512
NROW = NBLK * P
DN_ALPHA = 4.0 ** 0.25
LN_EPS = 1e-5
QK_EPS = 1e-6
SAME_SYNC = True


class Buf:
    __slots__ = ("lw", "rd")

    def __init__(self):
        self.lw = None
        self.rd = {}


class Sched:
    COMPUTE = ("pe", "act", "dve", "pool")

    def __init__(self, nc, st, nds=20):
        self.nc = nc
        self.names = ["pe", "act", "dve", "pool", "sp"]
        self.csem = {n: st.enter_context(nc.semaphore("c_" + n)) for n in self.names}
        self.cnt = {n: 0 for n in self.names}
        self.dsem = [st.enter_context(nc.semaphore("d%d" % i)) for i in range(nds)]
        self.dcnt = [0] * nds
        self.dnext = 0
        self.prog = {n: [] for n in self.names}
        self.known = {n: {} for n in self.names}
        self.bufs = {}
        self.ninstr = 0

    def B(self, *key):
        b = self.bufs.get(key)
        if b is None:
            b = self.bufs[key] = Buf()
        return b

    def _semobj(self, key):
        return self.csem[key] if isinstance(key, str) else self.dsem[key[1]]

    def _wait(self, eng, ev):
        key, val = ev
        if val <= 0 or self.known[eng].get(key, 0) >= val:
            return
        self.known[eng][key] = val
        sem = self._semobj(key)
        self.prog[eng].append(lambda e, sem=sem, val=val: e.wait_ge(sem, val))
        self.ninstr += 1

    def _sync(self, eng, reads, writes):
        evs = []
        for b in reads:
            if b.lw is not None:
                evs.append((b.lw, True))
        for b in writes:
            if b.lw is not None:
                evs.append((b.lw, True))
            for k, v in b.rd.items():
                evs.append(((k, v), False))
        for ev, strong in evs:
            if ev[0] == eng:
                if eng == "pe" or not (SAME_SYNC and strong):
                    continue
            self._wait(eng, ev)

    def _mark(self, ev, reads, writes):
        for b in reads:
            if b.rd.get(ev[0], 0) < ev[1]:
                b.rd[ev[0]] = ev[1]
        for b in writes:
            b.lw = ev
            b.rd = {}

    def op(self, eng, fn, reads=(), writes=()):
        self._sync(eng, reads, writes)
        self.cnt[eng] += 1
        ev = (eng, self.cnt[eng])
        sem = self.csem[eng]
        self.prog[eng].append(lambda e, fn=fn, sem=sem: fn(e).then_inc(sem, 1))
        self.ninstr += 1
        self._mark(ev, reads, writes)

    def dma(self, q, fn, reads=(), writes=()):
        self._sync(q, reads, writes)
        j = self.dnext
        self.dnext = (j + 1) % len(self.dsem)
        self._wait(q, (("d", j), self.dcnt[j]))
        self.dcnt[j] += 16
        ev = (("d", j), self.dcnt[j])
        sem = self.dsem[j]
        self.prog[q].append(lambda e, fn=fn, sem=sem: fn(e).then_inc(sem, 16))
        self.ninstr += 1
        self._mark(ev, reads, writes)

    def barrier(self):
        evs = [(n, self.cnt[n]) for n in self.COMPUTE]
        evs += [(("d", j), c) for j, c in enumerate(self.dcnt)]
        for n in self.names:
            for ev in evs:
                if ev[0] != n:
                    self._wait(n, ev)
        self.bufs = {}

    def reg(self, e, val):
        r = self.regcache.get(val)
        if r is None:
            r = self.regcache[val] = e.to_reg(val)
        return r

    def emit(self):
        self.regcache = {}
        with self.nc.Block() as block:
            for n, deco in (("pe", block.tensor), ("act", block.scalar), ("dve", block.vector),
                            ("pool", block.gpsimd), ("sp", block.sync)):
                prog = self.prog[n]

                def body(e, prog=prog):
                    for f in prog:
                        f(e)
                deco(body)
                self.prog[n] = []


class Ctx:
    pass


def build_program(debug=None, debug_outs=(), lite=False):
    nc = bass.Bass("TRN2", target_bir_lowering=False)
    g = Ctx()
    g.nc = nc
    g.debug = debug
    g.sfx = ""
    g.netab = 1 if lite else NE

    def din(name, shape, dt=F32):
        return nc.dram_tensor(name, list(shape), dt, kind="ExternalInput").ap()

    def dscr(name, shape, dt=F32):
        if debug_outs and name in debug_outs:
            return nc.dram_tensor(name, list(shape), dt, kind="ExternalOutput").ap()
        return nc.dram_tensor(name, list(shape), dt).ap()

    g.x = din("x", [SEQ, D])
    g.ctx = din("ctx", [CTX, D])
    g.cT = din("cT", [P, KC])
    g.ccT = din("ccT", [P, KC])
    g.ada_w = din("ada_w", [2, D, 6 * D])
    g.ada_b = din("ada_b", [2, 6 * D])
    g.ln_g = din("ln_g", [2, 2, D])
    g.ln_b = din("ln_b", [2, 2, D])
    g.w_qkv = din("w_qkv", [D, 1536])
    g.qn = din("qn", [1, P])
    g.kn = din("kn", [1, P])
    g.w_o = din("w_o", [D, D])
    g.w_in = din("w_in", [D, 3 * D])
    g.taps = din("taps", [3, D])
    g.w_out = din("w_out", [D, D])
    g.router_w = din("router_w", [2, D, NE])
    g.router_b = din("router_b", [2, NE])
    g.ew = [din("ew%d" % l, [(1 if lite else NE) * P, 6144]) for l in range(2)]
    g.c_iotab = din("c_iotab", [P, NBLK + 1])
    g.c_pidx = din("c_pidx", [P, 1])
    g.c_bigv = din("c_bigv", [P, 2])
    g.c_ucum = din("c_ucum", [P, 2 * NE])
    g.sgu = din("sgu", [2, D, 512])
    g.sdn = din("sdn", [2, 256, D])
    g.c_ident = din("c_ident", [P, P])
    g.c_cos = din("c_cos", [SEQ, 64])
    g.c_sin = din("c_sin", [SEQ, 64])
    g.c_iota = din("c_iota", [P, NE])
    g.c_cbase = din("c_cbase", [P, NE])
    g.c_triu = din("c_triu", [P, P])
    g.c_tokid = din("c_tokid", [P, NT * 2], I32)
    g.y = nc.dram_tensor("y", [SEQ, D], F32, kind="ExternalOutput").ap()

    g.MODD = dscr("MODD", [3, P, 6 * D])
    g.X1 = dscr("X1", [SEQ, D])
    g.X2 = dscr("X2", [SEQ, D])
    g.X3 = dscr("X3", [SEQ, D])
    g.HB = dscr("HB", [SEQ + 1, D], BF16)
    g.SH = dscr("SH", [SEQ, D])
    g.TBL = dscr("TBL", [NROW + 1, 2], I32)
    g.YE = dscr("YE", [NROW + 1, D], BF16)
    g.UU = dscr("UU", [SEQ + 2, D])
    g.BG = dscr("BG", [SEQ, D])
    g.WI = dscr("WI", [P, 3 * NBLK], I32)
    g.DBG = dscr("DBG", [P, 4096])

    with contextlib.ExitStack() as st:
        S = Sched(nc, st)
        g.S = S
        stages = [
            ("adaln", phase_adaln),
            ("attn", phase_attn),
            ("moe0", lambda g_, st_: phase_moe(g_, st_, 0, g.X1, g.X2)),
            ("conv", phase_conv),
            ("moe1", lambda g_, st_: phase_moe(g_, st_, 1, g.X3, g.y)),
        ]
        for name, fn in stages:
            fn(g, st)
            if debug and debug.startswith(name):
                break
        S.barrier()
        S.emit()
    return nc


def run_phase(g, fn):
    S = g.S
    with contextlib.ExitStack() as ph:
        S.barrier()
        fn(ph)
        S.barrier()
        S.emit()


def sbt(g, ph, name, shape, dt=F32):
    return ph.enter_context(g.nc.sbuf_tensor(name + g.sfx, list(shape), dt))


def pst(g, ph, name, shape, dt=F32):
    return ph.enter_context(g.nc.psum_tensor(name + g.sfx, list(shape), dt))


def phase_adaln(g, st):
    S = g.S

    def body(ph):
        sc = sbt(g, ph, "ad_sc", [P, 2, KC])
        scs = sbt(g, ph, "ad_scs", [P, 2, KC])
        scb = sbt(g, ph, "ad_scb", [P, 2, KC, P])
        wt = [sbt(g, ph, "ad_wt%d" % i, [P, KC, 512]) for i in range(2)]
        bt = [sbt(g, ph, "ad_bt%d" % i, [P, 512]) for i in range(2)]
        res = [sbt(g, ph, "ad_res%d" % i, [P, 512]) for i in range(3)]
        ps = [pst(g, ph, "ad_ps%d" % i, [P, 512]) for i in range(2)]
        S.dma("sp", lambda e: e.dma_start(out=sc[:, 0, :], in_=g.cT[:, :]), writes=[S.B("sc")])
        S.dma("sp", lambda e: e.dma_start(out=sc[:, 1, :], in_=g.ccT[:, :]), writes=[S.B("sc")])
        S.op("act", lambda e: e.activation(out=scs[:], in_=sc[:], func=AF.Silu),
             reads=[S.B("sc")], writes=[S.B("scs")])
        S.op("dve", lambda e: e.tensor_copy(out=scb[:], in_=scs[:].unsqueeze(3).to_broadcast([P, 2, KC, P])),
             reads=[S.B("scs")], writes=[S.B("scb")])
        it = 0
        rs = 0
        for layer in range(2):
            kinds = [(0, 0), (1, 1)] if layer == 0 else [(0, 2)]
            for n in range(12):
                s = it % 2
                it += 1
                wsrc = g.ada_w[layer, :, n * 512:(n + 1) * 512].rearrange("(k p) n -> p k n", p=P)
                S.dma("sp", lambda e, s=s, wsrc=wsrc: e.dma_start(out=wt[s][:], in_=wsrc),
                      writes=[S.B("wt", s)])
                bsrc = g.ada_b[layer:layer + 1, n * 512:(n + 1) * 512].to_broadcast([P, 512])
                S.dma("sp", lambda e, s=s, bsrc=bsrc: e.dma_start(out=bt[s][:], in_=bsrc),
                      writes=[S.B("bt", s)])
                for kind, idx in kinds:
                    pp = (it + kind) % 2
                    for k in range(KC):
                        S.op("pe", lambda e, pp=pp, kind=kind, k=k, s=s: e.matmul(
                            ps[pp][:], lhsT=scb[:, kind, k, :], rhs=wt[s][:, k, :],
                            start=(k == 0), stop=(k == KC - 1)),
                            reads=[S.B("scb"), S.B("wt", s)], writes=[S.B("ps", pp)])
                    r = rs % 3
                    rs += 1
                    add1 = 1.0 if n in (2, 3, 8, 9) else 0.0
                    S.op("dve", lambda e, r=r, pp=pp, s=s, add1=add1: e.scalar_tensor_tensor(
                        out=res[r][:], in0=ps[pp][:], scalar=add1, in1=bt[s][:], op0=ALU.add, op1=ALU.add),
                        reads=[S.B("ps", pp), S.B("bt", s)], writes=[S.B("res", r)])
                    dst = g.MODD[idx, :, n * 512:(n + 1) * 512]
                    S.dma("sp", lambda e, r=r, dst=dst: e.dma_start(out=dst, in_=res[r][:]),
                          reads=[S.B("res", r)], writes=[S.B("MODD", idx, n)])

    run_phase(g, body)


def load_mod(g, S, tile, idx, j, q="sp"):
    src = g.MODD[idx, :, j * D:(j + 1) * D]
    S.dma(q, lambda e: e.dma_start(out=tile[:], in_=src), writes=[S.B("modt", tile.name)])
    return S.B("modt", tile.name)


def load_bcast(g, S, tile, src_row, q="sp"):
    n = tile.shape[1]
    src = src_row.to_broadcast([P, n])
    S.dma(q, lambda e: e.dma_start(out=tile[:], in_=src), writes=[S.B("modt", tile.name)])
    return S.B("modt", tile.name)


def modulate_transpose(g, S, xt, xb, sc_t, sh_t, tmp, tmpb, hb, hbb, tp, tpb, hT, hTb, ident, identb, extra_reads=()):
    S.op("dve", lambda e: e.tensor_tensor(out=tmp[:], in0=xt, in1=sc_t[:], op=ALU.mult),
         reads=[xb, S.B("modt", sc_t.name)], writes=[tmpb])
    S.op("pool", lambda e: e.tensor_tensor(out=hb[:], in0=tmp[:], in1=sh_t[:], op=ALU.add),
         reads=[tmpb, S.B("modt", sh_t.name)], writes=[hbb])
    for k in range(KC):
        S.op("pe", lambda e, k=k: e.transpose(tp[:, k, :], hb[:, k * P:(k + 1) * P], ident[:]),
             reads=[hbb, identb], writes=[tpb])
    S.op("act", lambda e: e.activation(out=hT[:], in_=tp[:], func=AF.Copy), reads=[tpb], writes=[hTb])


def rms_rope(g, S, pfx, src_ps, src_b, nh, gain, gainb, cs, sn, csb, do_rope, sq, ss, rstd, qn, t1, t2, t3, t4, out_bf, out_b):
    B = lambda n: S.B("y1") if n == "sq" else S.B(pfx, n)
    S.op("act", lambda e: e.activation(out=sq[:, 0:nh * P], in_=src_ps, func=AF.Square),
         reads=[src_b], writes=[B("sq")])
    S.op("dve", lambda e: e.tensor_reduce(out=ss[:, 0:nh], in_=sq[:, 0:nh * P].rearrange("p (h d) -> p h d", d=P),
                                          axis=AX.X, op=ALU.add), reads=[B("sq")], writes=[B("ss")])
    S.op("dve", lambda e: e.tensor_scalar(out=ss[:, 0:nh], in0=ss[:, 0:nh], scalar1=1.0 / P, scalar2=QK_EPS,
                                          op0=ALU.mult, op1=ALU.add), reads=[B("ss")], writes=[B("ss")])
    S.op("act", lambda e: e.activation(out=ss[:, 0:nh], in_=ss[:, 0:nh], func=AF.Sqrt),
         reads=[B("ss")], writes=[B("ss")])
    S.op("dve", lambda e: e.reciprocal(out=rstd[:, 0:nh], in_=ss[:, 0:nh]), reads=[B("ss")], writes=[B("rstd")])
    S.op("dve", lambda e: e.tensor_tensor(
        out=qn[:, 0:nh, :], in0=src_ps.rearrange("p (h d) -> p h d", d=P),
        in1=rstd[:, 0:nh].unsqueeze(2).to_broadcast([P, nh, P]), op=ALU.mult),
        reads=[src_b, B("rstd")], writes=[B("qn")])
    if not do_rope:
        S.op("dve", lambda e: e.tensor_tensor(
            out=out_bf[:, 0:nh, :], in0=qn[:, 0:nh, :], in1=gain[:].unsqueeze(1).to_broadcast([P, nh, P]),
            op=ALU.mult), reads=[B("qn"), gainb], writes=[out_b])
        return
    S.op("pool", lambda e: e.tensor_tensor(
        out=qn[:, 0:nh, :], in0=qn[:, 0:nh, :], in1=gain[:].unsqueeze(1).to_broadcast([P, nh, P]),
        op=ALU.mult), reads=[B("qn"), gainb], writes=[B("qn")])
    q4 = qn[:, 0:nh, :].rearrange("p h (j two) -> p h j two", two=2)
    x0 = q4[:, :, :, 0]
    x1 = q4[:, :, :, 1]
    o4 = out_bf[:, 0:nh, :].rearrange("p h (j two) -> p h j two", two=2)
    cb = cs[:].unsqueeze(1).to_broadcast([P, nh, 64])
    sb_ = sn[:].unsqueeze(1).to_broadcast([P, nh, 64])
    S.op("dve", lambda e: e.tensor_tensor(out=t1[:, 0:nh, :], in0=x0, in1=cb, op=ALU.mult),
         reads=[B("qn"), csb], writes=[B("t1")])
    S.op("pool", lambda e: e.tensor_tensor(out=t2[:, 0:nh, :], in0=x1, in1=sb_, op=ALU.mult),
         reads=[B("qn"), csb], writes=[B("t2")])
    S.op("dve", lambda e: e.tensor_tensor(out=t3[:, 0:nh, :], in0=x0, in1=sb_, op=ALU.mult),
         reads=[B("qn"), csb], writes=[B("t3")])
    S.op("pool", lambda e: e.tensor_tensor(out=t4[:, 0:nh, :], in0=x1, in1=cb, op=ALU.mult),
         reads=[B("qn"), csb], writes=[B("t4")])
    S.op("dve", lambda e: e.tensor_tensor(out=o4[:, :, :, 0], in0=t1[:, 0:nh, :], in1=t2[:, 0:nh, :], op=ALU.subtract),
         reads=[B("t1"), B("t2")], writes=[out_b])
    S.op("dve", lambda e: e.tensor_tensor(out=o4[:, :, :, 1], in0=t3[:, 0:nh, :], in1=t4[:, 0:nh, :], op=ALU.add),
         reads=[B("t3"), B("t4")], writes=[out_b])


def layer_norm_store(g, S, pfx, t2, t2b, lng, lnb, stats, mv, rstd, xo, xob, dst, dstb):
    B = lambda n: S.B(pfx, n)
    for hh in range(2):
        S.op("dve", lambda e, hh=hh: e.bn_stats(out=stats[:, hh, :], in_=t2[:, hh * 512:(hh + 1) * 512]),
             reads=[t2b], writes=[B("stats")])
    S.op("dve", lambda e: e.bn_aggr(out=mv[:], in_=stats[:].rearrange("p a b -> p (a b)")),
         reads=[B("stats")], writes=[B("mv")])
    S.op("dve", lambda e: e.tensor_scalar(out=rstd[:], in0=mv[:, 1:2], scalar1=LN_EPS, scalar2=None, op0=ALU.add),
         reads=[B("mv")], writes=[B("rstd")])
    S.op("act", lambda e: e.activation(out=rstd[:], in_=rstd[:], func=AF.Sqrt), reads=[B("rstd")], writes=[B("rstd")])
    S.op("dve", lambda e: e.reciprocal(out=rstd[:], in_=rstd[:]), reads=[B("rstd")], writes=[B("rstd")])
    S.op("dve", lambda e: e.tensor_scalar(out=t2[:], in0=t2[:], scalar1=mv[:, 0:1], scalar2=rstd[:, 0:1],
                                          op0=ALU.subtract, op1=ALU.mult),
         reads=[t2b, B("mv"), B("rstd")], writes=[t2b])
    S.op("pool", lambda e: e.tensor_tensor(out=t2[:], in0=t2[:], in1=lng[:], op=ALU.mult),
         reads=[t2b, S.B("modt", lng.name)], writes=[t2b])
    S.op("dve", lambda e: e.tensor_tensor(out=xo[:], in0=t2[:], in1=lnb[:], op=ALU.add),
         reads=[t2b, S.B("modt", lnb.name)], writes=[xob])
    S.dma("sp", lambda e: e.dma_start(out=dst, in_=xo[:]), reads=[xob], writes=[dstb])


def phase_attn(g, st):
    S = g.S
    nc = g.nc

    def body(ph):
        wqkv = sbt(g, ph, "a_wqkv", [P, KC, 1536], BF16)
        wo = sbt(g, ph, "a_wo", [P, KC, D], BF16)
        ident = sbt(g, ph, "a_ident", [P, P], BF16)
        ones = sbt(g, ph, "a_ones", [P, P], BF16)
        KT = sbt(g, ph, "a_KT", [P, 2, NKC * P], BF16)
        V = sbt(g, ph, "a_V", [P, NKC, 256], BF16)
        shx = sbt(g, ph, "a_shx", [P, D])
        scx = sbt(g, ph, "a_scx", [P, D])
        gate = sbt(g, ph, "a_gate", [P, D])
        lng = sbt(g, ph, "a_lng", [P, D])
        lnb = sbt(g, ph, "a_lnb", [P, D])
        gq = sbt(g, ph, "a_gq", [P, P])
        gk = sbt(g, ph, "a_gk", [P, P])
        xt = sbt(g, ph, "a_xt", [P, 4, D])
        tmp = sbt(g, ph, "a_tmp", [P, D])
        hb = [sbt(g, ph, "a_hb%d" % i, [P, D], BF16) for i in range(2)]
        hT = [sbt(g, ph, "a_hT%d" % i, [P, KC, P], BF16) for i in range(2)]
        ss = sbt(g, ph, "a_ss", [P, 8])
        rstd = sbt(g, ph, "a_rstd", [P, 8])
        qn = sbt(g, ph, "a_qn", [P, 4, P])
        t1 = sbt(g, ph, "a_t1", [P, 4, 64])
        t2_ = sbt(g, ph, "a_t2", [P, 4, 64])
        t3 = sbt(g, ph, "a_t3", [P, 4, 64])
        t4 = sbt(g, ph, "a_t4", [P, 4, 64])
        qr = [sbt(g, ph, "a_qr%d" % i, [P, 8, P], BF16) for i in range(2)]
        cs = [sbt(g, ph, "a_cs%d" % i, [P, 64]) for i in range(2)]
        sn = [sbt(g, ph, "a_sn%d" % i, [P, 64]) for i in range(2)]
        QT = [sbt(g, ph, "a_QT%d" % i, [P, 8, 512], BF16) for i in range(2)]
        pT = [sbt(g, ph, "a_pT%d" % i, [P, 512], BF16) for i in range(3)]
        rz = sbt(g, ph, "a_rz", [P, 512])
        OT = sbt(g, ph, "a_OT", [P, 8, 512], BF16)
        y1 = sbt(g, ph, "a_y1", [P, D])
        stats = sbt(g, ph, "a_stats", [P, 2, 6])
        mv = sbt(g, ph, "a_mv", [P, 2])
        lrs = sbt(g, ph, "a_lrs", [P, 1])
        xo = [sbt(g, ph, "a_xo%d" % i, [P, D]) for i in range(1)]
        ps_s = [pst(g, ph, "a_ps_s%d" % i, [P, 512]) for i in range(2)]
        ps_o = pst(g, ph, "a_ps_o", [P, 512])
        ps_z = pst(g, ph, "a_ps_z", [P, 512])
        ps_tp = pst(g, ph, "a_ps_tp", [P, KC, P], BF16)
        ps_p = [pst(g, ph, "a_ps_p%d" % i, [P, 512]) for i in range(2)]
        ps_tq = pst(g, ph, "a_ps_tq", [P, 8, P], BF16)

        S.dma("pool", lambda e: e.dma_start(out=wqkv[:], in_=g.w_qkv.rearrange("(k p) n -> p k n", p=P)),
              writes=[S.B("wqkv")])
        S.dma("pool", lambda e: e.dma_start(out=wo[:], in_=g.w_o.rearrange("(k p) n -> p k n", p=P)),
              writes=[S.B("wo")])
        S.dma("pool", lambda e: e.dma_start(out=ident[:], in_=g.c_ident[:, :]), writes=[S.B("ident")])
        S.op("dve", lambda e: e.memset(ones[:], 1.0), writes=[S.B("ones")])
        load_mod(g, S, shx, 0, 0)
        load_mod(g, S, scx, 0, 1)
        shc, scc = gate, lng
        load_mod(g, S, shc, 1, 0)
        load_mod(g, S, scc, 1, 1)
        load_bcast(g, S, lnb, g.ln_b[0, 0:1, :])
        gqb = load_bcast(g, S, gq, g.qn[0:1, :])
        gkb = load_bcast(g, S, gk, g.kn[0:1, :])
        S.op("dve", lambda e: e.tensor_scalar(out=gq[:], in0=gq[:], scalar1=float(P) ** -0.5, scalar2=None,
                                              op0=ALU.mult), reads=[gqb], writes=[gqb])

        for i in range(NKC):
            s = i % 2
            is_ctx = i < 2
            src = g.ctx[i * P:(i + 1) * P, :] if is_ctx else g.x[(i - 2) * P:(i - 1) * P, :]
            xb = S.B("xq", s)
            S.dma("sp", lambda e, s=s, src=src: e.dma_start(out=xt[:, s, :], in_=src), writes=[xb])
            if not is_ctx:
                r0 = (i - 2) * P
                S.dma("sp", lambda e, s=s, r0=r0: e.dma_start(out=cs[s][:], in_=g.c_cos[r0:r0 + P, :]),
                      writes=[S.B("cs", s)])
                S.dma("sp", lambda e, s=s, r0=r0: e.dma_start(out=sn[s][:], in_=g.c_sin[r0:r0 + P, :]),
                      writes=[S.B("cs", s)])
            modulate_transpose(g, S, xt[:, s, :], xb, scc if is_ctx else scx, shc if is_ctx else shx,
                               tmp, S.B("tmp"), hb[s], S.B("hb", s), ps_tp, S.B("ps_tp"), hT[s], S.B("hT", s),
                               ident, S.B("ident"))
            pp = s
            for k in range(KC):
                S.op("pe", lambda e, k=k, s=s, pp=pp: e.matmul(ps_p[pp][:], lhsT=hT[s][:, k, :],
                                                               rhs=wqkv[:, k, 1024:1536],
                                                               start=(k == 0), stop=(k == KC - 1)),
                     reads=[S.B("hT", s), S.B("wqkv")], writes=[S.B("ps_p", pp)])
            rms_rope(g, S, "r", ps_p[pp][:, 0:256], S.B("ps_p", pp), 2, gk, gkb, cs[s], sn[s], S.B("cs", s),
                     not is_ctx, y1, ss, rstd, qn, t1, t2_, t3, t4, qr[s], S.B("qr", s, 0))
            for hh in range(2):
                S.op("pe", lambda e, hh=hh, s=s: e.transpose(ps_tq[:, hh, :], qr[s][:, hh, :], ident[:]),
                     reads=[S.B("qr", s, 0), S.B("ident")], writes=[S.B("ps_tq")])
            S.op("act", lambda e, i=i: e.activation(out=KT[:, :, i * P:(i + 1) * P], in_=ps_tq[:, 0:2, :],
                                                    func=AF.Copy),
                 reads=[S.B("ps_tq")], writes=[S.B("KT", i)])
            S.op("act", lambda e, i=i, pp=pp: e.activation(out=V[:, i, :], in_=ps_p[pp][:, 256:512], func=AF.Copy),
                 reads=[S.B("ps_p", pp)], writes=[S.B("V", i)])

        load_mod(g, S, gate, 0, 2)
        load_bcast(g, S, lng, g.ln_g[0, 0:1, :])
        kt_all = [S.B("KT", i) for i in range(NKC)]
        v_all = [S.B("V", i) for i in range(NKC)]
        def q_a1(ti):
            s = ti % 2
            xb = S.B("xq", s)
            S.dma("sp", lambda e, s=s, ti=ti: e.dma_start(out=xt[:, s, :], in_=g.x[ti * P:(ti + 1) * P, :]), writes=[xb])
            S.dma("sp", lambda e, s=s, ti=ti: e.dma_start(out=cs[s][:], in_=g.c_cos[ti * P:(ti + 1) * P, :]),
                  writes=[S.B("cs", s)])
            S.dma("sp", lambda e, s=s, ti=ti: e.dma_start(out=sn[s][:], in_=g.c_sin[ti * P:(ti + 1) * P, :]),
                  writes=[S.B("cs", s)])
            S.op("dve", lambda e, s=s: e.tensor_tensor(out=tmp[:], in0=xt[:, s, :], in1=scx[:], op=ALU.mult),
                 reads=[xb, S.B("modt", scx.name)], writes=[S.B("tmp")])
            S.op("pool", lambda e, s=s: e.tensor_tensor(out=hb[s][:], in0=tmp[:], in1=shx[:], op=ALU.add),
                 reads=[S.B("tmp"), S.B("modt", shx.name)], writes=[S.B("hb", s)])

        def q_a2(ti):
            s = ti % 2
            for k in range(KC):
                S.op("pe", lambda e, k=k, s=s: e.transpose(ps_tp[:, k, :], hb[s][:, k * P:(k + 1) * P], ident[:]),
                     reads=[S.B("hb", s), S.B("ident")], writes=[S.B("ps_tp")])
            S.op("act", lambda e, s=s: e.activation(out=hT[s][:], in_=ps_tp[:], func=AF.Copy),
                 reads=[S.B("ps_tp")], writes=[S.B("hT", s)])

        def q_b(ti):
            s = ti % 2
            for n in range(2):
                for k in range(KC):
                    S.op("pe", lambda e, k=k, s=s, n=n: e.matmul(ps_p[n][:], lhsT=hT[s][:, k, :],
                                                                 rhs=wqkv[:, k, n * 512:(n + 1) * 512],
                                                                 start=(k == 0), stop=(k == KC - 1)),
                         reads=[S.B("hT", s), S.B("wqkv")], writes=[S.B("ps_p", n)])
            for n in range(2):
                rms_rope(g, S, "r", ps_p[n][:, :], S.B("ps_p", n), 4, gq, gqb, cs[s], sn[s],
                         S.B("cs", s), True, y1, ss, rstd, qn, t1, t2_, t3, t4,
                         qr[s][:, n * 4:(n + 1) * 4, :], S.B("qr", s, n))

        def q_c(ti):
            s = ti % 2
            xs_ = (ti // 4) % 2
            j = ti % 4
            for hh in range(8):
                S.op("pe", lambda e, hh=hh, s=s: e.transpose(ps_tq[:, hh, :], qr[s][:, hh, :], ident[:]),
                     reads=[S.B("qr", s, hh // 4), S.B("ident")], writes=[S.B("ps_tq")])
            S.op("act", lambda e, xs_=xs_, j=j: e.activation(out=QT[xs_][:, :, j * P:(j + 1) * P], in_=ps_tq[:],
                                                             func=AF.Copy),
                 reads=[S.B("ps_tq")], writes=[S.B("QT", xs_)])

        for ti in range(4):
            q_a1(ti)
            q_a2(ti)
            q_b(ti)
            q_c(ti)
        NQB = SEQ // 512
        for qb in range(NQB):
            xs = qb % 2
            for h in range(8):
                kv = h // 4
                seq = []
                for kc in range(NKC):
                    seq.append(kc)
                def issue_s(kc, h=h, kv=kv, xs=xs):
                    b = kc % 2
                    S.op("pe", lambda e, kc=kc, b=b: e.matmul(ps_s[b][:], lhsT=KT[:, kv, kc * P:(kc + 1) * P],
                                                             rhs=QT[xs][:, h, :], start=True, stop=True),
                         reads=[kt_all[kc], S.B("QT", xs)], writes=[S.B("ps_s", b)])
                    pb = kc % 3
                    S.op("act", lambda e, b=b, pb=pb: e.activation(out=pT[pb][:], in_=ps_s[b][:], func=AF.Exp),
                         reads=[S.B("ps_s", b)], writes=[S.B("pT", pb)])

                def issue_pv(kc, h=h, kv=kv):
                    pb = kc % 3
                    S.op("pe", lambda e, kc=kc, pb=pb: e.matmul(ps_o[:], lhsT=V[:, kc, kv * P:(kv + 1) * P],
                                                               rhs=pT[pb][:], start=(kc == 0),
                                                               stop=(kc == NKC - 1)),
                         reads=[v_all[kc], S.B("pT", pb)], writes=[S.B("ps_o")])
                    S.op("pe", lambda e, kc=kc, pb=pb: e.matmul(ps_z[:], lhsT=ones[:], rhs=pT[pb][:],
                                                               start=(kc == 0), stop=(kc == NKC - 1)),
                         reads=[S.B("ones"), S.B("pT", pb)], writes=[S.B("ps_z")])
                issue_s(0)
                nt = (qb + 1) * 4 + h // 2
                for kc in range(NKC):
                    if kc + 1 < NKC:
                        issue_s(kc + 1)
                    issue_pv(kc)
                    if qb + 1 < NQB:
                        if h % 2 == 0 and kc == 2:
                            q_a1(nt)
                        elif h % 2 == 0 and kc == 12:
                            q_a2(nt)
                        elif h % 2 == 0 and kc == 20:
                            q_b(nt)
                        elif h % 2 == 1 and kc == 10:
                            q_c(nt)
                S.op("dve", lambda e: e.reciprocal(out=rz[:], in_=ps_z[:]), reads=[S.B("ps_z")], writes=[S.B("rz")])
                S.op("dve", lambda e, h=h: e.tensor_tensor(out=OT[:, h, :], in0=ps_o[:], in1=rz[:], op=ALU.mult),
                     reads=[S.B("ps_o"), S.B("rz")], writes=[S.B("OT", h)])
            for j in range(4):
                ti = qb * 4 + j
                o = 0
                for n in range(2):
                    for h in range(8):
                        S.op("pe", lambda e, h=h, n=n, j=j: e.matmul(ps_p[n][:], lhsT=OT[:, h, j * P:(j + 1) * P],
                                                                     rhs=wo[:, h, n * 512:(n + 1) * 512],
                                                                     start=(h == 0), stop=(h == 7)),
                             reads=[S.B("OT", h), S.B("wo")], writes=[S.B("ps_p", n)])
                    S.op("dve", lambda e, n=n: e.tensor_tensor(out=y1[:, n * 512:(n + 1) * 512], in0=ps_p[n][:],
                                                               in1=gate[:, n * 512:(n + 1) * 512], op=ALU.mult),
                         reads=[S.B("ps_p", n), S.B("modt", gate.name)], writes=[S.B("y1")])
                rs_ = 2 + ti % 2
                S.dma("sp", lambda e, rs_=rs_, ti=ti: e.dma_start(out=xt[:, rs_, :], in_=g.x[ti * P:(ti + 1) * P, :]),
                      writes=[S.B("xq", rs_)])
                S.op("dve", lambda e, rs_=rs_: e.scalar_tensor_tensor(
                    out=y1[:], in0=xt[:, rs_, :], scalar=DN_ALPHA, in1=y1[:], op0=ALU.mult, op1=ALU.add),
                    reads=[S.B("xq", rs_), S.B("y1")], writes=[S.B("y1")])
                layer_norm_store(g, S, "lna", y1, S.B("y1"), lng, lnb, stats, mv, lrs, xo[o], S.B("xo", o),
                                 g.X1[ti * P:(ti + 1) * P, :], S.B("X1", ti))

    run_phase(g, body)


def phase_moe(g, st, layer, XIN, XOUT):
    S = g.S
    nc = g.nc
    midx = 0 if layer == 0 else 2
    g.sfx = "_L%d" % layer
    YEv = g.YE[0:NROW, :].rearrange("(p j) d -> p j d", j=512)

    with contextlib.ExitStack() as outer:
        dest_all = sbt(g, outer, "m_dest", [P, NT, 8], I32)
        slot_all = sbt(g, outer, "m_slot", [P, NT, 8], I32)
        gate_all = sbt(g, outer, "m_gate", [P, NT, 8])

        def body_r(ph):
            wr = sbt(g, ph, "r_wr", [P, KC, NE], BF16)
            wsgu = sbt(g, ph, "r_wsgu", [P, KC, 512], BF16)
            wsdn = sbt(g, ph, "r_wsdn", [P, 2, D], BF16)
            rb = sbt(g, ph, "r_rb", [P, NE])
            iotaE = sbt(g, ph, "r_iota", [P, NE])
            iotaB = sbt(g, ph, "r_iotaB", [P, NBLK])
            pidx = sbt(g, ph, "r_pidx", [P, 1])
            identf = sbt(g, ph, "r_identf", [P, P])
            triu = sbt(g, ph, "r_triu", [P, P], BF16)
            ones = sbt(g, ph, "r_ones", [P, P], BF16)
            ident = sbt(g, ph, "r_ident", [P, P], BF16)
            tokid = sbt(g, ph, "r_tokid", [P, NT, 2], I32)
            sh3 = sbt(g, ph, "r_sh3", [P, D])
            sc4 = sbt(g, ph, "r_sc4", [P, D])
            cum = sbt(g, ph, "r_cum", [P, NE], BF16)
            tinit = sbt(g, ph, "r_tinit", [P, 1024], I32)
            zrow = sbt(g, ph, "r_zrow", [1, D], BF16)
            xt = [sbt(g, ph, "r_xt%d" % i, [P, D]) for i in range(2)]
            tmp = sbt(g, ph, "r_tmp", [P, D])
            hb = [sbt(g, ph, "r_hb%d" % i, [P, D], BF16) for i in range(2)]
            hT = [sbt(g, ph, "r_hT%d" % i, [P, KC, P], BF16) for i in range(2)]
            sgt = sbt(g, ph, "r_sgt", [P, 2, P])
            actT = sbt(g, ph, "r_actT", [P, 2, P], BF16)
            sho = [sbt(g, ph, "r_sho%d" % i, [P, D]) for i in range(2)]
            sc = sbt(g, ph, "r_sc", [P, NE])
            bi = sbt(g, ph, "r_bi", [P, NE])
            m8 = sbt(g, ph, "r_m8", [P, 8, 8])
            gs = sbt(g, ph, "r_gs", [P, 8])
            gm8 = sbt(g, ph, "r_gm8", [P, 8])
            gmask = sbt(g, ph, "r_gmask", [P, 8])
            mk = sbt(g, ph, "r_mk", [P, NE])
            v8 = sbt(g, ph, "r_v8", [P, 8])
            selb = sbt(g, ph, "r_selb", [P, NE], BF16)
            Gu = sbt(g, ph, "r_Gu", [P, NE])
            G = sbt(g, ph, "r_G", [P, NE])
            den = sbt(g, ph, "r_den", [P, 1])
            e8 = sbt(g, ph, "r_e8", [P, 8], U32)
            eall = sbt(g, ph, "r_eall", [P, NT, 8])
            posk = sbt(g, ph, "r_posk", [P, NT, 8])
            psk = sbt(g, ph, "r_psk", [P, NT, 8])
            junk = sbt(g, ph, "r_junk", [P, NE])
            cnt = sbt(g, ph, "r_cnt", [P, NE])
            cnti = sbt(g, ph, "r_cnti", [P, NE], I32)
            pc = sbt(g, ph, "r_pc", [P, NE])
            onesf = sbt(g, ph, "r_onesf", [P, NE])
            pend = sbt(g, ph, "r_pend", [P, NE])
            pstart = sbt(g, ph, "r_pstart", [P, NE])
            pendT = sbt(g, ph, "r_pendT", [P, 2])
            cmpb = sbt(g, ph, "r_cmpb", [P, 2, NBLK], BF16)
            blkf = sbt(g, ph, "r_blkf", [P, NBLK])
            wif = sbt(g, ph, "r_wif", [P, NBLK])
            wi_all = sbt(g, ph, "r_wi", [P, 3, NBLK], I32)
            pcT = sbt(g, ph, "r_pcT", [P, 2])
            pcb = sbt(g, ph, "r_pcb", [P, 2, P], BF16)
            ucum = sbt(g, ph, "r_ucum", [P, 2, NE], BF16)
            bigv = sbt(g, ph, "r_bigv", [P, 2])
            phf = sbt(g, ph, "r_phf", [P, NT * 8])
            lof = sbt(g, ph, "r_lof", [P, NT * 8])
            hif = sbt(g, ph, "r_hif", [P, NT * 8])
            ps_tp = pst(g, ph, "r_ps_tp", [P, KC, P], BF16)
            ps_r = pst(g, ph, "r_ps_r", [P, 512])
            ps_g = pst(g, ph, "r_ps_g", [P, 4, P])
            ps_d = [pst(g, ph, "r_ps_d%d" % i, [P, 512]) for i in range(2)]
            ps_pos = pst(g, ph, "r_ps_pos", [P, 512])

            S.dma("pool", lambda e: e.dma_start(out=wr[:], in_=g.router_w[layer].rearrange("(k p) n -> p k n", p=P)),
                  writes=[S.B("wr")])
            S.dma("pool", lambda e: e.dma_start(out=wsgu[:], in_=g.sgu[layer].rearrange("(k p) n -> p k n", p=P)),
                  writes=[S.B("wsgu")])
            S.dma("pool", lambda e: e.dma_start(out=wsdn[:], in_=g.sdn[layer].rearrange("(k p) n -> p k n", p=P)),
                  writes=[S.B("wsdn")])
            S.dma("pool", lambda e: e.dma_start(out=triu[:], in_=g.c_triu[:, :]), writes=[S.B("triu")])
            S.dma("pool", lambda e: e.dma_start(out=ident[:], in_=g.c_ident[:, :]), writes=[S.B("ident")])
            S.dma("sp", lambda e: e.dma_start(out=identf[:], in_=g.c_ident[:, :]), writes=[S.B("identf")])
            S.dma("sp", lambda e: e.dma_start(out=iotaE[:], in_=g.c_iota[:, :]), writes=[S.B("iotaE")])
            S.dma("sp", lambda e: e.dma_start(out=iotaB[:], in_=g.c_iotab[:, 0:NBLK]), writes=[S.B("iotaB")])
            S.dma("sp", lambda e: e.dma_start(out=pidx[:], in_=g.c_pidx[:, :]), writes=[S.B("pidx")])
            S.dma("sp", lambda e: e.dma_start(out=bigv[:], in_=g.c_bigv[:, :]), writes=[S.B("bigv")])
            S.dma("pool", lambda e: e.dma_start(out=ucum[:], in_=g.c_ucum.rearrange("p (c n) -> p c n", c=2)),
                  writes=[S.B("ucum")])
            S.dma("sp", lambda e: e.dma_start(out=tokid[:], in_=g.c_tokid.rearrange("p (t two) -> p t two", two=2)),
                  writes=[S.B("tokid")])
            load_bcast(g, S, rb, g.router_b[layer:layer + 1, :])
            load_mod(g, S, sh3, midx, 3)
            load_mod(g, S, sc4, midx, 4)
            S.op("dve", lambda e: e.memset(ones[:], 1.0), writes=[S.B("ones")])
            S.op("dve", lambda e: e.memset(onesf[:], 1.0), writes=[S.B("onesf")])
            S.op("dve", lambda e: e.memset(cum[:], 0.0), writes=[S.B("cum")])
            S.op("dve", lambda e: e.memset(zrow[:], 0.0), writes=[S.B("zrow")])
            S.op("pool", lambda e: e.iota(tinit[:], [[0, 1024]], base=(1 << 20), channel_multiplier=0), writes=[S.B("tinit")])
            S.dma("sp", lambda e: e.dma_start(out=g.TBL[0:NROW, :].rearrange("(p r) two -> p (r two)", p=P),
                                              in_=tinit[:]), reads=[S.B("tinit")], writes=[S.B("TBLinit")])
            S.dma("sp", lambda e: e.dma_start(out=g.HB[SEQ:SEQ + 1, :], in_=zrow[:]), reads=[S.B("zrow")])

            for i in range(NT):
                s = i % 2
                xb = S.B("xt", s)
                S.dma("sp", lambda e, s=s, i=i: e.dma_start(out=xt[s][:], in_=XIN[i * P:(i + 1) * P, :]), writes=[xb])
                modulate_transpose(g, S, xt[s][:], xb, sc4, sh3, tmp, S.B("tmp"), hb[s], S.B("hb", s),
                                   ps_tp, S.B("ps_tp"), hT[s], S.B("hT", s), ident, S.B("ident"))
                S.dma("sp", lambda e, s=s, i=i: e.dma_start(out=g.HB[i * P:(i + 1) * P, :], in_=hb[s][:]),
                      reads=[S.B("hb", s)])
                for k in range(KC):
                    S.op("pe", lambda e, k=k, s=s: e.matmul(ps_r[:, 0:NE], lhsT=hT[s][:, k, :], rhs=wr[:, k, :],
                                                            start=(k == 0), stop=(k == KC - 1)),
                         reads=[S.B("hT", s), S.B("wr")], writes=[S.B("ps_r")])
                for m in range(4):
                    for k in range(KC):
                        S.op("pe", lambda e, k=k, s=s, m=m: e.matmul(ps_g[:, m, :], lhsT=wsgu[:, k, m * P:(m + 1) * P],
                                                                     rhs=hT[s][:, k, :], start=(k == 0),
                                                                     stop=(k == KC - 1)),
                             reads=[S.B("hT", s), S.B("wsgu")], writes=[S.B("ps_g")])
                S.op("act", lambda e: e.activation(out=sgt[:], in_=ps_g[:, 0:2, :], func=AF.Silu),
                     reads=[S.B("ps_g")], writes=[S.B("sgt")])
                S.op("dve", lambda e: e.tensor_tensor(out=actT[:], in0=sgt[:], in1=ps_g[:, 2:4, :], op=ALU.mult),
                     reads=[S.B("sgt"), S.B("ps_g")], writes=[S.B("actT")])
                for n in range(2):
                    for j in range(2):
                        S.op("pe", lambda e, n=n, j=j: e.matmul(ps_d[n][:], lhsT=actT[:, j, :],
                                                                rhs=wsdn[:, j, n * 512:(n + 1) * 512],
                                                                start=(j == 0), stop=(j == 1)),
                             reads=[S.B("actT"), S.B("wsdn")], writes=[S.B("ps_d", n)])
                    S.op("act", lambda e, n=n, s=s: e.activation(out=sho[s][:, n * 512:(n + 1) * 512], in_=ps_d[n][:],
                                                                 func=AF.Copy),
                         reads=[S.B("ps_d", n)], writes=[S.B("sho", s)])
                S.dma("sp", lambda e, s=s, i=i: e.dma_start(out=g.SH[i * P:(i + 1) * P, :], in_=sho[s][:]),
                      reads=[S.B("sho", s)])
                S.op("act", lambda e: e.activation(out=sc[:], in_=ps_r[:, 0:NE], func=AF.Sigmoid),
                     reads=[S.B("ps_r")], writes=[S.B("sc")])
                S.op("dve", lambda e: e.tensor_tensor(out=bi[:], in0=sc[:], in1=rb[:], op=ALU.add),
                     reads=[S.B("sc"), S.B("modt", rb.name)], writes=[S.B("bi")])
                for gi in range(8):
                    S.op("dve", lambda e, gi=gi: e.max(out=m8[:, gi, :], in_=bi[:, gi * 32:(gi + 1) * 32]),
                         reads=[S.B("bi")], writes=[S.B("m8")])
                S.op("dve", lambda e: e.tensor_tensor(out=gs[:], in0=m8[:, :, 0], in1=m8[:, :, 1], op=ALU.add),
                     reads=[S.B("m8")], writes=[S.B("gs")])
                S.op("dve", lambda e: e.max(out=gm8[:], in_=gs[:]), reads=[S.B("gs")], writes=[S.B("gm8")])
                S.op("dve", lambda e: e.tensor_scalar(out=gmask[:], in0=gs[:], scalar1=gm8[:, 3:4], scalar2=None,
                                                      op0=ALU.is_ge), reads=[S.B("gs"), S.B("gm8")], writes=[S.B("gmask")])
                S.op("dve", lambda e: e.scalar_tensor_tensor(
                    out=mk[:].rearrange("p (a b) -> p a b", b=32), in0=bi[:].rearrange("p (a b) -> p a b", b=32),
                    scalar=2.0, in1=gmask[:].unsqueeze(2).to_broadcast([P, 8, 32]), op0=ALU.add, op1=ALU.mult),
                    reads=[S.B("bi"), S.B("gmask")], writes=[S.B("mk")])
                S.op("dve", lambda e: e.max(out=v8[:], in_=mk[:]), reads=[S.B("mk")], writes=[S.B("v8")])
                S.op("dve", lambda e: e.max_index(out=e8[:], in_max=v8[:], in_values=mk[:]),
                     reads=[S.B("mk"), S.B("v8")], writes=[S.B("e8")])
                S.op("dve", lambda e, i=i: e.tensor_copy(out=eall[:, i, :], in_=e8[:]), reads=[S.B("e8")],
                     writes=[S.B("eall", i)])
                S.op("dve", lambda e: e.tensor_scalar(out=selb[:], in0=mk[:], scalar1=v8[:, 7:8], scalar2=None,
                                                      op0=ALU.is_ge), reads=[S.B("mk"), S.B("v8")], writes=[S.B("selb")])
                S.op("dve", lambda e: e.scalar_tensor_tensor(out=Gu[:], in0=sc[:], scalar=1.0, in1=selb[:],
                                                             op0=ALU.mult, op1=ALU.mult, accum_out=den[:]),
                     reads=[S.B("sc"), S.B("selb")], writes=[S.B("Gu"), S.B("den")])
                S.op("dve", lambda e: e.reciprocal(out=den[:], in_=den[:]), reads=[S.B("den")], writes=[S.B("den")])
                S.op("dve", lambda e: e.tensor_scalar(out=G[:], in0=Gu[:], scalar1=den[:, 0:1], scalar2=2.5,
                                                      op0=ALU.mult, op1=ALU.mult),
                     reads=[S.B("Gu"), S.B("den")], writes=[S.B("G")])
                S.op("pe", lambda e: e.matmul(ps_pos[:, 0:NE], lhsT=triu[:], rhs=selb[:], start=True, stop=False),
                     reads=[S.B("triu"), S.B("selb")], writes=[S.B("ps_pos")])
                S.op("pe", lambda e: e.matmul(ps_pos[:, 0:NE], lhsT=ones[:], rhs=cum[:], start=False, stop=True),
                     reads=[S.B("ones"), S.B("cum")], writes=[S.B("ps_pos")])
                S.op("dve", lambda e: e.tensor_tensor(out=cum[:], in0=cum[:], in1=selb[:], op=ALU.add),
                     reads=[S.B("cum"), S.B("selb")], writes=[S.B("cum")])
                for k in range(8):
                    S.op("dve", lambda e, i=i, k=k: e.scalar_tensor_tensor(
                        out=junk[:], in0=iotaE[:], scalar=eall[:, i, k:k + 1], in1=G[:], op0=ALU.is_equal, op1=ALU.mult,
                        accum_out=gate_all[:, i, k:k + 1]),
                        reads=[S.B("iotaE"), S.B("eall", i), S.B("G")], writes=[S.B("junk"), S.B("gate", i)])
                    S.op("dve", lambda e, i=i, k=k: e.scalar_tensor_tensor(
                        out=junk[:], in0=iotaE[:], scalar=eall[:, i, k:k + 1], in1=ps_pos[:, 0:NE], op0=ALU.is_equal,
                        op1=ALU.mult, accum_out=posk[:, i, k:k + 1]),
                        reads=[S.B("iotaE"), S.B("eall", i), S.B("ps_pos")], writes=[S.B("junk"), S.B("posk", i)])

            S.op("pe", lambda e: e.matmul(ps_pos[:, 0:NE], lhsT=ones[:], rhs=cum[:], start=True, stop=True),
                 reads=[S.B("ones"), S.B("cum")], writes=[S.B("ps_pos")])
            S.op("dve", lambda e: e.tensor_copy(out=cnt[:], in_=ps_pos[:, 0:NE]), reads=[S.B("ps_pos")], writes=[S.B("cnt")])
            S.op("dve", lambda e: e.memset(pc[:], 0.0), writes=[S.B("pc")])
            for j in range(32):
                S.op("dve", lambda e, j=j: e.scalar_tensor_tensor(out=pc[:], in0=cnt[:], scalar=float(P * j), in1=pc[:],
                                                                  op0=ALU.is_gt, op1=ALU.add),
                     reads=[S.B("cnt"), S.B("pc")], writes=[S.B("pc")])
            S.op("dve", lambda e: e.tensor_scalar(out=pc[:], in0=pc[:], scalar1=float(P), scalar2=None, op0=ALU.mult),
                 reads=[S.B("pc")], writes=[S.B("pc")])
            for c in range(2):
                S.op("dve", lambda e, c=c: e.scalar_tensor_tensor(
                    out=junk[:, 0:P], in0=pc[:, c * P:(c + 1) * P], scalar=1.0, in1=identf[:], op0=ALU.mult,
                    op1=ALU.mult, accum_out=pcT[:, c:c + 1]),
                    reads=[S.B("pc"), S.B("identf")], writes=[S.B("junk"), S.B("pcT")])
            for c in range(2):
                S.op("dve", lambda e, c=c: e.tensor_copy(out=pcb[:, c, :], in_=pcT[:, c:c + 1].to_broadcast([P, P])),
                     reads=[S.B("pcT")], writes=[S.B("pcb")])
            for c in range(2):
                S.op("pe", lambda e, c=c: e.matmul(ps_pos[:, 0:NE], lhsT=pcb[:, c, :], rhs=ucum[:, c, :],
                                                   start=(c == 0), stop=(c == 1)),
                     reads=[S.B("pcb"), S.B("ucum")], writes=[S.B("ps_pos")])
            S.op("dve", lambda e: e.tensor_copy(out=pend[:], in_=ps_pos[:, 0:NE]), reads=[S.B("ps_pos")],
                 writes=[S.B("pend")])
            S.op("dve", lambda e: e.tensor_tensor(out=pstart[:], in0=pend[:], in1=pc[:], op=ALU.subtract),
                 reads=[S.B("pend"), S.B("pc")], writes=[S.B("pstart")])
            for c in range(2):
                S.op("dve", lambda e, c=c: e.scalar_tensor_tensor(
                    out=junk[:, 0:P], in0=pend[:, c * P:(c + 1) * P], scalar=1.0, in1=identf[:], op0=ALU.mult,
                    op1=ALU.mult, accum_out=pendT[:, c:c + 1]),
                    reads=[S.B("pend"), S.B("identf")], writes=[S.B("junk"), S.B("pendT")])
            S.op("dve", lambda e: e.tensor_tensor(out=pendT[:], in0=pendT[:], in1=bigv[:], op=ALU.add),
                 reads=[S.B("pendT"), S.B("bigv")], writes=[S.B("pendT")])
            for c in range(2):
                S.op("dve", lambda e, c=c: e.tensor_scalar(out=cmpb[:, c, :], in0=iotaB[:], scalar1=pendT[:, c:c + 1],
                                                           scalar2=None, op0=ALU.is_ge),
                     reads=[S.B("iotaB"), S.B("pendT")], writes=[S.B("cmpb")])
            for c in range(2):
                S.op("pe", lambda e, c=c: e.matmul(ps_r[:, 0:NBLK], lhsT=ones[:], rhs=cmpb[:, c, :], start=(c == 0),
                                                   stop=(c == 1)),
                     reads=[S.B("ones"), S.B("cmpb")], writes=[S.B("ps_r")])
            S.op("dve", lambda e: e.tensor_copy(out=blkf[:], in_=ps_r[:, 0:NBLK]), reads=[S.B("ps_r")],
                 writes=[S.B("blkf")])
            S.op("dve", lambda e: e.tensor_scalar(out=wif[:], in0=blkf[:], scalar1=float(P) if g.netab == NE else 0.0, scalar2=pidx[:, 0:1],
                                                  op0=ALU.mult, op1=ALU.add),
                 reads=[S.B("blkf"), S.B("pidx")], writes=[S.B("wif")])
            S.op("dve", lambda e: e.tensor_scalar(out=blkf[:], in0=iotaB[:], scalar1=pend[:, NE - 1:NE], scalar2=1.0e6,
                                                  op0=ALU.is_ge, op1=ALU.mult),
                 reads=[S.B("iotaB"), S.B("pend"), S.B("wif")], writes=[S.B("blkf")])
            S.op("dve", lambda e: e.tensor_tensor(out=wif[:], in0=wif[:], in1=blkf[:], op=ALU.add),
                 reads=[S.B("wif"), S.B("blkf")], writes=[S.B("wif")])
            S.op("dve", lambda e: e.tensor_copy(out=wi_all[:, 2, :], in_=wif[:]), reads=[S.B("wif")], writes=[S.B("wi")])
            S.op("dve", lambda e: e.tensor_scalar(out=wi_all[:, 0, :], in0=wif[:], scalar1=2.0, scalar2=None,
                                                  op0=ALU.mult), reads=[S.B("wif")], writes=[S.B("wi")])
            S.op("dve", lambda e: e.tensor_scalar(out=wi_all[:, 1, :], in0=wif[:], scalar1=2.0, scalar2=1.0,
                                                  op0=ALU.mult, op1=ALU.add), reads=[S.B("wif")], writes=[S.B("wi")])
            S.dma("sp", lambda e: e.dma_start(out=g.WI[:, :], in_=wi_all[:].rearrange("p a b -> p (a b)")),
                  reads=[S.B("wi")])
            for i in range(NT):
                for k in range(8):
                    S.op("dve", lambda e, i=i, k=k: e.scalar_tensor_tensor(
                        out=junk[:], in0=iotaE[:], scalar=eall[:, i, k:k + 1], in1=pstart[:], op0=ALU.is_equal,
                        op1=ALU.mult, accum_out=psk[:, i, k:k + 1]),
                        reads=[S.B("iotaE"), S.B("eall", i), S.B("pstart")], writes=[S.B("junk"), S.B("psk")])
            pskb = [S.B("psk")] + [S.B("posk", i) for i in range(NT)]
            posf = posk[:].rearrange("p a b -> p (a b)")
            pskf = psk[:].rearrange("p a b -> p (a b)")
            S.op("dve", lambda e: e.memset(phf[:], 0.0), writes=[S.B("phf")])
            for j in range(1, 32):
                S.op("dve", lambda e, j=j: e.scalar_tensor_tensor(out=phf[:], in0=posf, scalar=float(P * j), in1=phf[:],
                                                                  op0=ALU.is_ge, op1=ALU.add),
                     reads=pskb + [S.B("phf")], writes=[S.B("phf")])
            S.op("dve", lambda e: e.scalar_tensor_tensor(out=lof[:], in0=phf[:], scalar=-float(P), in1=posf,
                                                         op0=ALU.mult, op1=ALU.add),
                 reads=pskb + [S.B("phf")], writes=[S.B("lof")])
            S.op("dve", lambda e: e.scalar_tensor_tensor(out=hif[:], in0=pskf, scalar=1.0 / P, in1=phf[:],
                                                         op0=ALU.mult, op1=ALU.add),
                 reads=pskb + [S.B("phf")], writes=[S.B("hif")])
            S.op("dve", lambda e: e.scalar_tensor_tensor(out=dest_all[:].rearrange("p a b -> p (a b)"), in0=lof[:],
                                                         scalar=512.0, in1=hif[:], op0=ALU.mult, op1=ALU.add),
                 reads=[S.B("lof"), S.B("hif")], writes=[S.B("dest")])
            S.op("dve", lambda e: e.tensor_tensor(out=slot_all[:].rearrange("p a b -> p (a b)"), in0=pskf, in1=posf,
                                                  op=ALU.add),
                 reads=pskb, writes=[S.B("slotn")])
            if g.debug and g.debug.endswith("_r"):
                dbg = sbt(g, ph, "r_dbg", [P, 4096])
                S.op("dve", lambda e: e.memset(dbg[:], 0.0), writes=[S.B("dbg")])
                items = [(cnt[:], 256), (pc[:], 256), (pend[:], 256), (pstart[:], 256), (blkf[:], 512),
                         (psk[:].rearrange("p a b -> p (a b)"), 256), (posk[:].rearrange("p a b -> p (a b)"), 256),
                         (eall[:].rearrange("p a b -> p (a b)"), 256), (dest_all[:].rearrange("p a b -> p (a b)"), 256),
                         (gate_all[:].rearrange("p a b -> p (a b)"), 256), (phf[:], 256), (pendT[:], 2)]
                off = 0
                allb = [S.B(n) for n in ("cnt", "pc", "pend", "pstart", "blkf", "psk", "dest", "phf", "pendT")]
                allb += [S.B("posk", i) for i in range(NT)] + [S.B("eall", i) for i in range(NT)] + [S.B("gate", i) for i in range(NT)]
                for ap, n in items:
                    S.op("dve", lambda e, ap=ap, n=n, off=off: e.tensor_copy(out=dbg[:, off:off + n], in_=ap),
                         reads=allb, writes=[S.B("dbg")])
                    off += n
                S.dma("sp", lambda e: e.dma_start(out=g.DBG[:, :], in_=dbg[:]), reads=[S.B("dbg")])
            for i in range(NT):
                for k in range(8):
                    S.dma("pool", lambda e, i=i, k=k: e.indirect_dma_start(
                        out=g.TBL[:, :], out_offset=bass.IndirectOffsetOnAxis(ap=dest_all[:, i, k:k + 1], axis=0),
                        in_=tokid[:, i, :], in_offset=None, bounds_check=S.reg(e, NROW - 1), oob_is_err=False),
                        reads=[S.B("dest"), S.B("tokid"), S.B("TBLinit")])

        run_phase(g, body_r)
        if g.debug == 'moe%d_r' % layer:
            return

        def body_e(ph):
            ident = sbt(g, ph, "e_ident", [P, P], BF16)
            idx2 = sbt(g, ph, "e_idx", [P, 512, 2], I32)
            wi = sbt(g, ph, "e_wi", [P, 3, NBLK], I32)
            NS = 4
            wf = [sbt(g, ph, "e_wf%d" % i, [P, 6144]) for i in range(NS)]
            wgu = [sbt(g, ph, "e_wgu%d" % i, [P, KC, 512], BF16) for i in range(NS)]
            wdn = [sbt(g, ph, "e_wdn%d" % i, [P, 2, D], BF16) for i in range(NS)]
            xg = [sbt(g, ph, "e_xg%d" % i, [P, D], BF16) for i in range(NS)]
            xT = [sbt(g, ph, "e_xT%d" % i, [P, KC, P], BF16) for i in range(2)]
            sgt = [sbt(g, ph, "e_sgt%d" % i, [P, 2, P]) for i in range(2)]
            actT = [sbt(g, ph, "e_actT%d" % i, [P, 2, P], BF16) for i in range(2)]
            ysb = [sbt(g, ph, "e_ysb%d" % i, [P, D], BF16) for i in range(2)]
            ps_t = [pst(g, ph, "e_ps_t%d" % i, [P, KC, P], BF16) for i in range(2)]
            ps_a = [pst(g, ph, "e_ps_a%d" % i, [P, 4, P]) for i in range(2)]
            ps_y = [pst(g, ph, "e_ps_y%d" % i, [P, 512]) for i in range(4)]

            S.dma("pool", lambda e: e.dma_start(out=ident[:], in_=g.c_ident[:, :]), writes=[S.B("ident")])
            S.dma("sp", lambda e: e.dma_start(out=idx2[:], in_=g.TBL[0:NROW, :].rearrange("(p j) two -> p j two", j=512)),
                  writes=[S.B("idx2")])
            S.dma("sp", lambda e: e.dma_start(out=wi[:].rearrange("p a b -> p (a b)"), in_=g.WI[:, :]),
                  writes=[S.B("wi")])
            for i_ in range(NS):
                S.op("dve", lambda e, i_=i_: e.memset(xg[i_][:], 0.0), writes=[S.B("xg", i_)])

            def fetch(b):
                s = b % NS
                S.dma("pool", lambda e, s=s, b=b: e.indirect_dma_start(
                    out=wf[s][:], out_offset=None, in_=g.ew[layer][:, :],
                    in_offset=bass.IndirectOffsetOnAxis(ap=wi[:, 2, b:b + 1], axis=0),
                    bounds_check=S.reg(e, g.netab * P - 1), oob_is_err=False),
                    reads=[S.B("wi")], writes=[S.B("wf", s)])
                S.dma("pool", lambda e, s=s, b=b: e.indirect_dma_start(
                    out=xg[s][:], out_offset=None, in_=g.HB[:, :],
                    in_offset=bass.IndirectOffsetOnAxis(ap=idx2[:, b, 0:1], axis=0),
                    bounds_check=S.reg(e, SEQ - 1), oob_is_err=False),
                    reads=[S.B("idx2")], writes=[S.B("xg", s)])

            def cast_w(b):
                s = b % NS
                s2 = b % 2
                wgv = wf[s][:, 0:4096].rearrange("p (k n) -> p k n", n=512)
                wdv = wf[s][:, 4096:6144].rearrange("p (k n) -> p k n", n=D)
                S.op("act", lambda e, s=s, wgv=wgv: e.activation(out=wgu[s][:, 0:5, :], in_=wgv[:, 0:5, :], func=AF.Copy),
                     reads=[S.B("wf", s)], writes=[S.B("wgu", s, 0)])
                S.op("dve", lambda e, s=s, wgv=wgv: e.tensor_copy(out=wgu[s][:, 5:8, :], in_=wgv[:, 5:8, :]),
                     reads=[S.B("wf", s)], writes=[S.B("wgu", s, 1)])
                S.op("dve", lambda e, s=s, wdv=wdv: e.tensor_copy(out=wdn[s][:], in_=wdv),
                     reads=[S.B("wf", s)], writes=[S.B("wdn", s)])

            def stage_a(b):
                s = b % NS
                s2 = b % 2
                cast_w(b)
                for k in range(KC):
                    S.op("pe", lambda e, s=s, s2=s2, k=k: e.transpose(ps_t[s2][:, k, :], xg[s][:, k * P:(k + 1) * P],
                                                                      ident[:]),
                         reads=[S.B("xg", s), S.B("ident")], writes=[S.B("ps_t", s2)])
                S.op("act", lambda e, s2=s2: e.activation(out=xT[s2][:], in_=ps_t[s2][:], func=AF.Copy),
                     reads=[S.B("ps_t", s2)], writes=[S.B("xT", s2)])

            def stage_b(b):
                s = b % NS
                s2 = b % 2
                wb = [S.B("wgu", s, 0), S.B("wgu", s, 1)]
                for m in range(4):
                    for k in range(KC):
                        S.op("pe", lambda e, s=s, s2=s2, m=m, k=k: e.matmul(
                            ps_a[s2][:, m, :], lhsT=wgu[s][:, k, m * P:(m + 1) * P], rhs=xT[s2][:, k, :],
                            start=(k == 0), stop=(k == KC - 1)),
                            reads=[S.B("xT", s2)] + wb, writes=[S.B("ps_a", s2)])
                S.op("act", lambda e, s2=s2: e.activation(out=sgt[s2][:], in_=ps_a[s2][:, 0:2, :], func=AF.Silu),
                     reads=[S.B("ps_a", s2)], writes=[S.B("sgt", s2)])
                S.op("dve", lambda e, s2=s2: e.tensor_tensor(out=actT[s2][:], in0=sgt[s2][:], in1=ps_a[s2][:, 2:4, :],
                                                             op=ALU.mult),
                     reads=[S.B("sgt", s2), S.B("ps_a", s2)], writes=[S.B("actT", s2)])

            def stage_c(b):
                s = b % NS
                s2 = b % 2
                for n in range(2):
                    pi = s2 * 2 + n
                    for j in range(2):
                        S.op("pe", lambda e, s=s, s2=s2, n=n, j=j, pi=pi: e.matmul(
                            ps_y[pi][:], lhsT=actT[s2][:, j, :], rhs=wdn[s][:, j, n * 512:(n + 1) * 512],
                            start=(j == 0), stop=(j == 1)),
                            reads=[S.B("actT", s2), S.B("wdn", s)], writes=[S.B("ps_y", pi)])
                    if n == 0:
                        S.op("act", lambda e, s2=s2, n=n, pi=pi: e.activation(
                            out=ysb[s2][:, n * 512:(n + 1) * 512], in_=ps_y[pi][:], func=AF.Copy),
                            reads=[S.B("ps_y", pi)], writes=[S.B("ysb", s2)])
                    else:
                        S.op("dve", lambda e, s2=s2, n=n, pi=pi: e.tensor_copy(
                            out=ysb[s2][:, n * 512:(n + 1) * 512], in_=ps_y[pi][:]),
                            reads=[S.B("ps_y", pi)], writes=[S.B("ysb", s2)])
                S.dma("sp", lambda e, s2=s2, b=b: e.dma_start(out=g.YE[b * P:(b + 1) * P, :], in_=ysb[s2][:]),
                      reads=[S.B("ysb", s2)])

            fetch(0)
            fetch(1)
            fetch(2)
            for it in range(NBLK + 2):
                if it + 3 < NBLK:
                    fetch(it + 3)
                if it < NBLK:
                    stage_a(it)
                if 0 <= it - 1 < NBLK:
                    stage_b(it - 1)
                if 0 <= it - 2 < NBLK:
                    stage_c(it - 2)

        run_phase(g, body_e)
        if g.debug == 'moe%d_e' % layer:
            return

        def body_c(ph):
            gate5 = sbt(g, ph, "c_gate5", [P, D])
            lng = sbt(g, ph, "c_lng", [P, D])
            lnb = sbt(g, ph, "c_lnb", [P, D])
            xt = [sbt(g, ph, "c_xt%d" % i, [P, D]) for i in range(2)]
            acc = [sbt(g, ph, "c_acc%d" % i, [P, D]) for i in range(2)]
            R = [sbt(g, ph, "c_R%d" % i, [P, 8, D], BF16) for i in range(2)]
            stats = sbt(g, ph, "c_stats", [P, 2, 6])
            mv = sbt(g, ph, "c_mv", [P, 2])
            lrs = sbt(g, ph, "c_lrs", [P, 1])
            xo = [sbt(g, ph, "c_xo%d" % i, [P, D]) for i in range(2)]
            load_mod(g, S, gate5, midx, 5)
            load_bcast(g, S, lng, g.ln_g[layer, 1:2, :])
            load_bcast(g, S, lnb, g.ln_b[layer, 1:2, :])
            for i in range(NT):
                s = i % 2
                S.dma("sp", lambda e, s=s, i=i: e.dma_start(out=xt[s][:], in_=XIN[i * P:(i + 1) * P, :]),
                      writes=[S.B("xt", s)])
                S.dma("sp", lambda e, s=s, i=i: e.dma_start(out=acc[s][:], in_=g.SH[i * P:(i + 1) * P, :]),
                      writes=[S.B("acc", s)])
                for k in range(8):
                    S.dma("pool", lambda e, s=s, i=i, k=k: e.indirect_dma_start(
                        out=R[s][:, k, :], out_offset=None, in_=g.YE[:, :],
                        in_offset=bass.IndirectOffsetOnAxis(ap=slot_all[:, i, k:k + 1], axis=0),
                        bounds_check=S.reg(e, NROW - 1), oob_is_err=False),
                        writes=[S.B("R", s, k)])
                for k in range(8):
                    S.op("dve", lambda e, s=s, i=i, k=k: e.scalar_tensor_tensor(
                        out=acc[s][:], in0=R[s][:, k, :], scalar=gate_all[:, i, k:k + 1], in1=acc[s][:],
                        op0=ALU.mult, op1=ALU.add),
                        reads=[S.B("R", s, k), S.B("acc", s)], writes=[S.B("acc", s)])
                S.op("pool", lambda e, s=s: e.tensor_tensor(out=acc[s][:], in0=acc[s][:], in1=gate5[:], op=ALU.mult),
                     reads=[S.B("acc", s), S.B("modt", gate5.name)], writes=[S.B("acc", s)])
                S.op("dve", lambda e, s=s: e.scalar_tensor_tensor(out=acc[s][:], in0=xt[s][:], scalar=DN_ALPHA,
                                                                  in1=acc[s][:], op0=ALU.mult, op1=ALU.add),
                     reads=[S.B("xt", s), S.B("acc", s)], writes=[S.B("acc", s)])
                layer_norm_store(g, S, "lnc", acc[s], S.B("acc", s), lng, lnb, stats, mv, lrs, xo[s], S.B("xo", s),
                                 XOUT[i * P:(i + 1) * P, :], S.B("XOUT", i))

        run_phase(g, body_c)


def phase_conv(g, st):
    S = g.S
    g.sfx = ""

    def body1(ph):
        win = sbt(g, ph, "v_win", [P, KC, 3 * D], BF16)
        ident = sbt(g, ph, "v_ident", [P, P], BF16)
        shx = sbt(g, ph, "v_shx", [P, D])
        scx = sbt(g, ph, "v_scx", [P, D])
        zrow = sbt(g, ph, "v_zrow", [1, D])
        xt = [sbt(g, ph, "v_xt%d" % i, [P, D]) for i in range(2)]
        tmp = sbt(g, ph, "v_tmp", [P, D])
        hb = [sbt(g, ph, "v_hb%d" % i, [P, D], BF16) for i in range(2)]
        hT = [sbt(g, ph, "v_hT%d" % i, [P, KC, P], BF16) for i in range(2)]
        bgt = [sbt(g, ph, "v_bgt%d" % i, [P, D]) for i in range(2)]
        vt = sbt(g, ph, "v_vt", [P, D])
        ut = [sbt(g, ph, "v_ut%d" % i, [P, D]) for i in range(2)]
        ps_tp = pst(g, ph, "v_ps_tp", [P, KC, P], BF16)
        ps = [pst(g, ph, "v_ps%d" % i, [P, 512]) for i in range(6)]
        for c in range(2):
            S.dma("pool", lambda e, c=c: e.dma_start(
                out=win[:, :, c * 1536:(c + 1) * 1536],
                in_=g.w_in[:, c * 1536:(c + 1) * 1536].rearrange("(k p) n -> p k n", p=P)), writes=[S.B("win", c)])
        S.dma("pool", lambda e: e.dma_start(out=ident[:], in_=g.c_ident[:, :]), writes=[S.B("ident")])
        load_mod(g, S, shx, 2, 0)
        load_mod(g, S, scx, 2, 1)
        S.op("dve", lambda e: e.memset(zrow[:], 0.0), writes=[S.B("zrow")])
        S.dma("sp", lambda e: e.dma_start(out=g.UU[0:1, :], in_=zrow[:]), reads=[S.B("zrow")])
        S.dma("sp", lambda e: e.dma_start(out=g.UU[SEQ + 1:SEQ + 2, :], in_=zrow[:]), reads=[S.B("zrow")])
        wb = [S.B("win", 0), S.B("win", 1)]
        for i in range(NT):
            s = i % 2
            xb = S.B("xt", s)
            S.dma("sp", lambda e, s=s, i=i: e.dma_start(out=xt[s][:], in_=g.X2[i * P:(i + 1) * P, :]), writes=[xb])
            modulate_transpose(g, S, xt[s][:], xb, scx, shx, tmp, S.B("tmp"), hb[s], S.B("hb", s),
                               ps_tp, S.B("ps_tp"), hT[s], S.B("hT", s), ident, S.B("ident"))
            for n in range(6):
                for k in range(KC):
                    S.op("pe", lambda e, s=s, n=n, k=k: e.matmul(ps[n][:], lhsT=hT[s][:, k, :],
                                                                 rhs=win[:, k, n * 512:(n + 1) * 512],
                                                                 start=(k == 0), stop=(k == KC - 1)),
                         reads=[S.B("hT", s)] + wb, writes=[S.B("ps", n)])
            for n in range(2):
                S.op("act", lambda e, s=s, n=n: e.activation(out=bgt[s][:, n * 512:(n + 1) * 512], in_=ps[n][:],
                                                             func=AF.Copy),
                     reads=[S.B("ps", n)], writes=[S.B("bgt", s)])
                S.op("act", lambda e, n=n: e.activation(out=vt[:, n * 512:(n + 1) * 512], in_=ps[4 + n][:],
                                                        func=AF.Copy),
                     reads=[S.B("ps", 4 + n)], writes=[S.B("vt")])
                S.op("dve", lambda e, s=s, n=n: e.tensor_tensor(out=ut[s][:, n * 512:(n + 1) * 512], in0=ps[2 + n][:],
                                                                in1=vt[:, n * 512:(n + 1) * 512], op=ALU.mult),
                     reads=[S.B("ps", 2 + n), S.B("vt")], writes=[S.B("ut", s)])
            S.dma("sp", lambda e, s=s, i=i: e.dma_start(out=g.BG[i * P:(i + 1) * P, :], in_=bgt[s][:]),
                  reads=[S.B("bgt", s)])
            S.dma("sp", lambda e, s=s, i=i: e.dma_start(out=g.UU[1 + i * P:1 + (i + 1) * P, :], in_=ut[s][:]),
                  reads=[S.B("ut", s)])

    run_phase(g, body1)

    def body2(ph):
        wout = sbt(g, ph, "w_wout", [P, KC, D], BF16)
        ident = sbt(g, ph, "w_ident", [P, P], BF16)
        tp = [sbt(g, ph, "w_tap%d" % i, [P, D]) for i in range(3)]
        gate = sbt(g, ph, "w_gate", [P, D])
        lng = sbt(g, ph, "w_lng", [P, D])
        lnb = sbt(g, ph, "w_lnb", [P, D])
        xt = [sbt(g, ph, "w_xt%d" % i, [P, D]) for i in range(2)]
        up = [sbt(g, ph, "w_up%d" % i, [P, D]) for i in range(2)]
        uc = [sbt(g, ph, "w_uc%d" % i, [P, D]) for i in range(2)]
        un = [sbt(g, ph, "w_un%d" % i, [P, D]) for i in range(2)]
        bgt = [sbt(g, ph, "w_bgt%d" % i, [P, D]) for i in range(2)]
        zb = sbt(g, ph, "w_zb", [P, D], BF16)
        zT = sbt(g, ph, "w_zT", [P, KC, P], BF16)
        y1 = sbt(g, ph, "w_y1", [P, D])
        stats = sbt(g, ph, "w_stats", [P, 2, 6])
        mv = sbt(g, ph, "w_mv", [P, 2])
        lrs = sbt(g, ph, "w_lrs", [P, 1])
        xo = [sbt(g, ph, "w_xo%d" % i, [P, D]) for i in range(2)]
        ps_tp = pst(g, ph, "w_ps_tp", [P, KC, P], BF16)
        ps_p = [pst(g, ph, "w_ps_p%d" % i, [P, 512]) for i in range(2)]
        S.dma("pool", lambda e: e.dma_start(out=wout[:], in_=g.w_out.rearrange("(k p) n -> p k n", p=P)),
              writes=[S.B("wout")])
        S.dma("pool", lambda e: e.dma_start(out=ident[:], in_=g.c_ident[:, :]), writes=[S.B("ident")])
        for j in range(3):
            load_bcast(g, S, tp[j], g.taps[j:j + 1, :])
        load_mod(g, S, gate, 2, 2)
        load_bcast(g, S, lng, g.ln_g[1, 0:1, :])
        load_bcast(g, S, lnb, g.ln_b[1, 0:1, :])
        for i in range(NT):
            s = i % 2
            S.dma("sp", lambda e, s=s, i=i: e.dma_start(out=xt[s][:], in_=g.X2[i * P:(i + 1) * P, :]),
                  writes=[S.B("xt", s)])
            S.dma("sp", lambda e, s=s, i=i: e.dma_start(out=up[s][:], in_=g.UU[i * P:(i + 1) * P, :]),
                  writes=[S.B("up", s)])
            S.dma("sp", lambda e, s=s, i=i: e.dma_start(out=uc[s][:], in_=g.UU[1 + i * P:1 + (i + 1) * P, :]),
                  writes=[S.B("uc", s)])
            S.dma("sp", lambda e, s=s, i=i: e.dma_start(out=un[s][:], in_=g.UU[2 + i * P:2 + (i + 1) * P, :]),
                  writes=[S.B("un", s)])
            S.dma("sp", lambda e, s=s, i=i: e.dma_start(out=bgt[s][:], in_=g.BG[i * P:(i + 1) * P, :]),
                  writes=[S.B("bgt", s)])
            S.op("dve", lambda e, s=s: e.tensor_tensor(out=up[s][:], in0=up[s][:], in1=tp[0][:], op=ALU.mult),
                 reads=[S.B("up", s), S.B("modt", tp[0].name)], writes=[S.B("up", s)])
            S.op("pool", lambda e, s=s: e.tensor_tensor(out=uc[s][:], in0=uc[s][:], in1=tp[1][:], op=ALU.mult),
                 reads=[S.B("uc", s), S.B("modt", tp[1].name)], writes=[S.B("uc", s)])
            S.op("pool", lambda e, s=s: e.tensor_tensor(out=un[s][:], in0=un[s][:], in1=tp[2][:], op=ALU.mult),
                 reads=[S.B("un", s), S.B("modt", tp[2].name)], writes=[S.B("un", s)])
            S.op("dve", lambda e, s=s: e.tensor_tensor(out=up[s][:], in0=up[s][:], in1=uc[s][:], op=ALU.add),
                 reads=[S.B("up", s), S.B("uc", s)], writes=[S.B("up", s)])
            S.op("dve", lambda e, s=s: e.tensor_tensor(out=up[s][:], in0=up[s][:], in1=un[s][:], op=ALU.add),
                 reads=[S.B("up", s), S.B("un", s)], writes=[S.B("up", s)])
            S.op("dve", lambda e, s=s: e.tensor_tensor(out=zb[:], in0=up[s][:], in1=bgt[s][:], op=ALU.mult),
                 reads=[S.B("up", s), S.B("bgt", s)], writes=[S.B("zb")])
            for k in range(KC):
                S.op("pe", lambda e, k=k: e.transpose(ps_tp[:, k, :], zb[:, k * P:(k + 1) * P], ident[:]),
                     reads=[S.B("zb"), S.B("ident")], writes=[S.B("ps_tp")])
            S.op("act", lambda e: e.activation(out=zT[:], in_=ps_tp[:], func=AF.Copy),
                 reads=[S.B("ps_tp")], writes=[S.B("zT")])
            for n in range(2):
                for k in range(KC):
                    S.op("pe", lambda e, n=n, k=k: e.matmul(ps_p[n][:], lhsT=zT[:, k, :],
                                                            rhs=wout[:, k, n * 512:(n + 1) * 512],
                                                            start=(k == 0), stop=(k == KC - 1)),
                         reads=[S.B("zT"), S.B("wout")], writes=[S.B("ps_p", n)])
                S.op("dve", lambda e, n=n: e.tensor_tensor(out=y1[:, n * 512:(n + 1) * 512], in0=ps_p[n][:],
                                                           in1=gate[:, n * 512:(n + 1) * 512], op=ALU.mult),
                     reads=[S.B("ps_p", n), S.B("modt", gate.name)], writes=[S.B("y1")])
            S.op("dve", lambda e, s=s: e.scalar_tensor_tensor(out=y1[:], in0=xt[s][:], scalar=DN_ALPHA, in1=y1[:],
                                                              op0=ALU.mult, op1=ALU.add),
                 reads=[S.B("xt", s), S.B("y1")], writes=[S.B("y1")])
            layer_norm_store(g, S, "lnv", y1, S.B("y1"), lng, lnb, stats, mv, lrs, xo[s], S.B("xo", s),
                             g.X3[i * P:(i + 1) * P, :], S.B("X3", i))

    run_phase(g, body2)


_CACHE = {}


def _consts():
    ident = np.eye(P, dtype=np.float32)
    t = np.arange(SEQ)
    row = (t // 64).astype(np.float32)
    col = (t % 64).astype(np.float32)
    freqs = (np.float32(10000.0) ** (-np.arange(0, 64, 2, dtype=np.float32) / np.float32(64))).astype(np.float32)
    ang = np.concatenate([row[:, None] * freqs[None, :], col[:, None] * freqs[None, :]], axis=-1).astype(np.float32)
    cos = np.cos(ang).astype(np.float32)
    sin = np.sin(ang).astype(np.float32)
    iota = np.tile(np.arange(NE, dtype=np.float32)[None, :], (P, 1))
    cbase = np.zeros((P, NE), dtype=np.float32)
    iotab = np.zeros((P, NBLK + 1), dtype=np.float32)
    iotab[:, :NBLK] = (np.arange(NBLK) * P)[None, :]
    iotab[:, NBLK] = np.arange(P)
    triu = np.triu(np.ones((P, P), dtype=np.float32), k=1)
    tokid = np.zeros((P, NT, 2), dtype=np.int32)
    tokid[:, :, 0] = np.arange(NT)[None, :] * P + np.arange(P)[:, None]
    tokid = tokid.reshape(P, NT * 2)
    pidx = np.arange(P, dtype=np.float32).reshape(P, 1)
    bigv = np.zeros((P, 2), dtype=np.float32)
    bigv[P - 1, 1] = 1e9
    ee = np.arange(NE)
    ucum = np.zeros((P, 2, NE), dtype=np.float32)
    for c in range(2):
        ucum[:, c, :] = ((c * P + np.arange(P))[:, None] <= ee[None, :]).astype(np.float32)
    ucum = ucum.reshape(P, 2 * NE)
    return dict(c_pidx=pidx, c_bigv=bigv, c_ucum=ucum, c_ident=ident, c_cos=cos, c_sin=sin, c_iota=iota, c_cbase=cbase, c_triu=triu, c_tokid=tokid, c_iotab=iotab)


def _relayout(w, nk, lite):
    w = np.asarray(w, dtype=np.float32)
    if lite:
        w = w[:, 0:1]
    L, E, R, N = w.shape
    return np.ascontiguousarray(w.reshape(L, E, nk, P, N).transpose(0, 1, 3, 2, 4))


def _merge_w(inputs, l, lite):
    a = _relayout(inputs["exp_w_gate_up"][l:l + 1], KC, lite).reshape(-1, 4096)
    b = _relayout(inputs["exp_w_down"][l:l + 1], 2, lite).reshape(-1, 2048)
    return np.ascontiguousarray(np.concatenate([a, b], axis=1))


def make_in_maps(inputs, lite=False, cores=range(8)):
    f = lambda a: np.ascontiguousarray(np.asarray(a, dtype=np.float32))
    shared = dict(
        ada_w=f(inputs["ada_w"]), ada_b=f(inputs["ada_b"]), ln_g=f(inputs["ln_g"]), ln_b=f(inputs["ln_b"]),
        w_qkv=f(inputs["attn_w_qkv"][0]), qn=f(inputs["attn_q_norm"][0]).reshape(1, P),
        kn=f(inputs["attn_k_norm"][0]).reshape(1, P), w_o=f(inputs["attn_w_o"][0]),
        w_in=f(inputs["conv_w_in"][0]), taps=f(inputs["conv_taps"][0]), w_out=f(inputs["conv_w_out"][0]),
        router_w=f(inputs["router_w"]), router_b=f(inputs["router_bias"]),
        ew0=_merge_w(inputs, 0, lite), ew1=_merge_w(inputs, 1, lite),
        sgu=f(inputs["shared_w_gate_up"]), sdn=f(inputs["shared_w_down"]),
    )
    shared.update(_consts())
    ccT = f(np.asarray(inputs["c_ctx"]).reshape(KC, P).T)
    maps = []
    for b in cores:
        m = dict(shared)
        m["x"] = f(inputs["x"][b])
        m["ctx"] = f(inputs["ctx"][b])
        m["cT"] = f(np.asarray(inputs["c"][b]).reshape(KC, P).T)
        m["ccT"] = ccT
        maps.append(m)
    return maps


def kernel(**inputs):
    if "nc" not in _CACHE:
        _CACHE["nc"] = build_program()
    nc = _CACHE["nc"]
    maps = make_in_maps(inputs)
    res = run_bass_kernel_spmd(nc, maps, core_ids=list(range(8)))
    return np.stack([np.asarray(r["y"], dtype=np.float32) for r in res.results], axis=0)
```

```python
import contextlib
import numpy as np
import concourse.bass as bass
import concourse.mybir as mybir
from concourse.bass_utils import run_bass_kernel_spmd

F32 = mybir.dt.float32
BF16 = mybir.dt.bfloat16
I32 = mybir.dt.int32
U32 = mybir.dt.uint32
AF = mybir.ActivationFunctionType
ALU = mybir.AluOpType
AX = mybir.AxisListType

P = 128
D = 1024
KC = 8
SEQ = 4096
CTX = 256
NT = SEQ // P
NKC = (SEQ + CTX) // P
NE = 256
NBLK = 512
NROW = NBLK * P
DN_ALPHA = 4.0 ** 0.25
LN_EPS = 1e-5
QK_EPS = 1e-6
SAME_SYNC = True


class Buf:
    __slots__ = ("lw", "rd")

    def __init__(self):
        self.lw = None
        self.rd = {}


class Sched:
    COMPUTE = ("pe", "act", "dve", "pool")

    def __init__(self, nc, st, nds=20):
        self.nc = nc
        self.names = ["pe", "act", "dve", "pool", "sp"]
        self.csem = {n: st.enter_context(nc.semaphore("c_" + n)) for n in self.names}
        self.cnt = {n: 0 for n in self.names}
        self.dsem = [st.enter_context(nc.semaphore("d%d" % i)) for i in range(nds)]
        self.dcnt = [0] * nds
        self.dnext = 0
        self.prog = {n: [] for n in self.names}
        self.known = {n: {} for n in self.names}
        self.bufs = {}
        self.ninstr = 0

    def B(self, *key):
        b = self.bufs.get(key)
        if b is None:
            b = self.bufs[key] = Buf()
        return b

    def _semobj(self, key):
        return self.csem[key] if isinstance(key, str) else self.dsem[key[1]]

    def _wait(self, eng, ev):
        key, val = ev
        if val <= 0 or self.known[eng].get(key, 0) >= val:
            return
        self.known[eng][key] = val
        sem = self._semobj(key)
        self.prog[eng].append(lambda e, sem=sem, val=val: e.wait_ge(sem, val))
        self.ninstr += 1

    def _sync(self, eng, reads, writes):
        evs = []
        for b in reads:
            if b.lw is not None:
                evs.append((b.lw, True))
        for b in writes:
            if b.lw is not None:
                evs.append((b.lw, True))
            for k, v in b.rd.items():
                evs.append(((k, v), False))
        for ev, strong in evs:
            if ev[0] == eng:
                if eng == "pe" or not (SAME_SYNC and strong):
                    continue
            self._wait(eng, ev)

    def _mark(self, ev, reads, writes):
        for b in reads:
            if b.rd.get(ev[0], 0) < ev[1]:
                b.rd[ev[0]] = ev[1]
        for b in writes:
            b.lw = ev
            b.rd = {}

    def op(self, eng, fn, reads=(), writes=()):
        self._sync(eng, reads, writes)
        self.cnt[eng] += 1
        ev = (eng, self.cnt[eng])
        sem = self.csem[eng]
        self.prog[eng].append(lambda e, fn=fn, sem=sem: fn(e).then_inc(sem, 1))
        self.ninstr += 1
        self._mark(ev, reads, writes)

    def dma(self, q, fn, reads=(), writes=()):
        self._sync(q, reads, writes)
        j = self.dnext
        self.dnext = (j + 1) % len(self.dsem)
        self._wait(q, (("d", j), self.dcnt[j]))
        self.dcnt[j] += 16
        ev = (("d", j), self.dcnt[j])
        sem = self.dsem[j]
        self.prog[q].append(lambda e, fn=fn, sem=sem: fn(e).then_inc(sem, 16))
        self.ninstr += 1
        self._mark(ev, reads, writes)

    def barrier(self):
        evs = [(n, self.cnt[n]) for n in self.COMPUTE]
        evs += [(("d", j), c) for j, c in enumerate(self.dcnt)]
        for n in self.names:
            for ev in evs:
                if ev[0] != n:
                    self._wait(n, ev)
        self.bufs = {}

    def reg(self, e, val):
        r = self.regcache.get(val)
        if r is None:
            r = self.regcache[val] = e.to_reg(val)
        return r

    def emit(self):
        self.regcache = {}
        with self.nc.Block() as block:
            for n, deco in (("pe", block.tensor), ("act", block.scalar), ("dve", block.vector),
                            ("pool", block.gpsimd), ("sp", block.sync)):
                prog = self.prog[n]

                def body(e, prog=prog):
                    for f in prog:
                        f(e)
                deco(body)
                self.prog[n] = []


class Ctx:
    pass


def build_program(debug=None, debug_outs=(), lite=False):
    nc = bass.Bass("TRN2", target_bir_lowering=False)
    g = Ctx()
    g.nc = nc
    g.debug = debug
    g.sfx = ""
    g.netab = 1 if lite else NE

    def din(name, shape, dt=F32):
        return nc.dram_tensor(name, list(shape), dt, kind="ExternalInput").ap()

    def dscr(name, shape, dt=F32):
        if debug_outs and name in debug_outs:
            return nc.dram_tensor(name, list(shape), dt, kind="ExternalOutput").ap()
        return nc.dram_tensor(name, list(shape), dt).ap()

    g.x = din("x", [SEQ, D])
    g.ctx = din("ctx", [CTX, D])
    g.cT = din("cT", [P, KC])
    g.ccT = din("ccT", [P, KC])
    g.ada_w = din("ada_w", [2, D, 6 * D])
    g.ada_b = din("ada_b", [2, 6 * D])
    g.ln_g = din("ln_g", [2, 2, D])
    g.ln_b = din("ln_b", [2, 2, D])
    g.w_qkv = din("w_qkv", [D, 1536])
    g.qn = din("qn", [1, P])
    g.kn = din("kn", [1, P])
    g.w_o = din("w_o", [D, D])
    g.w_in = din("w_in", [D, 3 * D])
    g.taps = din("taps", [3, D])
    g.w_out = din("w_out", [D, D])
    g.router_w = din("router_w", [2, D, NE])
    g.router_b = din("router_b", [2, NE])
    g.ew = [din("ew%d" % l, [(1 if lite else NE) * P, 6144]) for l in range(2)]
    g.c_iotab = din("c_iotab", [P, NBLK + 1])
    g.c_pidx = din("c_pidx", [P, 1])
    g.c_bigv = din("c_bigv", [P, 2])
    g.c_ucum = din("c_ucum", [P, 2 * NE])
    g.sgu = din("sgu", [2, D, 512])
    g.sdn = din("sdn", [2, 256, D])
    g.c_ident = din("c_ident", [P, P])
    g.c_cos = din("c_cos", [SEQ, 64])
    g.c_sin = din("c_sin", [SEQ, 64])
    g.c_iota = din("c_iota", [P, NE])
    g.c_cbase = din("c_cbase", [P, NE])
    g.c_triu = din("c_triu", [P, P])
    g.c_tokid = din("c_tokid", [P, NT * 2], I32)
    g.y = nc.dram_tensor("y", [SEQ, D], F32, kind="ExternalOutput").ap()

    g.MODD = dscr("MODD", [3, P, 6 * D])
    g.X1 = dscr("X1", [SEQ, D])
    g.X2 = dscr("X2", [SEQ, D])
    g.X3 = dscr("X3", [SEQ, D])
    g.HB = dscr("HB", [SEQ + 1, D], BF16)
    g.SH = dscr("SH", [SEQ, D])
    g.TBL = dscr("TBL", [NROW + 1, 2], I32)
    g.YE = dscr("YE", [NROW + 1, D], BF16)
    g.UU = dscr("UU", [SEQ + 2, D])
    g.BG = dscr("BG", [SEQ, D])
    g.WI = dscr("WI", [P, 3 * NBLK], I32)
    g.DBG = dscr("DBG", [P, 4096])

    with contextlib.ExitStack() as st:
        S = Sched(nc, st)
        g.S = S
        stages = [
            ("adaln", phase_adaln),
            ("attn", phase_attn),
            ("moe0", lambda g_, st_: phase_moe(g_, st_, 0, g.X1, g.X2)),
            ("conv", phase_conv),
            ("moe1", lambda g_, st_: phase_moe(g_, st_, 1, g.X3, g.y)),
        ]
        for name, fn in stages:
            fn(g, st)
            if debug and debug.startswith(name):
                break
        S.barrier()
        S.emit()
    return nc


def run_phase(g, fn):
    S = g.S
    with contextlib.ExitStack() as ph:
        S.barrier()
        fn(ph)
        S.barrier()
        S.emit()


def sbt(g, ph, name, shape, dt=F32):
    return ph.enter_context(g.nc.sbuf_tensor(name + g.sfx, list(shape), dt))


def pst(g, ph, name, shape, dt=F32):
    return ph.enter_context(g.nc.psum_tensor(name + g.sfx, list(shape), dt))


def phase_adaln(g, st):
    S = g.S

    def body(ph):
        sc = sbt(g, ph, "ad_sc", [P, 2, KC])
        scs = sbt(g, ph, "ad_scs", [P, 2, KC])
        scb = sbt(g, ph, "ad_scb", [P, 2, KC, P])
        wt = [sbt(g, ph, "ad_wt%d" % i, [P, KC, 512]) for i in range(2)]
        bt = [sbt(g, ph, "ad_bt%d" % i, [P, 512]) for i in range(2)]
        res = [sbt(g, ph, "ad_res%d" % i, [P, 512]) for i in range(3)]
        ps = [pst(g, ph, "ad_ps%d" % i, [P, 512]) for i in range(2)]
        S.dma("sp", lambda e: e.dma_start(out=sc[:, 0, :], in_=g.cT[:, :]), writes=[S.B("sc")])
        S.dma("sp", lambda e: e.dma_start(out=sc[:, 1, :], in_=g.ccT[:, :]), writes=[S.B("sc")])
        S.op("act", lambda e: e.activation(out=scs[:], in_=sc[:], func=AF.Silu),
             reads=[S.B("sc")], writes=[S.B("scs")])
        S.op("dve", lambda e: e.tensor_copy(out=scb[:], in_=scs[:].unsqueeze(3).to_broadcast([P, 2, KC, P])),
             reads=[S.B("scs")], writes=[S.B("scb")])
        it = 0
        rs = 0
        for layer in range(2):
            kinds = [(0, 0), (1, 1)] if layer == 0 else [(0, 2)]
            for n in range(12):
                s = it % 2
                it += 1
                wsrc = g.ada_w[layer, :, n * 512:(n + 1) * 512].rearrange("(k p) n -> p k n", p=P)
                S.dma("sp", lambda e, s=s, wsrc=wsrc: e.dma_start(out=wt[s][:], in_=wsrc),
                      writes=[S.B("wt", s)])
                bsrc = g.ada_b[layer:layer + 1, n * 512:(n + 1) * 512].to_broadcast([P, 512])
                S.dma("sp", lambda e, s=s, bsrc=bsrc: e.dma_start(out=bt[s][:], in_=bsrc),
                      writes=[S.B("bt", s)])
                for kind, idx in kinds:
                    pp = (it + kind) % 2
                    for k in range(KC):
                        S.op("pe", lambda e, pp=pp, kind=kind, k=k, s=s: e.matmul(
                            ps[pp][:], lhsT=scb[:, kind, k, :], rhs=wt[s][:, k, :],
                            start=(k == 0), stop=(k == KC - 1)),
                            reads=[S.B("scb"), S.B("wt", s)], writes=[S.B("ps", pp)])
                    r = rs % 3
                    rs += 1
                    add1 = 1.0 if n in (2, 3, 8, 9) else 0.0
                    S.op("dve", lambda e, r=r, pp=pp, s=s, add1=add1: e.scalar_tensor_tensor(
                        out=res[r][:], in0=ps[pp][:], scalar=add1, in1=bt[s][:], op0=ALU.add, op1=ALU.add),
                        reads=[S.B("ps", pp), S.B("bt", s)], writes=[S.B("res", r)])
                    dst = g.MODD[idx, :, n * 512:(n + 1) * 512]
                    S.dma("sp", lambda e, r=r, dst=dst: e.dma_start(out=dst, in_=res[r][:]),
                          reads=[S.B("res", r)], writes=[S.B("MODD", idx, n)])

    run_phase(g, body)


def load_mod(g, S, tile, idx, j, q="sp"):
    src = g.MODD[idx, :, j * D:(j + 1) * D]
    S.dma(q, lambda e: e.dma_start(out=tile[:], in_=src), writes=[S.B("modt", tile.name)])
    return S.B("modt", tile.name)


def load_bcast(g, S, tile, src_row, q="sp"):
    n = tile.shape[1]
    src = src_row.to_broadcast([P, n])
    S.dma(q, lambda e: e.dma_start(out=tile[:], in_=src), writes=[S.B("modt", tile.name)])
    return S.B("modt", tile.name)


def modulate_transpose(g, S, xt, xb, sc_t, sh_t, tmp, tmpb, hb, hbb, tp, tpb, hT, hTb, ident, identb, extra_reads=()):
    S.op("dve", lambda e: e.tensor_tensor(out=tmp[:], in0=xt, in1=sc_t[:], op=ALU.mult),
         reads=[xb, S.B("modt", sc_t.name)], writes=[tmpb])
    S.op("pool", lambda e: e.tensor_tensor(out=hb[:], in0=tmp[:], in1=sh_t[:], op=ALU.add),
         reads=[tmpb, S.B("modt", sh_t.name)], writes=[hbb])
    for k in range(KC):
        S.op("pe", lambda e, k=k: e.transpose(tp[:, k, :], hb[:, k * P:(k + 1) * P], ident[:]),
             reads=[hbb, identb], writes=[tpb])
    S.op("act", lambda e: e.activation(out=hT[:], in_=tp[:], func=AF.Copy), reads=[tpb], writes=[hTb])


def rms_rope(g, S, pfx, src_ps, src_b, nh, gain, gainb, cs, sn, csb, do_rope, sq, ss, rstd, qn, t1, t2, t3, t4, out_bf, out_b):
    B = lambda n: S.B("y1") if n == "sq" else S.B(pfx, n)
    S.op("act", lambda e: e.activation(out=sq[:, 0:nh * P], in_=src_ps, func=AF.Square),
         reads=[src_b], writes=[B("sq")])
    S.op("dve", lambda e: e.tensor_reduce(out=ss[:, 0:nh], in_=sq[:, 0:nh * P].rearrange("p (h d) -> p h d", d=P),
                                          axis=AX.X, op=ALU.add), reads=[B("sq")], writes=[B("ss")])
    S.op("dve", lambda e: e.tensor_scalar(out=ss[:, 0:nh], in0=ss[:, 0:nh], scalar1=1.0 / P, scalar2=QK_EPS,
                                          op0=ALU.mult, op1=ALU.add), reads=[B("ss")], writes=[B("ss")])
    S.op("act", lambda e: e.activation(out=ss[:, 0:nh], in_=ss[:, 0:nh], func=AF.Sqrt),
         reads=[B("ss")], writes=[B("ss")])
    S.op("dve", lambda e: e.reciprocal(out=rstd[:, 0:nh], in_=ss[:, 0:nh]), reads=[B("ss")], writes=[B("rstd")])
    S.op("dve", lambda e: e.tensor_tensor(
        out=qn[:, 0:nh, :], in0=src_ps.rearrange("p (h d) -> p h d", d=P),
        in1=rstd[:, 0:nh].unsqueeze(2).to_broadcast([P, nh, P]), op=ALU.mult),
        reads=[src_b, B("rstd")], writes=[B("qn")])
    if not do_rope:
        S.op("dve", lambda e: e.tensor_tensor(
            out=out_bf[:, 0:nh, :], in0=qn[:, 0:nh, :], in1=gain[:].unsqueeze(1).to_broadcast([P, nh, P]),
            op=ALU.mult), reads=[B("qn"), gainb], writes=[out_b])
        return
    S.op("pool", lambda e: e.tensor_tensor(
        out=qn[:, 0:nh, :], in0=qn[:, 0:nh, :], in1=gain[:].unsqueeze(1).to_broadcast([P, nh, P]),
        op=ALU.mult), reads=[B("qn"), gainb], writes=[B("qn")])
    q4 = qn[:, 0:nh, :].rearrange("p h (j two) -> p h j two", two=2)
    x0 = q4[:, :, :, 0]
    x1 = q4[:, :, :, 1]
    o4 = out_bf[:, 0:nh, :].rearrange("p h (j two) -> p h j two", two=2)
    cb = cs[:].unsqueeze(1).to_broadcast([P, nh, 64])
    sb_ = sn[:].unsqueeze(1).to_broadcast([P, nh, 64])
    S.op("dve", lambda e: e.tensor_tensor(out=t1[:, 0:nh, :], in0=x0, in1=cb, op=ALU.mult),
         reads=[B("qn"), csb], writes=[B("t1")])
    S.op("pool", lambda e: e.tensor_tensor(out=t2[:, 0:nh, :], in0=x1, in1=sb_, op=ALU.mult),
         reads=[B("qn"), csb], writes=[B("t2")])
    S.op("dve", lambda e: e.tensor_tensor(out=t3[:, 0:nh, :], in0=x0, in1=sb_, op=ALU.mult),
         reads=[B("qn"), csb], writes=[B("t3")])
    S.op("pool", lambda e: e.tensor_tensor(out=t4[:, 0:nh, :], in0=x1, in1=cb, op=ALU.mult),
         reads=[B("qn"), csb], writes=[B("t4")])
    S.op("dve", lambda e: e.tensor_tensor(out=o4[:, :, :, 0], in0=t1[:, 0:nh, :], in1=t2[:, 0:nh, :], op=ALU.subtract),
         reads=[B("t1"), B("t2")], writes=[out_b])
    S.op("dve", lambda e: e.tensor_tensor(out=o4[:, :, :, 1], in0=t3[:, 0:nh, :], in1=t4[:, 0:nh, :], op=ALU.add),
         reads=[B("t3"), B("t4")], writes=[out_b])


def layer_norm_store(g, S, pfx, t2, t2b, lng, lnb, stats, mv, rstd, xo, xob, dst, dstb):
    B = lambda n: S.B(pfx, n)
    for hh in range(2):
        S.op("dve", lambda e, hh=hh: e.bn_stats(out=stats[:, hh, :], in_=t2[:, hh * 512:(hh + 1) * 512]),
             reads=[t2b], writes=[B("stats")])
    S.op("dve", lambda e: e.bn_aggr(out=mv[:], in_=stats[:].rearrange("p a b -> p (a b)")),
         reads=[B("stats")], writes=[B("mv")])
    S.op("dve", lambda e: e.tensor_scalar(out=rstd[:], in0=mv[:, 1:2], scalar1=LN_EPS, scalar2=None, op0=ALU.add),
         reads=[B("mv")], writes=[B("rstd")])
    S.op("act", lambda e: e.activation(out=rstd[:], in_=rstd[:], func=AF.Sqrt), reads=[B("rstd")], writes=[B("rstd")])
    S.op("dve", lambda e: e.reciprocal(out=rstd[:], in_=rstd[:]), reads=[B("rstd")], writes=[B("rstd")])
    S.op("dve", lambda e: e.tensor_scalar(out=t2[:], in0=t2[:], scalar1=mv[:, 0:1], scalar2=rstd[:, 0:1],
                                          op0=ALU.subtract, op1=ALU.mult),
         reads=[t2b, B("mv"), B("rstd")], writes=[t2b])
    S.op("pool", lambda e: e.tensor_tensor(out=t2[:], in0=t2[:], in1=lng[:], op=ALU.mult),
         reads=[t2b, S.B("modt", lng.name)], writes=[t2b])
    S.op("dve", lambda e: e.tensor_tensor(out=xo[:], in0=t2[:], in1=lnb[:], op=ALU.add),
         reads=[t2b, S.B("modt", lnb.name)], writes=[xob])
    S.dma("sp", lambda e: e.dma_start(out=dst, in_=xo[:]), reads=[xob], writes=[dstb])


def phase_attn(g, st):
    S = g.S
    nc = g.nc

    def body(ph):
        wqkv = sbt(g, ph, "a_wqkv", [P, KC, 1536], BF16)
        wo = sbt(g, ph, "a_wo", [P, KC, D], BF16)
        ident = sbt(g, ph, "a_ident", [P, P], BF16)
        ones = sbt(g, ph, "a_ones", [P, P], BF16)
        KT = sbt(g, ph, "a_KT", [P, 2, NKC * P], BF16)
        V = sbt(g, ph, "a_V", [P, NKC, 256], BF16)
        shx = sbt(g, ph, "a_shx", [P, D])
        scx = sbt(g, ph, "a_scx", [P, D])
        gate = sbt(g, ph, "a_gate", [P, D])
        lng = sbt(g, ph, "a_lng", [P, D])
        lnb = sbt(g, ph, "a_lnb", [P, D])
        gq = sbt(g, ph, "a_gq", [P, P])
        gk = sbt(g, ph, "a_gk", [P, P])
        xt = sbt(g, ph, "a_xt", [P, 4, D])
        tmp = sbt(g, ph, "a_tmp", [P, D])
        hb = [sbt(g, ph, "a_hb%d" % i, [P, D], BF16) for i in range(2)]
        hT = [sbt(g, ph, "a_hT%d" % i, [P, KC, P], BF16) for i in range(2)]
        ss = sbt(g, ph, "a_ss", [P, 8])
        rstd = sbt(g, ph, "a_rstd", [P, 8])
        qn = sbt(g, ph, "a_qn", [P, 4, P])
        t1 = sbt(g, ph, "a_t1", [P, 4, 64])
        t2_ = sbt(g, ph, "a_t2", [P, 4, 64])
        t3 = sbt(g, ph, "a_t3", [P, 4, 64])
        t4 = sbt(g, ph, "a_t4", [P, 4, 64])
        qr = [sbt(g, ph, "a_qr%d" % i, [P, 8, P], BF16) for i in range(2)]
        cs = [sbt(g, ph, "a_cs%d" % i, [P, 64]) for i in range(2)]
        sn = [sbt(g, ph, "a_sn%d" % i, [P, 64]) for i in range(2)]
        QT = [sbt(g, ph, "a_QT%d" % i, [P, 8, 512], BF16) for i in range(2)]
        pT = [sbt(g, ph, "a_pT%d" % i, [P, 512], BF16) for i in range(3)]
        rz = sbt(g, ph, "a_rz", [P, 512])
        OT = sbt(g, ph, "a_OT", [P, 8, 512], BF16)
        y1 = sbt(g, ph, "a_y1", [P, D])
        stats = sbt(g, ph, "a_stats", [P, 2, 6])
        mv = sbt(g, ph, "a_mv", [P, 2])
        lrs = sbt(g, ph, "a_lrs", [P, 1])
        xo = [sbt(g, ph, "a_xo%d" % i, [P, D]) for i in range(1)]
        ps_s = [pst(g, ph, "a_ps_s%d" % i, [P, 512]) for i in range(2)]
        ps_o = pst(g, ph, "a_ps_o", [P, 512])
        ps_z = pst(g, ph, "a_ps_z", [P, 512])
        ps_tp = pst(g, ph, "a_ps_tp", [P, KC, P], BF16)
        ps_p = [pst(g, ph, "a_ps_p%d" % i, [P, 512]) for i in range(2)]
        ps_tq = pst(g, ph, "a_ps_tq", [P, 8, P], BF16)

        S.dma("pool", lambda e: e.dma_start(out=wqkv[:], in_=g.w_qkv.rearrange("(k p) n -> p k n", p=P)),
              writes=[S.B("wqkv")])
        S.dma("pool", lambda e: e.dma_start(out=wo[:], in_=g.w_o.rearrange("(k p) n -> p k n", p=P)),
              writes=[S.B("wo")])
        S.dma("pool", lambda e: e.dma_start(out=ident[:], in_=g.c_ident[:, :]), writes=[S.B("ident")])
        S.op("dve", lambda e: e.memset(ones[:], 1.0), writes=[S.B("ones")])
        load_mod(g, S, shx, 0, 0)
        load_mod(g, S, scx, 0, 1)
        shc, scc = gate, lng
        load_mod(g, S, shc, 1, 0)
        load_mod(g, S, scc, 1, 1)
        load_bcast(g, S, lnb, g.ln_b[0, 0:1, :])
        gqb = load_bcast(g, S, gq, g.qn[0:1, :])
        gkb = load_bcast(g, S, gk, g.kn[0:1, :])
        S.op("dve", lambda e: e.tensor_scalar(out=gq[:], in0=gq[:], scalar1=float(P) ** -0.5, scalar2=None,
                                              op0=ALU.mult), reads=[gqb], writes=[gqb])

        for i in range(NKC):
            s = i % 2
            is_ctx = i < 2
            src = g.ctx[i * P:(i + 1) * P, :] if is_ctx else g.x[(i - 2) * P:(i - 1) * P, :]
            xb = S.B("xq", s)
            S.dma("sp", lambda e, s=s, src=src: e.dma_start(out=xt[:, s, :], in_=src), writes=[xb])
            if not is_ctx:
                r0 = (i - 2) * P
                S.dma("sp", lambda e, s=s, r0=r0: e.dma_start(out=cs[s][:], in_=g.c_cos[r0:r0 + P, :]),
                      writes=[S.B("cs", s)])
                S.dma("sp", lambda e, s=s, r0=r0: e.dma_start(out=sn[s][:], in_=g.c_sin[r0:r0 + P, :]),
                      writes=[S.B("cs", s)])
            modulate_transpose(g, S, xt[:, s, :], xb, scc if is_ctx else scx, shc if is_ctx else shx,
                               tmp, S.B("tmp"), hb[s], S.B("hb", s), ps_tp, S.B("ps_tp"), hT[s], S.B("hT", s),
                               ident, S.B("ident"))
            pp = s
            for k in range(KC):
                S.op("pe", lambda e, k=k, s=s, pp=pp: e.matmul(ps_p[pp][:], lhsT=hT[s][:, k, :],
                                                               rhs=wqkv[:, k, 1024:1536],
                                                               start=(k == 0), stop=(k == KC - 1)),
                     reads=[S.B("hT", s), S.B("wqkv")], writes=[S.B("ps_p", pp)])
            rms_rope(g, S, "r", ps_p[pp][:, 0:256], S.B("ps_p", pp), 2, gk, gkb, cs[s], sn[s], S.B("cs", s),
                     not is_ctx, y1, ss, rstd, qn, t1, t2_, t3, t4, qr[s], S.B("qr", s, 0))
            for hh in range(2):
                S.op("pe", lambda e, hh=hh, s=s: e.transpose(ps_tq[:, hh, :], qr[s][:, hh, :], ident[:]),
                     reads=[S.B("qr", s, 0), S.B("ident")], writes=[S.B("ps_tq")])
            S.op("act", lambda e, i=i: e.activation(out=KT[:, :, i * P:(i + 1) * P], in_=ps_tq[:, 0:2, :],
                                                    func=AF.Copy),
                 reads=[S.B("ps_tq")], writes=[S.B("KT", i)])
            S.op("act", lambda e, i=i, pp=pp: e.activation(out=V[:, i, :], in_=ps_p[pp][:, 256:512], func=AF.Copy),
                 reads=[S.B("ps_p", pp)], writes=[S.B("V", i)])

        load_mod(g, S, gate, 0, 2)
        load_bcast(g, S, lng, g.ln_g[0, 0:1, :])
        kt_all = [S.B("KT", i) for i in range(NKC)]
        v_all = [S.B("V", i) for i in range(NKC)]
        def q_a1(ti):
            s = ti % 2
            xb = S.B("xq", s)
            S.dma("sp", lambda e, s=s, ti=ti: e.dma_start(out=xt[:, s, :], in_=g.x[ti * P:(ti + 1) * P, :]), writes=[xb])
            S.dma("sp", lambda e, s=s, ti=ti: e.dma_start(out=cs[s][:], in_=g.c_cos[ti * P:(ti + 1) * P, :]),
                  writes=[S.B("cs", s)])
            S.dma("sp", lambda e, s=s, ti=ti: e.dma_start(out=sn[s][:], in_=g.c_sin[ti * P:(ti + 1) * P, :]),
                  writes=[S.B("cs", s)])
            S.op("dve", lambda e, s=s: e.tensor_tensor(out=tmp[:], in0=xt[:, s, :], in1=scx[:], op=ALU.mult),
                 reads=[xb, S.B("modt", scx.name)], writes=[S.B("tmp")])
            S.op("pool", lambda e, s=s: e.tensor_tensor(out=hb[s][:], in0=tmp[:], in1=shx[:], op=ALU.add),
                 reads=[S.B("tmp"), S.B("modt", shx.name)], writes=[S.B("hb", s)])

        def q_a2(ti):
            s = ti % 2
            for k in range(KC):
                S.op("pe", lambda e, k=k, s=s: e.transpose(ps_tp[:, k, :], hb[s][:, k * P:(k + 1) * P], ident[:]),
                     reads=[S.B("hb", s), S.B("ident")], writes=[S.B("ps_tp")])
            S.op("act", lambda e, s=s: e.activation(out=hT[s][:], in_=ps_tp[:], func=AF.Copy),
                 reads=[S.B("ps_tp")], writes=[S.B("hT", s)])

        def q_b(ti):
            s = ti % 2
            for n in range(2):
                for k in range(KC):
                    S.op("pe", lambda e, k=k, s=s, n=n: e.matmul(ps_p[n][:], lhsT=hT[s][:, k, :],
                                                                 rhs=wqkv[:, k, n * 512:(n + 1) * 512],
                                                                 start=(k == 0), stop=(k == KC - 1)),
                         reads=[S.B("hT", s), S.B("wqkv")], writes=[S.B("ps_p", n)])
            for n in range(2):
                rms_rope(g, S, "r", ps_p[n][:, :], S.B("ps_p", n), 4, gq, gqb, cs[s], sn[s],
                         S.B("cs", s), True, y1, ss, rstd, qn, t1, t2_, t3, t4,
                         qr[s][:, n * 4:(n + 1) * 4, :], S.B("qr", s, n))

        def q_c(ti):
            s = ti % 2
            xs_ = (ti // 4) % 2
            j = ti % 4
            for hh in range(8):
                S.op("pe", lambda e, hh=hh, s=s: e.transpose(ps_tq[:, hh, :], qr[s][:, hh, :], ident[:]),
                     reads=[S.B("qr", s, hh // 4), S.B("ident")], writes=[S.B("ps_tq")])
            S.op("act", lambda e, xs_=xs_, j=j: e.activation(out=QT[xs_][:, :, j * P:(j + 1) * P], in_=ps_tq[:],
                                                             func=AF.Copy),
                 reads=[S.B("ps_tq")], writes=[S.B("QT", xs_)])

        for ti in range(4):
            q_a1(ti)
            q_a2(ti)
            q_b(ti)
            q_c(ti)
        NQB = SEQ // 512
        for qb in range(NQB):
            xs = qb % 2
            for h in range(8):
                kv = h // 4
                seq = []
                for kc in range(NKC):
                    seq.append(kc)
                def issue_s(kc, h=h, kv=kv, xs=xs):
                    b = kc % 2
                    S.op("pe", lambda e, kc=kc, b=b: e.matmul(ps_s[b][:], lhsT=KT[:, kv, kc * P:(kc + 1) * P],
                                                             rhs=QT[xs][:, h, :], start=True, stop=True),
                         reads=[kt_all[kc], S.B("QT", xs)], writes=[S.B("ps_s", b)])
                    pb = kc % 3
                    S.op("act", lambda e, b=b, pb=pb: e.activation(out=pT[pb][:], in_=ps_s[b][:], func=AF.Exp),
                         reads=[S.B("ps_s", b)], writes=[S.B("pT", pb)])

                def issue_pv(kc, h=h, kv=kv):
                    pb = kc % 3
                    S.op("pe", lambda e, kc=kc, pb=pb: e.matmul(ps_o[:], lhsT=V[:, kc, kv * P:(kv + 1) * P],
                                                               rhs=pT[pb][:], start=(kc == 0),
                                                               stop=(kc == NKC - 1)),
                         reads=[v_all[kc], S.B("pT", pb)], writes=[S.B("ps_o")])
                    S.op("pe", lambda e, kc=kc, pb=pb: e.matmul(ps_z[:], lhsT=ones[:], rhs=pT[pb][:],
                                                               start=(kc == 0), stop=(kc == NKC - 1)),
                         reads=[S.B("ones"), S.B("pT", pb)], writes=[S.B("ps_z")])
                issue_s(0)
                nt = (qb + 1) * 4 + h // 2
                for kc in range(NKC):
                    if kc + 1 < NKC:
                        issue_s(kc + 1)
                    issue_pv(kc)
                    if qb + 1 < NQB:
                        if h % 2 == 0 and kc == 2:
                            q_a1(nt)
                        elif h % 2 == 0 and kc == 12:
                            q_a2(nt)
                        elif h % 2 == 0 and kc == 20:
                            q_b(nt)
                        elif h % 2 == 1 and kc == 10:
                            q_c(nt)
                S.op("dve", lambda e: e.reciprocal(out=rz[:], in_=ps_z[:]), reads=[S.B("ps_z")], writes=[S.B("rz")])
                S.op("dve", lambda e, h=h: e.tensor_tensor(out=OT[:, h, :], in0=ps_o[:], in1=rz[:], op=ALU.mult),
                     reads=[S.B("ps_o"), S.B("rz")], writes=[S.B("OT", h)])
            for j in range(4):
                ti = qb * 4 + j
                o = 0
                for n in range(2):
                    for h in range(8):
                        S.op("pe", lambda e, h=h, n=n, j=j: e.matmul(ps_p[n][:], lhsT=OT[:, h, j * P:(j + 1) * P],
                                                                     rhs=wo[:, h, n * 512:(n + 1) * 512],
                                                                     start=(h == 0), stop=(h == 7)),
                             reads=[S.B("OT", h), S.B("wo")], writes=[S.B("ps_p", n)])
                    S.op("dve", lambda e, n=n: e.tensor_tensor(out=y1[:, n * 512:(n + 1) * 512], in0=ps_p[n][:],
                                                               in1=gate[:, n * 512:(n + 1) * 512], op=ALU.mult),
                         reads=[S.B("ps_p", n), S.B("modt", gate.name)], writes=[S.B("y1")])
                rs_ = 2 + ti % 2
                S.dma("sp", lambda e, rs_=rs_, ti=ti: e.dma_start(out=xt[:, rs_, :], in_=g.x[ti * P:(ti + 1) * P, :]),
                      writes=[S.B("xq", rs_)])
                S.op("dve", lambda e, rs_=rs_: e.scalar_tensor_tensor(
                    out=y1[:], in0=xt[:, rs_, :], scalar=DN_ALPHA, in1=y1[:], op0=ALU.mult, op1=ALU.add),
                    reads=[S.B("xq", rs_), S.B("y1")], writes=[S.B("y1")])
                layer_norm_store(g, S, "lna", y1, S.B("y1"), lng, lnb, stats, mv, lrs, xo[o], S.B("xo", o),
                                 g.X1[ti * P:(ti + 1) * P, :], S.B("X1", ti))

    run_phase(g, body)


def phase_moe(g, st, layer, XIN, XOUT):
    S = g.S
    nc = g.nc
    midx = 0 if layer == 0 else 2
    g.sfx = "_L%d" % layer
    YEv = g.YE[0:NROW, :].rearrange("(p j) d -> p j d", j=512)

    with contextlib.ExitStack() as outer:
        dest_all = sbt(g, outer, "m_dest", [P, NT, 8], I32)
        slot_all = sbt(g, outer, "m_slot", [P, NT, 8], I32)
        gate_all = sbt(g, outer, "m_gate", [P, NT, 8])

        def body_r(ph):
            wr = sbt(g, ph, "r_wr", [P, KC, NE], BF16)
            wsgu = sbt(g, ph, "r_wsgu", [P, KC, 512], BF16)
            wsdn = sbt(g, ph, "r_wsdn", [P, 2, D], BF16)
            rb = sbt(g, ph, "r_rb", [P, NE])
            iotaE = sbt(g, ph, "r_iota", [P, NE])
            iotaB = sbt(g, ph, "r_iotaB", [P, NBLK])
            pidx = sbt(g, ph, "r_pidx", [P, 1])
            identf = sbt(g, ph, "r_identf", [P, P])
            triu = sbt(g, ph, "r_triu", [P, P], BF16)
            ones = sbt(g, ph, "r_ones", [P, P], BF16)
            ident = sbt(g, ph, "r_ident", [P, P], BF16)
            tokid = sbt(g, ph, "r_tokid", [P, NT, 2], I32)
            sh3 = sbt(g, ph, "r_sh3", [P, D])
            sc4 = sbt(g, ph, "r_sc4", [P, D])
            cum = sbt(g, ph, "r_cum", [P, NE], BF16)
            tinit = sbt(g, ph, "r_tinit", [P, 1024], I32)
            zrow = sbt(g, ph, "r_zrow", [1, D], BF16)
            xt = [sbt(g, ph, "r_xt%d" % i, [P, D]) for i in range(2)]
            tmp = sbt(g, ph, "r_tmp", [P, D])
            hb = [sbt(g, ph, "r_hb%d" % i, [P, D], BF16) for i in range(2)]
            hT = [sbt(g, ph, "r_hT%d" % i, [P, KC, P], BF16) for i in range(2)]
            sgt = sbt(g, ph, "r_sgt", [P, 2, P])
            actT = sbt(g, ph, "r_actT", [P, 2, P], BF16)
            sho = [sbt(g, ph, "r_sho%d" % i, [P, D]) for i in range(2)]
            sc = sbt(g, ph, "r_sc", [P, NE])
            bi = sbt(g, ph, "r_bi", [P, NE])
            m8 = sbt(g, ph, "r_m8", [P, 8, 8])
            gs = sbt(g, ph, "r_gs", [P, 8])
            gm8 = sbt(g, ph, "r_gm8", [P, 8])
            gmask = sbt(g, ph, "r_gmask", [P, 8])
            mk = sbt(g, ph, "r_mk", [P, NE])
            v8 = sbt(g, ph, "r_v8", [P, 8])
            selb = sbt(g, ph, "r_selb", [P, NE], BF16)
            Gu = sbt(g, ph, "r_Gu", [P, NE])
            G = sbt(g, ph, "r_G", [P, NE])
            den = sbt(g, ph, "r_den", [P, 1])
            e8 = sbt(g, ph, "r_e8", [P, 8], U32)
            eall = sbt(g, ph, "r_eall", [P, NT, 8])
            posk = sbt(g, ph, "r_posk", [P, NT, 8])
            psk = sbt(g, ph, "r_psk", [P, NT, 8])
            junk = sbt(g, ph, "r_junk", [P, NE])
            cnt = sbt(g, ph, "r_cnt", [P, NE])
            cnti = sbt(g, ph, "r_cnti", [P, NE], I32)
            pc = sbt(g, ph, "r_pc", [P, NE])
            onesf = sbt(g, ph, "r_onesf", [P, NE])
            pend = sbt(g, ph, "r_pend", [P, NE])
            pstart = sbt(g, ph, "r_pstart", [P, NE])
            pendT = sbt(g, ph, "r_pendT", [P, 2])
            cmpb = sbt(g, ph, "r_cmpb", [P, 2, NBLK], BF16)
            blkf = sbt(g, ph, "r_blkf", [P, NBLK])
            wif = sbt(g, ph, "r_wif", [P, NBLK])
            wi_all = sbt(g, ph, "r_wi", [P, 3, NBLK], I32)
            pcT = sbt(g, ph, "r_pcT", [P, 2])
            pcb = sbt(g, ph, "r_pcb", [P, 2, P], BF16)
            ucum = sbt(g, ph, "r_ucum", [P, 2, NE], BF16)
            bigv = sbt(g, ph, "r_bigv", [P, 2])
            phf = sbt(g, ph, "r_phf", [P, NT * 8])
            lof = sbt(g, ph, "r_lof", [P, NT * 8])
            hif = sbt(g, ph, "r_hif", [P, NT * 8])
            ps_tp = pst(g, ph, "r_ps_tp", [P, KC, P], BF16)
            ps_r = pst(g, ph, "r_ps_r", [P, 512])
            ps_g = pst(g, ph, "r_ps_g", [P, 4, P])
            ps_d = [pst(g, ph, "r_ps_d%d" % i, [P, 512]) for i in range(2)]
            ps_pos = pst(g, ph, "r_ps_pos", [P, 512])

            S.dma("pool", lambda e: e.dma_start(out=wr[:], in_=g.router_w[layer].rearrange("(k p) n -> p k n", p=P)),
                  writes=[S.B("wr")])
            S.dma("pool", lambda e: e.dma_start(out=wsgu[:], in_=g.sgu[layer].rearrange("(k p) n -> p k n", p=P)),
                  writes=[S.B("wsgu")])
            S.dma("pool", lambda e: e.dma_start(out=wsdn[:], in_=g.sdn[layer].rearrange("(k p) n -> p k n", p=P)),
                  writes=[S.B("wsdn")])
            S.dma("pool", lambda e: e.dma_start(out=triu[:], in_=g.c_triu[:, :]), writes=[S.B("triu")])
            S.dma("pool", lambda e: e.dma_start(out=ident[:], in_=g.c_ident[:, :]), writes=[S.B("ident")])
            S.dma("sp", lambda e: e.dma_start(out=identf[:], in_=g.c_ident[:, :]), writes=[S.B("identf")])
            S.dma("sp", lambda e: e.dma_start(out=iotaE[:], in_=g.c_iota[:, :]), writes=[S.B("iotaE")])
            S.dma("sp", lambda e: e.dma_start(out=iotaB[:], in_=g.c_iotab[:, 0:NBLK]), writes=[S.B("iotaB")])
            S.dma("sp", lambda e: e.dma_start(out=pidx[:], in_=g.c_pidx[:, :]), writes=[S.B("pidx")])
            S.dma("sp", lambda e: e.dma_start(out=bigv[:], in_=g.c_bigv[:, :]), writes=[S.B("bigv")])
            S.dma("pool", lambda e: e.dma_start(out=ucum[:], in_=g.c_ucum.rearrange("p (c n) -> p c n", c=2)),
                  writes=[S.B("ucum")])
            S.dma("sp", lambda e: e.dma_start(out=tokid[:], in_=g.c_tokid.rearrange("p (t two) -> p t two", two=2)),
                  writes=[S.B("tokid")])
            load_bcast(g, S, rb, g.router_b[layer:layer + 1, :])
            load_mod(g, S, sh3, midx, 3)
            load_mod(g, S, sc4, midx, 4)
            S.op("dve", lambda e: e.memset(ones[:], 1.0), writes=[S.B("ones")])
            S.op("dve", lambda e: e.memset(onesf[:], 1.0), writes=[S.B("onesf")])
            S.op("dve", lambda e: e.memset(cum[:], 0.0), writes=[S.B("cum")])
            S.op("dve", lambda e: e.memset(zrow[:], 0.0), writes=[S.B("zrow")])
            S.op("pool", lambda e: e.iota(tinit[:], [[0, 1024]], base=(1 << 20), channel_multiplier=0), writes=[S.B("tinit")])
            S.dma("sp", lambda e: e.dma_start(out=g.TBL[0:NROW, :].rearrange("(p r) two -> p (r two)", p=P),
                                              in_=tinit[:]), reads=[S.B("tinit")], writes=[S.B("TBLinit")])
            S.dma("sp", lambda e: e.dma_start(out=g.HB[SEQ:SEQ + 1, :], in_=zrow[:]), reads=[S.B("zrow")])

            def r_load(i):
                s = i % 2
                S.dma("sp", lambda e, s=s, i=i: e.dma_start(out=xt[s][:], in_=XIN[i * P:(i + 1) * P, :]),
                      writes=[S.B("xt", s)])
            r_load(0)
            for i in range(NT):
                s = i % 2
                xb = S.B("xt", s)
                if i + 1 < NT:
                    r_load(i + 1)
                modulate_transpose(g, S, xt[s][:], xb, sc4, sh3, tmp, S.B("tmp"), hb[s], S.B("hb", s),
                                   ps_tp, S.B("ps_tp"), hT[s], S.B("hT", s), ident, S.B("ident"))
                S.dma("sp", lambda e, s=s, i=i: e.dma_start(out=g.HB[i * P:(i + 1) * P, :], in_=hb[s][:]),
                      reads=[S.B("hb", s)])
                for k in range(KC):
                    S.op("pe", lambda e, k=k, s=s: e.matmul(ps_r[:, 0:NE], lhsT=hT[s][:, k, :], rhs=wr[:, k, :],
                                                            start=(k == 0), stop=(k == KC - 1)),
                         reads=[S.B("hT", s), S.B("wr")], writes=[S.B("ps_r")])
                for m in range(4):
                    for k in range(KC):
                        S.op("pe", lambda e, k=k, s=s, m=m: e.matmul(ps_g[:, m, :], lhsT=wsgu[:, k, m * P:(m + 1) * P],
                                                                     rhs=hT[s][:, k, :], start=(k == 0),
                                                                     stop=(k == KC - 1)),
                             reads=[S.B("hT", s), S.B("wsgu")], writes=[S.B("ps_g")])
                S.op("act", lambda e: e.activation(out=sgt[:], in_=ps_g[:, 0:2, :], func=AF.Silu),
                     reads=[S.B("ps_g")], writes=[S.B("sgt")])
                S.op("dve", lambda e: e.tensor_tensor(out=actT[:], in0=sgt[:], in1=ps_g[:, 2:4, :], op=ALU.mult),
                     reads=[S.B("sgt"), S.B("ps_g")], writes=[S.B("actT")])
                for n in range(2):
                    for j in range(2):
                        S.op("pe", lambda e, n=n, j=j: e.matmul(ps_d[n][:], lhsT=actT[:, j, :],
                                                                rhs=wsdn[:, j, n * 512:(n + 1) * 512],
                                                                start=(j == 0), stop=(j == 1)),
                             reads=[S.B("actT"), S.B("wsdn")], writes=[S.B("ps_d", n)])
                    S.op("act", lambda e, n=n, s=s: e.activation(out=sho[s][:, n * 512:(n + 1) * 512], in_=ps_d[n][:],
                                                                 func=AF.Copy),
                         reads=[S.B("ps_d", n)], writes=[S.B("sho", s)])
                S.dma("sp", lambda e, s=s, i=i: e.dma_start(out=g.SH[i * P:(i + 1) * P, :], in_=sho[s][:]),
                      reads=[S.B("sho", s)])
                S.op("act", lambda e: e.activation(out=sc[:], in_=ps_r[:, 0:NE], func=AF.Sigmoid),
                     reads=[S.B("ps_r")], writes=[S.B("sc")])
                S.op("dve", lambda e: e.tensor_tensor(out=bi[:], in0=sc[:], in1=rb[:], op=ALU.add),
                     reads=[S.B("sc"), S.B("modt", rb.name)], writes=[S.B("bi")])
                for gi in range(8):
                    S.op("dve", lambda e, gi=gi: e.max(out=m8[:, gi, :], in_=bi[:, gi * 32:(gi + 1) * 32]),
                         reads=[S.B("bi")], writes=[S.B("m8")])
                S.op("dve", lambda e: e.tensor_tensor(out=gs[:], in0=m8[:, :, 0], in1=m8[:, :, 1], op=ALU.add),
                     reads=[S.B("m8")], writes=[S.B("gs")])
                S.op("dve", lambda e: e.max(out=gm8[:], in_=gs[:]), reads=[S.B("gs")], writes=[S.B("gm8")])
                S.op("dve", lambda e: e.tensor_scalar(out=gmask[:], in0=gs[:], scalar1=gm8[:, 3:4], scalar2=None,
                                                      op0=ALU.is_ge), reads=[S.B("gs"), S.B("gm8")], writes=[S.B("gmask")])
                S.op("dve", lambda e: e.scalar_tensor_tensor(
                    out=mk[:].rearrange("p (a b) -> p a b", b=32), in0=bi[:].rearrange("p (a b) -> p a b", b=32),
                    scalar=2.0, in1=gmask[:].unsqueeze(2).to_broadcast([P, 8, 32]), op0=ALU.add, op1=ALU.mult),
                    reads=[S.B("bi"), S.B("gmask")], writes=[S.B("mk")])
                S.op("dve", lambda e: e.max(out=v8[:], in_=mk[:]), reads=[S.B("mk")], writes=[S.B("v8")])
                S.op("dve", lambda e: e.max_index(out=e8[:], in_max=v8[:], in_values=mk[:]),
                     reads=[S.B("mk"), S.B("v8")], writes=[S.B("e8")])
                S.op("dve", lambda e, i=i: e.tensor_copy(out=eall[:, i, :], in_=e8[:]), reads=[S.B("e8")],
                     writes=[S.B("eall", i)])
                S.op("dve", lambda e: e.tensor_scalar(out=selb[:], in0=mk[:], scalar1=v8[:, 7:8], scalar2=None,
                                                      op0=ALU.is_ge), reads=[S.B("mk"), S.B("v8")], writes=[S.B("selb")])
                S.op("dve", lambda e: e.scalar_tensor_tensor(out=Gu[:], in0=sc[:], scalar=1.0, in1=selb[:],
                                                             op0=ALU.mult, op1=ALU.mult, accum_out=den[:]),
                     reads=[S.B("sc"), S.B("selb")], writes=[S.B("Gu"), S.B("den")])
                S.op("dve", lambda e: e.reciprocal(out=den[:], in_=den[:]), reads=[S.B("den")], writes=[S.B("den")])
                S.op("dve", lambda e: e.tensor_scalar(out=G[:], in0=Gu[:], scalar1=den[:, 0:1], scalar2=2.5,
                                                      op0=ALU.mult, op1=ALU.mult),
                     reads=[S.B("Gu"), S.B("den")], writes=[S.B("G")])
                S.op("pe", lambda e: e.matmul(ps_pos[:, 0:NE], lhsT=triu[:], rhs=selb[:], start=True, stop=False),
                     reads=[S.B("triu"), S.B("selb")], writes=[S.B("ps_pos")])
                S.op("pe", lambda e: e.matmul(ps_pos[:, 0:NE], lhsT=ones[:], rhs=cum[:], start=False, stop=True),
                     reads=[S.B("ones"), S.B("cum")], writes=[S.B("ps_pos")])
                S.op("dve", lambda e: e.tensor_tensor(out=cum[:], in0=cum[:], in1=selb[:], op=ALU.add),
                     reads=[S.B("cum"), S.B("selb")], writes=[S.B("cum")])
                for k in range(8):
                    S.op("dve", lambda e, i=i, k=k: e.scalar_tensor_tensor(
                        out=junk[:], in0=iotaE[:], scalar=eall[:, i, k:k + 1], in1=G[:], op0=ALU.is_equal, op1=ALU.mult,
                        accum_out=gate_all[:, i, k:k + 1]),
                        reads=[S.B("iotaE"), S.B("eall", i), S.B("G")], writes=[S.B("junk"), S.B("gate", i)])
                    S.op("dve", lambda e, i=i, k=k: e.scalar_tensor_tensor(
                        out=junk[:], in0=iotaE[:], scalar=eall[:, i, k:k + 1], in1=ps_pos[:, 0:NE], op0=ALU.is_equal,
                        op1=ALU.mult, accum_out=posk[:, i, k:k + 1]),
                        reads=[S.B("iotaE"), S.B("eall", i), S.B("ps_pos")], writes=[S.B("junk"), S.B("posk", i)])

            S.op("pe", lambda e: e.matmul(ps_pos[:, 0:NE], lhsT=ones[:], rhs=cum[:], start=True, stop=True),
                 reads=[S.B("ones"), S.B("cum")], writes=[S.B("ps_pos")])
            S.op("dve", lambda e: e.tensor_copy(out=cnt[:], in_=ps_pos[:, 0:NE]), reads=[S.B("ps_pos")], writes=[S.B("cnt")])
            S.op("dve", lambda e: e.memset(pc[:], 0.0), writes=[S.B("pc")])
            for j in range(32):
                S.op("dve", lambda e, j=j: e.scalar_tensor_tensor(out=pc[:], in0=cnt[:], scalar=float(P * j), in1=pc[:],
                                                                  op0=ALU.is_gt, op1=ALU.add),
                     reads=[S.B("cnt"), S.B("pc")], writes=[S.B("pc")])
            S.op("dve", lambda e: e.tensor_scalar(out=pc[:], in0=pc[:], scalar1=float(P), scalar2=None, op0=ALU.mult),
                 reads=[S.B("pc")], writes=[S.B("pc")])
            for c in range(2):
                S.op("dve", lambda e, c=c: e.scalar_tensor_tensor(
                    out=junk[:, 0:P], in0=pc[:, c * P:(c + 1) * P], scalar=1.0, in1=identf[:], op0=ALU.mult,
                    op1=ALU.mult, accum_out=pcT[:, c:c + 1]),
                    reads=[S.B("pc"), S.B("identf")], writes=[S.B("junk"), S.B("pcT")])
            for c in range(2):
                S.op("dve", lambda e, c=c: e.tensor_copy(out=pcb[:, c, :], in_=pcT[:, c:c + 1].to_broadcast([P, P])),
                     reads=[S.B("pcT")], writes=[S.B("pcb")])
            for c in range(2):
                S.op("pe", lambda e, c=c: e.matmul(ps_pos[:, 0:NE], lhsT=pcb[:, c, :], rhs=ucum[:, c, :],
                                                   start=(c == 0), stop=(c == 1)),
                     reads=[S.B("pcb"), S.B("ucum")], writes=[S.B("ps_pos")])
            S.op("dve", lambda e: e.tensor_copy(out=pend[:], in_=ps_pos[:, 0:NE]), reads=[S.B("ps_pos")],
                 writes=[S.B("pend")])
            S.op("dve", lambda e: e.tensor_tensor(out=pstart[:], in0=pend[:], in1=pc[:], op=ALU.subtract),
                 reads=[S.B("pend"), S.B("pc")], writes=[S.B("pstart")])
            for c in range(2):
                S.op("dve", lambda e, c=c: e.scalar_tensor_tensor(
                    out=junk[:, 0:P], in0=pend[:, c * P:(c + 1) * P], scalar=1.0, in1=identf[:], op0=ALU.mult,
                    op1=ALU.mult, accum_out=pendT[:, c:c + 1]),
                    reads=[S.B("pend"), S.B("identf")], writes=[S.B("junk"), S.B("pendT")])
            S.op("dve", lambda e: e.tensor_tensor(out=pendT[:], in0=pendT[:], in1=bigv[:], op=ALU.add),
                 reads=[S.B("pendT"), S.B("bigv")], writes=[S.B("pendT")])
            for c in range(2):
                S.op("dve", lambda e, c=c: e.tensor_scalar(out=cmpb[:, c, :], in0=iotaB[:], scalar1=pendT[:, c:c + 1],
                                                           scalar2=None, op0=ALU.is_ge),
                     reads=[S.B("iotaB"), S.B("pendT")], writes=[S.B("cmpb")])
            for c in range(2):
                S.op("pe", lambda e, c=c: e.matmul(ps_r[:, 0:NBLK], lhsT=ones[:], rhs=cmpb[:, c, :], start=(c == 0),
                                                   stop=(c == 1)),
                     reads=[S.B("ones"), S.B("cmpb")], writes=[S.B("ps_r")])
            S.op("dve", lambda e: e.tensor_copy(out=blkf[:], in_=ps_r[:, 0:NBLK]), reads=[S.B("ps_r")],
                 writes=[S.B("blkf")])
            S.op("dve", lambda e: e.tensor_scalar(out=wif[:], in0=blkf[:], scalar1=float(P) if g.netab == NE else 0.0, scalar2=pidx[:, 0:1],
                                                  op0=ALU.mult, op1=ALU.add),
                 reads=[S.B("blkf"), S.B("pidx")], writes=[S.B("wif")])
            S.op("dve", lambda e: e.tensor_scalar(out=blkf[:], in0=iotaB[:], scalar1=pend[:, NE - 1:NE], scalar2=1.0e6,
                                                  op0=ALU.is_ge, op1=ALU.mult),
                 reads=[S.B("iotaB"), S.B("pend"), S.B("wif")], writes=[S.B("blkf")])
            S.op("dve", lambda e: e.tensor_tensor(out=wif[:], in0=wif[:], in1=blkf[:], op=ALU.add),
                 reads=[S.B("wif"), S.B("blkf")], writes=[S.B("wif")])
            S.op("dve", lambda e: e.tensor_copy(out=wi_all[:, 2, :], in_=wif[:]), reads=[S.B("wif")], writes=[S.B("wi")])
            S.op("dve", lambda e: e.tensor_scalar(out=wi_all[:, 0, :], in0=wif[:], scalar1=2.0, scalar2=None,
                                                  op0=ALU.mult), reads=[S.B("wif")], writes=[S.B("wi")])
            S.op("dve", lambda e: e.tensor_scalar(out=wi_all[:, 1, :], in0=wif[:], scalar1=2.0, scalar2=1.0,
                                                  op0=ALU.mult, op1=ALU.add), reads=[S.B("wif")], writes=[S.B("wi")])
            S.dma("sp", lambda e: e.dma_start(out=g.WI[:, :], in_=wi_all[:].rearrange("p a b -> p (a b)")),
                  reads=[S.B("wi")])
            for i in range(NT):
                for k in range(8):
                    S.op("dve", lambda e, i=i, k=k: e.scalar_tensor_tensor(
                        out=junk[:], in0=iotaE[:], scalar=eall[:, i, k:k + 1], in1=pstart[:], op0=ALU.is_equal,
                        op1=ALU.mult, accum_out=psk[:, i, k:k + 1]),
                        reads=[S.B("iotaE"), S.B("eall", i), S.B("pstart")], writes=[S.B("junk"), S.B("psk")])
            pskb = [S.B("psk")] + [S.B("posk", i) for i in range(NT)]
            posf = posk[:].rearrange("p a b -> p (a b)")
            pskf = psk[:].rearrange("p a b -> p (a b)")
            S.op("dve", lambda e: e.memset(phf[:], 0.0), writes=[S.B("phf")])
            for j in range(1, 32):
                S.op("dve", lambda e, j=j: e.scalar_tensor_tensor(out=phf[:], in0=posf, scalar=float(P * j), in1=phf[:],
                                                                  op0=ALU.is_ge, op1=ALU.add),
                     reads=pskb + [S.B("phf")], writes=[S.B("phf")])
            S.op("dve", lambda e: e.scalar_tensor_tensor(out=lof[:], in0=phf[:], scalar=-float(P), in1=posf,
                                                         op0=ALU.mult, op1=ALU.add),
                 reads=pskb + [S.B("phf")], writes=[S.B("lof")])
            S.op("dve", lambda e: e.scalar_tensor_tensor(out=hif[:], in0=pskf, scalar=1.0 / P, in1=phf[:],
                                                         op0=ALU.mult, op1=ALU.add),
                 reads=pskb + [S.B("phf")], writes=[S.B("hif")])
            S.op("dve", lambda e: e.scalar_tensor_tensor(out=dest_all[:].rearrange("p a b -> p (a b)"), in0=lof[:],
                                                         scalar=512.0, in1=hif[:], op0=ALU.mult, op1=ALU.add),
                 reads=[S.B("lof"), S.B("hif")], writes=[S.B("dest")])
            S.op("dve", lambda e: e.tensor_tensor(out=slot_all[:].rearrange("p a b -> p (a b)"), in0=pskf, in1=posf,
                                                  op=ALU.add),
                 reads=pskb, writes=[S.B("slotn")])
            if g.debug and g.debug.endswith("_r"):
                dbg = sbt(g, ph, "r_dbg", [P, 4096])
                S.op("dve", lambda e: e.memset(dbg[:], 0.0), writes=[S.B("dbg")])
                items = [(cnt[:], 256), (pc[:], 256), (pend[:], 256), (pstart[:], 256), (blkf[:], 512),
                         (psk[:].rearrange("p a b -> p (a b)"), 256), (posk[:].rearrange("p a b -> p (a b)"), 256),
                         (eall[:].rearrange("p a b -> p (a b)"), 256), (dest_all[:].rearrange("p a b -> p (a b)"), 256),
                         (gate_all[:].rearrange("p a b -> p (a b)"), 256), (phf[:], 256), (pendT[:], 2)]
                off = 0
                allb = [S.B(n) for n in ("cnt", "pc", "pend", "pstart", "blkf", "psk", "dest", "phf", "pendT")]
                allb += [S.B("posk", i) for i in range(NT)] + [S.B("eall", i) for i in range(NT)] + [S.B("gate", i) for i in range(NT)]
                for ap, n in items:
                    S.op("dve", lambda e, ap=ap, n=n, off=off: e.tensor_copy(out=dbg[:, off:off + n], in_=ap),
                         reads=allb, writes=[S.B("dbg")])
                    off += n
                S.dma("sp", lambda e: e.dma_start(out=g.DBG[:, :], in_=dbg[:]), reads=[S.B("dbg")])
            for i in range(NT):
                for k in range(8):
                    S.dma("pool", lambda e, i=i, k=k: e.indirect_dma_start(
                        out=g.TBL[:, :], out_offset=bass.IndirectOffsetOnAxis(ap=dest_all[:, i, k:k + 1], axis=0),
                        in_=tokid[:, i, :], in_offset=None, bounds_check=S.reg(e, NROW - 1), oob_is_err=False),
                        reads=[S.B("dest"), S.B("tokid"), S.B("TBLinit")])

        run_phase(g, body_r)
        if g.debug == 'moe%d_r' % layer:
            return

        def body_e(ph):
            ident = sbt(g, ph, "e_ident", [P, P], BF16)
            idx2 = sbt(g, ph, "e_idx", [P, 512, 2], I32)
            wi = sbt(g, ph, "e_wi", [P, 3, NBLK], I32)
            NS = 4
            wf = [sbt(g, ph, "e_wf%d" % i, [P, 6144]) for i in range(NS)]
            wgu = [sbt(g, ph, "e_wgu%d" % i, [P, KC, 512], BF16) for i in range(NS)]
            wdn = [sbt(g, ph, "e_wdn%d" % i, [P, 2, D], BF16) for i in range(NS)]
            xg = [sbt(g, ph, "e_xg%d" % i, [P, D], BF16) for i in range(NS)]
            xT = [sbt(g, ph, "e_xT%d" % i, [P, KC, P], BF16) for i in range(2)]
            sgt = [sbt(g, ph, "e_sgt%d" % i, [P, 2, P]) for i in range(2)]
            actT = [sbt(g, ph, "e_actT%d" % i, [P, 2, P], BF16) for i in range(2)]
            ysb = [sbt(g, ph, "e_ysb%d" % i, [P, D], BF16) for i in range(2)]
            ps_t = [pst(g, ph, "e_ps_t%d" % i, [P, KC, P], BF16) for i in range(2)]
            ps_a = [pst(g, ph, "e_ps_a%d" % i, [P, 4, P]) for i in range(2)]
            ps_y = [pst(g, ph, "e_ps_y%d" % i, [P, 512]) for i in range(4)]

            S.dma("pool", lambda e: e.dma_start(out=ident[:], in_=g.c_ident[:, :]), writes=[S.B("ident")])
            S.dma("sp", lambda e: e.dma_start(out=idx2[:], in_=g.TBL[0:NROW, :].rearrange("(p j) two -> p j two", j=512)),
                  writes=[S.B("idx2")])
            S.dma("sp", lambda e: e.dma_start(out=wi[:].rearrange("p a b -> p (a b)"), in_=g.WI[:, :]),
                  writes=[S.B("wi")])
            for i_ in range(NS):
                S.op("dve", lambda e, i_=i_: e.memset(xg[i_][:], 0.0), writes=[S.B("xg", i_)])

            def fetch(b):
                s = b % NS
                S.dma("pool", lambda e, s=s, b=b: e.indirect_dma_start(
                    out=wf[s][:], out_offset=None, in_=g.ew[layer][:, :],
                    in_offset=bass.IndirectOffsetOnAxis(ap=wi[:, 2, b:b + 1], axis=0),
                    bounds_check=S.reg(e, g.netab * P - 1), oob_is_err=False),
                    reads=[S.B("wi")], writes=[S.B("wf", s)])
                S.dma("pool", lambda e, s=s, b=b: e.indirect_dma_start(
                    out=xg[s][:], out_offset=None, in_=g.HB[:, :],
                    in_offset=bass.IndirectOffsetOnAxis(ap=idx2[:, b, 0:1], axis=0),
                    bounds_check=S.reg(e, SEQ - 1), oob_is_err=False),
                    reads=[S.B("idx2")], writes=[S.B("xg", s)])

            def cast_w(b):
                s = b % NS
                s2 = b % 2
                wgv = wf[s][:, 0:4096].rearrange("p (k n) -> p k n", n=512)
                wdv = wf[s][:, 4096:6144].rearrange("p (k n) -> p k n", n=D)
                S.op("act", lambda e, s=s, wgv=wgv: e.activation(out=wgu[s][:, 0:5, :], in_=wgv[:, 0:5, :], func=AF.Copy),
                     reads=[S.B("wf", s)], writes=[S.B("wgu", s, 0)])
                S.op("dve", lambda e, s=s, wgv=wgv: e.tensor_copy(out=wgu[s][:, 5:8, :], in_=wgv[:, 5:8, :]),
                     reads=[S.B("wf", s)], writes=[S.B("wgu", s, 1)])
                S.op("dve", lambda e, s=s, wdv=wdv: e.tensor_copy(out=wdn[s][:], in_=wdv),
                     reads=[S.B("wf", s)], writes=[S.B("wdn", s)])

            def stage_a(b):
                s = b % NS
                s2 = b % 2
                cast_w(b)
                for k in range(KC):
                    S.op("pe", lambda e, s=s, s2=s2, k=k: e.transpose(ps_t[s2][:, k, :], xg[s][:, k * P:(k + 1) * P],
                                                                      ident[:]),
                         reads=[S.B("xg", s), S.B("ident")], writes=[S.B("ps_t", s2)])
                S.op("act", lambda e, s2=s2: e.activation(out=xT[s2][:], in_=ps_t[s2][:], func=AF.Copy),
                     reads=[S.B("ps_t", s2)], writes=[S.B("xT", s2)])

            def stage_b(b):
                s = b % NS
                s2 = b % 2
                wb = [S.B("wgu", s, 0), S.B("wgu", s, 1)]
                for m in range(4):
                    for k in range(KC):
                        S.op("pe", lambda e, s=s, s2=s2, m=m, k=k: e.matmul(
                            ps_a[s2][:, m, :], lhsT=wgu[s][:, k, m * P:(m + 1) * P], rhs=xT[s2][:, k, :],
                            start=(k == 0), stop=(k == KC - 1)),
                            reads=[S.B("xT", s2)] + wb, writes=[S.B("ps_a", s2)])
                S.op("act", lambda e, s2=s2: e.activation(out=sgt[s2][:], in_=ps_a[s2][:, 0:2, :], func=AF.Silu),
                     reads=[S.B("ps_a", s2)], writes=[S.B("sgt", s2)])
                S.op("dve", lambda e, s2=s2: e.tensor_tensor(out=actT[s2][:], in0=sgt[s2][:], in1=ps_a[s2][:, 2:4, :],
                                                             op=ALU.mult),
                     reads=[S.B("sgt", s2), S.B("ps_a", s2)], writes=[S.B("actT", s2)])

            def stage_c(b):
                s = b % NS
                s2 = b % 2
                for n in range(2):
                    pi = s2 * 2 + n
                    for j in range(2):
                        S.op("pe", lambda e, s=s, s2=s2, n=n, j=j, pi=pi: e.matmul(
                            ps_y[pi][:], lhsT=actT[s2][:, j, :], rhs=wdn[s][:, j, n * 512:(n + 1) * 512],
                            start=(j == 0), stop=(j == 1)),
                            reads=[S.B("actT", s2), S.B("wdn", s)], writes=[S.B("ps_y", pi)])
                    if n == 0:
                        S.op("act", lambda e, s2=s2, n=n, pi=pi: e.activation(
                            out=ysb[s2][:, n * 512:(n + 1) * 512], in_=ps_y[pi][:], func=AF.Copy),
                            reads=[S.B("ps_y", pi)], writes=[S.B("ysb", s2)])
                    else:
                        S.op("dve", lambda e, s2=s2, n=n, pi=pi: e.tensor_copy(
                            out=ysb[s2][:, n * 512:(n + 1) * 512], in_=ps_y[pi][:]),
                            reads=[S.B("ps_y", pi)], writes=[S.B("ysb", s2)])
                S.dma("sp", lambda e, s2=s2, b=b: e.dma_start(out=g.YE[b * P:(b + 1) * P, :], in_=ysb[s2][:]),
                      reads=[S.B("ysb", s2)])

            fetch(0)
            fetch(1)
            fetch(2)
            for it in range(NBLK + 2):
                if it + 3 < NBLK:
                    fetch(it + 3)
                if it < NBLK:
                    stage_a(it)
                if 0 <= it - 1 < NBLK:
                    stage_b(it - 1)
                if 0 <= it - 2 < NBLK:
                    stage_c(it - 2)

        run_phase(g, body_e)
        if g.debug == 'moe%d_e' % layer:
            return

        def body_c(ph):
            gate5 = sbt(g, ph, "c_gate5", [P, D])
            lng = sbt(g, ph, "c_lng", [P, D])
            lnb = sbt(g, ph, "c_lnb", [P, D])
            xt = [sbt(g, ph, "c_xt%d" % i, [P, D]) for i in range(2)]
            acc = [sbt(g, ph, "c_acc%d" % i, [P, D]) for i in range(2)]
            R = [sbt(g, ph, "c_R%d" % i, [P, 8, D], BF16) for i in range(2)]
            stats = sbt(g, ph, "c_stats", [P, 2, 6])
            mv = sbt(g, ph, "c_mv", [P, 2])
            lrs = sbt(g, ph, "c_lrs", [P, 1])
            xo = [sbt(g, ph, "c_xo%d" % i, [P, D]) for i in range(2)]
            load_mod(g, S, gate5, midx, 5)
            load_bcast(g, S, lng, g.ln_g[layer, 1:2, :])
            load_bcast(g, S, lnb, g.ln_b[layer, 1:2, :])
            def c_load(i):
                s = i % 2
                S.dma("sp", lambda e, s=s, i=i: e.dma_start(out=xt[s][:], in_=XIN[i * P:(i + 1) * P, :]),
                      writes=[S.B("xt", s)])
                S.dma("sp", lambda e, s=s, i=i: e.dma_start(out=acc[s][:], in_=g.SH[i * P:(i + 1) * P, :]),
                      writes=[S.B("acc", s)])
                for k in range(8):
                    S.dma("pool", lambda e, s=s, i=i, k=k: e.indirect_dma_start(
                        out=R[s][:, k, :], out_offset=None, in_=g.YE[:, :],
                        in_offset=bass.IndirectOffsetOnAxis(ap=slot_all[:, i, k:k + 1], axis=0),
                        bounds_check=S.reg(e, NROW - 1), oob_is_err=False),
                        writes=[S.B("R", s, k)])
            c_load(0)
            for i in range(NT):
                s = i % 2
                if i + 1 < NT:
                    c_load(i + 1)
                for k in range(8):
                    S.op("dve", lambda e, s=s, i=i, k=k: e.scalar_tensor_tensor(
                        out=acc[s][:], in0=R[s][:, k, :], scalar=gate_all[:, i, k:k + 1], in1=acc[s][:],
                        op0=ALU.mult, op1=ALU.add),
                        reads=[S.B("R", s, k), S.B("acc", s)], writes=[S.B("acc", s)])
                S.op("pool", lambda e, s=s: e.tensor_tensor(out=acc[s][:], in0=acc[s][:], in1=gate5[:], op=ALU.mult),
                     reads=[S.B("acc", s), S.B("modt", gate5.name)], writes=[S.B("acc", s)])
                S.op("dve", lambda e, s=s: e.scalar_tensor_tensor(out=acc[s][:], in0=xt[s][:], scalar=DN_ALPHA,
                                                                  in1=acc[s][:], op0=ALU.mult, op1=ALU.add),
                     reads=[S.B("xt", s), S.B("acc", s)], writes=[S.B("acc", s)])
                layer_norm_store(g, S, "lnc", acc[s], S.B("acc", s), lng, lnb, stats, mv, lrs, xo[s], S.B("xo", s),
                                 XOUT[i * P:(i + 1) * P, :], S.B("XOUT", i))

        run_phase(g, body_c)


def phase_conv(g, st):
    S = g.S
    g.sfx = ""

    def body1(ph):
        win = sbt(g, ph, "v_win", [P, KC, 3 * D], BF16)
        ident = sbt(g, ph, "v_ident", [P, P], BF16)
        shx = sbt(g, ph, "v_shx", [P, D])
        scx = sbt(g, ph, "v_scx", [P, D])
        zrow = sbt(g, ph, "v_zrow", [1, D])
        xt = [sbt(g, ph, "v_xt%d" % i, [P, D]) for i in range(2)]
        tmp = sbt(g, ph, "v_tmp", [P, D])
        hb = [sbt(g, ph, "v_hb%d" % i, [P, D], BF16) for i in range(2)]
        hT = [sbt(g, ph, "v_hT%d" % i, [P, KC, P], BF16) for i in range(2)]
        bgt = [sbt(g, ph, "v_bgt%d" % i, [P, D]) for i in range(2)]
        vt = sbt(g, ph, "v_vt", [P, D])
        ut = [sbt(g, ph, "v_ut%d" % i, [P, D]) for i in range(2)]
        ps_tp = pst(g, ph, "v_ps_tp", [P, KC, P], BF16)
        ps = [pst(g, ph, "v_ps%d" % i, [P, 512]) for i in range(6)]
        for c in range(2):
            S.dma("pool", lambda e, c=c: e.dma_start(
                out=win[:, :, c * 1536:(c + 1) * 1536],
                in_=g.w_in[:, c * 1536:(c + 1) * 1536].rearrange("(k p) n -> p k n", p=P)), writes=[S.B("win", c)])
        S.dma("pool", lambda e: e.dma_start(out=ident[:], in_=g.c_ident[:, :]), writes=[S.B("ident")])
        load_mod(g, S, shx, 2, 0)
        load_mod(g, S, scx, 2, 1)
        S.op("dve", lambda e: e.memset(zrow[:], 0.0), writes=[S.B("zrow")])
        S.dma("sp", lambda e: e.dma_start(out=g.UU[0:1, :], in_=zrow[:]), reads=[S.B("zrow")])
        S.dma("sp", lambda e: e.dma_start(out=g.UU[SEQ + 1:SEQ + 2, :], in_=zrow[:]), reads=[S.B("zrow")])
        wb = [S.B("win", 0), S.B("win", 1)]
        def v_load(i):
            s = i % 2
            S.dma("sp", lambda e, s=s, i=i: e.dma_start(out=xt[s][:], in_=g.X2[i * P:(i + 1) * P, :]),
                  writes=[S.B("xt", s)])
        v_load(0)
        for i in range(NT):
            s = i % 2
            xb = S.B("xt", s)
            if i + 1 < NT:
                v_load(i + 1)
            modulate_transpose(g, S, xt[s][:], xb, scx, shx, tmp, S.B("tmp"), hb[s], S.B("hb", s),
                               ps_tp, S.B("ps_tp"), hT[s], S.B("hT", s), ident, S.B("ident"))
            for n in range(6):
                for k in range(KC):
                    S.op("pe", lambda e, s=s, n=n, k=k: e.matmul(ps[n][:], lhsT=hT[s][:, k, :],
                                                                 rhs=win[:, k, n * 512:(n + 1) * 512],
                                                                 start=(k == 0), stop=(k == KC - 1)),
                         reads=[S.B("hT", s)] + wb, writes=[S.B("ps", n)])
            for n in range(2):
                S.op("act", lambda e, s=s, n=n: e.activation(out=bgt[s][:, n * 512:(n + 1) * 512], in_=ps[n][:],
                                                             func=AF.Copy),
                     reads=[S.B("ps", n)], writes=[S.B("bgt", s)])
                S.op("act", lambda e, n=n: e.activation(out=vt[:, n * 512:(n + 1) * 512], in_=ps[4 + n][:],
                                                        func=AF.Copy),
                     reads=[S.B("ps", 4 + n)], writes=[S.B("vt")])
                S.op("dve", lambda e, s=s, n=n: e.tensor_tensor(out=ut[s][:, n * 512:(n + 1) * 512], in0=ps[2 + n][:],
                                                                in1=vt[:, n * 512:(n + 1) * 512], op=ALU.mult),
                     reads=[S.B("ps", 2 + n), S.B("vt")], writes=[S.B("ut", s)])
            S.dma("sp", lambda e, s=s, i=i: e.dma_start(out=g.BG[i * P:(i + 1) * P, :], in_=bgt[s][:]),
                  reads=[S.B("bgt", s)])
            S.dma("sp", lambda e, s=s, i=i: e.dma_start(out=g.UU[1 + i * P:1 + (i + 1) * P, :], in_=ut[s][:]),
                  reads=[S.B("ut", s)])

    run_phase(g, body1)

    def body2(ph):
        wout = sbt(g, ph, "w_wout", [P, KC, D], BF16)
        ident = sbt(g, ph, "w_ident", [P, P], BF16)
        tp = [sbt(g, ph, "w_tap%d" % i, [P, D]) for i in range(3)]
        gate = sbt(g, ph, "w_gate", [P, D])
        lng = sbt(g, ph, "w_lng", [P, D])
        lnb = sbt(g, ph, "w_lnb", [P, D])
        xt = [sbt(g, ph, "w_xt%d" % i, [P, D]) for i in range(2)]
        up = [sbt(g, ph, "w_up%d" % i, [P, D]) for i in range(2)]
        uc = [sbt(g, ph, "w_uc%d" % i, [P, D]) for i in range(2)]
        un = [sbt(g, ph, "w_un%d" % i, [P, D]) for i in range(2)]
        bgt = [sbt(g, ph, "w_bgt%d" % i, [P, D]) for i in range(2)]
        zb = sbt(g, ph, "w_zb", [P, D], BF16)
        zT = sbt(g, ph, "w_zT", [P, KC, P], BF16)
        y1 = sbt(g, ph, "w_y1", [P, D])
        stats = sbt(g, ph, "w_stats", [P, 2, 6])
        mv = sbt(g, ph, "w_mv", [P, 2])
        lrs = sbt(g, ph, "w_lrs", [P, 1])
        xo = [sbt(g, ph, "w_xo%d" % i, [P, D]) for i in range(2)]
        ps_tp = pst(g, ph, "w_ps_tp", [P, KC, P], BF16)
        ps_p = [pst(g, ph, "w_ps_p%d" % i, [P, 512]) for i in range(2)]
        S.dma("pool", lambda e: e.dma_start(out=wout[:], in_=g.w_out.rearrange("(k p) n -> p k n", p=P)),
              writes=[S.B("wout")])
        S.dma("pool", lambda e: e.dma_start(out=ident[:], in_=g.c_ident[:, :]), writes=[S.B("ident")])
        for j in range(3):
            load_bcast(g, S, tp[j], g.taps[j:j + 1, :])
        load_mod(g, S, gate, 2, 2)
        load_bcast(g, S, lng, g.ln_g[1, 0:1, :])
        load_bcast(g, S, lnb, g.ln_b[1, 0:1, :])
        def w_load(i):
            s = i % 2
            S.dma("sp", lambda e, s=s, i=i: e.dma_start(out=xt[s][:], in_=g.X2[i * P:(i + 1) * P, :]),
                  writes=[S.B("xt", s)])
            S.dma("sp", lambda e, s=s, i=i: e.dma_start(out=up[s][:], in_=g.UU[i * P:(i + 1) * P, :]),
                  writes=[S.B("up", s)])
            S.dma("sp", lambda e, s=s, i=i: e.dma_start(out=uc[s][:], in_=g.UU[1 + i * P:1 + (i + 1) * P, :]),
                  writes=[S.B("uc", s)])
            S.dma("sp", lambda e, s=s, i=i: e.dma_start(out=un[s][:], in_=g.UU[2 + i * P:2 + (i + 1) * P, :]),
                  writes=[S.B("un", s)])
            S.dma("sp", lambda e, s=s, i=i: e.dma_start(out=bgt[s][:], in_=g.BG[i * P:(i + 1) * P, :]),
                  writes=[S.B("bgt", s)])
        w_load(0)
        for i in range(NT):
            s = i % 2
            if i + 1 < NT:
                w_load(i + 1)
            S.op("dve", lambda e, s=s: e.tensor_tensor(out=up[s][:], in0=up[s][:], in1=tp[0][:], op=ALU.mult),
                 reads=[S.B("up", s), S.B("modt", tp[0].name)], writes=[S.B("up", s)])
            S.op("pool", lambda e, s=s: e.tensor_tensor(out=uc[s][:], in0=uc[s][:], in1=tp[1][:], op=ALU.mult),
                 reads=[S.B("uc", s), S.B("modt", tp[1].name)], writes=[S.B("uc", s)])
            S.op("pool", lambda e, s=s: e.tensor_tensor(out=un[s][:], in0=un[s][:], in1=tp[2][:], op=ALU.mult),
                 reads=[S.B("un", s), S.B("modt", tp[2].name)], writes=[S.B("un", s)])
            S.op("dve", lambda e, s=s: e.tensor_tensor(out=up[s][:], in0=up[s][:], in1=uc[s][:], op=ALU.add),
                 reads=[S.B("up", s), S.B("uc", s)], writes=[S.B("up", s)])
            S.op("dve", lambda e, s=s: e.tensor_tensor(out=up[s][:], in0=up[s][:], in1=un[s][:], op=ALU.add),
                 reads=[S.B("up", s), S.B("un", s)], writes=[S.B("up", s)])
            S.op("dve", lambda e, s=s: e.tensor_tensor(out=zb[:], in0=up[s][:], in1=bgt[s][:], op=ALU.mult),
                 reads=[S.B("up", s), S.B("bgt", s)], writes=[S.B("zb")])
            for k in range(KC):
                S.op("pe", lambda e, k=k: e.transpose(ps_tp[:, k, :], zb[:, k * P:(k + 1) * P], ident[:]),
                     reads=[S.B("zb"), S.B("ident")], writes=[S.B("ps_tp")])
            S.op("act", lambda e: e.activation(out=zT[:], in_=ps_tp[:], func=AF.Copy),
                 reads=[S.B("ps_tp")], writes=[S.B("zT")])
            for n in range(2):
                for k in range(KC):
                    S.op("pe", lambda e, n=n, k=k: e.matmul(ps_p[n][:], lhsT=zT[:, k, :],
                                                            rhs=wout[:, k, n * 512:(n + 1) * 512],
                                                            start=(k == 0), stop=(k == KC - 1)),
                         reads=[S.B("zT"), S.B("wout")], writes=[S.B("ps_p", n)])
                S.op("dve", lambda e, n=n: e.tensor_tensor(out=y1[:, n * 512:(n + 1) * 512], in0=ps_p[n][:],
                                                           in1=gate[:, n * 512:(n + 1) * 512], op=ALU.mult),
                     reads=[S.B("ps_p", n), S.B("modt", gate.name)], writes=[S.B("y1")])
            S.op("dve", lambda e, s=s: e.scalar_tensor_tensor(out=y1[:], in0=xt[s][:], scalar=DN_ALPHA, in1=y1[:],
                                                              op0=ALU.mult, op1=ALU.add),
                 reads=[S.B("xt", s), S.B("y1")], writes=[S.B("y1")])
            layer_norm_store(g, S, "lnv", y1, S.B("y1"), lng, lnb, stats, mv, lrs, xo[s], S.B("xo", s),
                             g.X3[i * P:(i + 1) * P, :], S.B("X3", i))

    run_phase(g, body2)


_CACHE = {}


def _consts():
    ident = np.eye(P, dtype=np.float32)
    t = np.arange(SEQ)
    row = (t // 64).astype(np.float32)
    col = (t % 64).astype(np.float32)
    freqs = (np.float32(10000.0) ** (-np.arange(0, 64, 2, dtype=np.float32) / np.float32(64))).astype(np.float32)
    ang = np.concatenate([row[:, None] * freqs[None, :], col[:, None] * freqs[None, :]], axis=-1).astype(np.float32)
    cos = np.cos(ang).astype(np.float32)
    sin = np.sin(ang).astype(np.float32)
    iota = np.tile(np.arange(NE, dtype=np.float32)[None, :], (P, 1))
    cbase = np.zeros((P, NE), dtype=np.float32)
    iotab = np.zeros((P, NBLK + 1), dtype=np.float32)
    iotab[:, :NBLK] = (np.arange(NBLK) * P)[None, :]
    iotab[:, NBLK] = np.arange(P)
    triu = np.triu(np.ones((P, P), dtype=np.float32), k=1)
    tokid = np.zeros((P, NT, 2), dtype=np.int32)
    tokid[:, :, 0] = np.arange(NT)[None, :] * P + np.arange(P)[:, None]
    tokid = tokid.reshape(P, NT * 2)
    pidx = np.arange(P, dtype=np.float32).reshape(P, 1)
    bigv = np.zeros((P, 2), dtype=np.float32)
    bigv[P - 1, 1] = 1e9
    ee = np.arange(NE)
    ucum = np.zeros((P, 2, NE), dtype=np.float32)
    for c in range(2):
        ucum[:, c, :] = ((c * P + np.arange(P))[:, None] <= ee[None, :]).astype(np.float32)
    ucum = ucum.reshape(P, 2 * NE)
    return dict(c_pidx=pidx, c_bigv=bigv, c_ucum=ucum, c_ident=ident, c_cos=cos, c_sin=sin, c_iota=iota, c_cbase=cbase, c_triu=triu, c_tokid=tokid, c_iotab=iotab)


def _relayout(w, nk, lite):
    w = np.asarray(w, dtype=np.float32)
    if lite:
        w = w[:, 0:1]
    L, E, R, N = w.shape
    return np.ascontiguousarray(w.reshape(L, E, nk, P, N).transpose(0, 1, 3, 2, 4))


def _merge_w(inputs, l, lite):
    a = _relayout(inputs["exp_w_gate_up"][l:l + 1], KC, lite).reshape(-1, 4096)
    b = _relayout(inputs["exp_w_down"][l:l + 1], 2, lite).reshape(-1, 2048)
    return np.ascontiguousarray(np.concatenate([a, b], axis=1))


def make_in_maps(inputs, lite=False, cores=range(8)):
    f = lambda a: np.ascontiguousarray(np.asarray(a, dtype=np.float32))
    shared = dict(
        ada_w=f(inputs["ada_w"]), ada_b=f(inputs["ada_b"]), ln_g=f(inputs["ln_g"]), ln_b=f(inputs["ln_b"]),
        w_qkv=f(inputs["attn_w_qkv"][0]), qn=f(inputs["attn_q_norm"][0]).reshape(1, P),
        kn=f(inputs["attn_k_norm"][0]).reshape(1, P), w_o=f(inputs["attn_w_o"][0]),
        w_in=f(inputs["conv_w_in"][0]), taps=f(inputs["conv_taps"][0]), w_out=f(inputs["conv_w_out"][0]),
        router_w=f(inputs["router_w"]), router_b=f(inputs["router_bias"]),
        ew0=_merge_w(inputs, 0, lite), ew1=_merge_w(inputs, 1, lite),
        sgu=f(inputs["shared_w_gate_up"]), sdn=f(inputs["shared_w_down"]),
    )
    shared.update(_consts())
    ccT = f(np.asarray(inputs["c_ctx"]).reshape(KC, P).T)
    maps = []
    for b in cores:
        m = dict(shared)
        m["x"] = f(inputs["x"][b])
        m["ctx"] = f(inputs["ctx"][b])
        m["cT"] = f(np.asarray(inputs["c"][b]).reshape(KC, P).T)
        m["ccT"] = ccT
        maps.append(m)
    return maps


def kernel(**inputs):
    if "nc" not in _CACHE:
        _CACHE["nc"] = build_program()
    nc = _CACHE["nc"]
    maps = make_in_maps(inputs)
    res = run_bass_kernel_spmd(nc, maps, core_ids=list(range(8)))
    return np.stack([np.asarray(r["y"], dtype=np.float32) for r in res.results], axis=0)
```
